# Optimizing a Trainium2 kernel written in Bass

```python
import jax, jax.numpy as jnp
from jax import lax
import numpy as np

D_MODEL = 1024
BATCH = 8
SEQ = 4096
DEPTH = 2

CHUNK = 64
N_A_LAYERS = DEPTH // 2
N_B_LAYERS = DEPTH - N_A_LAYERS
N_DENSE = (DEPTH + 1) // 2
N_MOE = DEPTH // 2

MLSTM_HEADS = 8
MLSTM_QK_DIM = D_MODEL // 16
MLSTM_V_DIM = D_MODEL // 8
MLSTM_QK = MLSTM_HEADS * MLSTM_QK_DIM
MLSTM_V = MLSTM_HEADS * MLSTM_V_DIM
MLSTM_PROJ = 2 * MLSTM_QK + 2 * MLSTM_V + 2 * MLSTM_HEADS
GATE_SOFTCAP = 15.0

SB_HEADS = 16
SB_HEAD_DIM = D_MODEL // SB_HEADS
SB_WIDTH = SB_HEADS * SB_HEAD_DIM
Q_BLOCK = 128

FFN_DIM = 2816
N_EXPERTS = 8
TOP_K = 2
EXPERT_DIM = 3584
EPS = 1e-6

kernel_name = "yoco_mlstm_stickbreaking_moe_trunk"


def rmsnorm(x, g):
    xf = x.astype(jnp.float32)
    y = xf * lax.rsqrt(jnp.mean(xf * xf, axis=-1, keepdims=True) + EPS)
    return (y * g.astype(jnp.float32)).astype(x.dtype)


def head_rms(x, g):
    xf = x.astype(jnp.float32)
    return xf * lax.rsqrt(jnp.mean(xf * xf, axis=-1, keepdims=True) + EPS) * g.astype(jnp.float32)


def swiglu(x, w_gate, w_up, w_down):
    return (jax.nn.silu(x @ w_gate) * (x @ w_up)) @ w_down


def soft_cap(z):
    return GATE_SOFTCAP * jnp.tanh(z / GATE_SOFTCAP)


def mlstm_chunkwise(q, k, v, ig, fg):
    B, H, S, dk = q.shape
    dv = v.shape[-1]
    L = CHUNK
    NC = S // L
    q = q.reshape(B, H, NC, L, dk)
    k = k.reshape(B, H, NC, L, dk)
    v = v.reshape(B, H, NC, L, dv)
    ig = ig.reshape(B, H, NC, L)
    b = lax.cumsum(jax.nn.log_sigmoid(fg).reshape(B, H, NC, L), axis=3)
    b_last = b[..., -1]
    g = b_last[..., None] - b + ig
    g_max = jnp.max(g, axis=-1)
    w = jnp.exp(g - g_max[..., None])
    c_loc = jnp.einsum('bhclv,bhclk->bhcvk', v * w[..., None], k)
    n_loc = jnp.einsum('bhcl,bhclk->bhck', w, k)

    def step(carry, inp):
        c, n, m = carry
        cl, nl, bl, gm = inp
        m_new = jnp.maximum(bl + m, gm)
        a = jnp.exp(bl + m - m_new)
        e = jnp.exp(gm - m_new)
        c_new = a[..., None, None] * c + e[..., None, None] * cl
        n_new = a[..., None] * n + e[..., None] * nl
        return (c_new, n_new, m_new), (c, n, m)

    init = (jnp.zeros((B, H, dv, dk), jnp.float32),
            jnp.zeros((B, H, dk), jnp.float32),
            jnp.full((B, H), -jnp.inf, jnp.float32))
    xs = (jnp.moveaxis(c_loc, 2, 0), jnp.moveaxis(n_loc, 2, 0),
          jnp.moveaxis(b_last, 2, 0), jnp.moveaxis(g_max, 2, 0))
    _, (c_prev, n_prev, m_prev) = lax.scan(step, init, xs)
    c_prev = jnp.moveaxis(c_prev, 0, 2)
    n_prev = jnp.moveaxis(n_prev, 0, 2)
    m_prev = jnp.moveaxis(m_prev, 0, 2)

    causal = jnp.tril(jnp.ones((L, L), dtype=bool))
    log_d = jnp.where(causal, b[..., :, None] - b[..., None, :] + ig[..., None, :], -jnp.inf)
    a_inter = b + m_prev[..., None]
    m_t = jnp.maximum(a_inter, jnp.max(log_d, axis=-1))
    s = jnp.einsum('bhctk,bhcsk->bhcts', q, k) * jnp.exp(log_d - m_t[..., None])
    w_inter = jnp.exp(a_inter - m_t)
    num = (jnp.einsum('bhcts,bhcsv->bhctv', s, v)
           + w_inter[..., None] * jnp.einsum('bhctk,bhcvk->bhctv', q, c_prev))
    den = jnp.sum(s, axis=-1) + w_inter * jnp.einsum('bhctk,bhck->bhct', q, n_prev)
    h = num / jnp.maximum(jnp.abs(den), jnp.exp(-m_t))[..., None]
    return h.reshape(B, H, S, dv)


def mlstm_mixer(xn, w_in, b_igate, b_fgate, g_h, w_out):
    B, S, _ = xn.shape
    H = MLSTM_HEADS
    proj = xn @ w_in
    o0 = 0
    q = proj[..., o0:o0 + MLSTM_QK]; o0 += MLSTM_QK
    k = proj[..., o0:o0 + MLSTM_QK]; o0 += MLSTM_QK
    v = proj[..., o0:o0 + MLSTM_V]; o0 += MLSTM_V
    og = proj[..., o0:o0 + MLSTM_V]; o0 += MLSTM_V
    ig = proj[..., o0:o0 + H]; o0 += H
    fg = proj[..., o0:o0 + H]
    to_heads = lambda t, d: jnp.transpose(t.reshape(B, S, H, d), (0, 2, 1, 3)).astype(jnp.float32)
    q = to_heads(q, MLSTM_QK_DIM)
    k = to_heads(k, MLSTM_QK_DIM) * (MLSTM_QK_DIM ** -0.5)
    v = to_heads(v, MLSTM_V_DIM)
    ig = soft_cap(jnp.transpose(ig.astype(jnp.float32) + b_igate, (0, 2, 1)))
    fg = soft_cap(jnp.transpose(fg.astype(jnp.float32) + b_fgate, (0, 2, 1)))
    h = mlstm_chunkwise(q, k, v, ig, fg)
    h = head_rms(h, jnp.ones((), jnp.float32))
    h = jnp.transpose(h, (0, 2, 1, 3)).reshape(B, S, MLSTM_V) * g_h.astype(jnp.float32)
    h = h * jax.nn.sigmoid(og.astype(jnp.float32))
    return h.astype(xn.dtype) @ w_out


def stick_breaking(q, k, v):
    B, H, S, d = q.shape
    NB = S // Q_BLOCK
    qb = jnp.moveaxis(q.reshape(B, H, NB, Q_BLOCK, d), 2, 0)
    key_pos = jnp.arange(S)

    def block(args):
        qi, t0 = args
        z = jnp.einsum('bhtd,bhsd->bhts', qi, k) * (d ** -0.5)
        t_pos = t0 + jnp.arange(Q_BLOCK)
        mask = key_pos[None, :] < t_pos[:, None]
        log_beta = jax.nn.log_sigmoid(z)
        log_1m = jnp.where(mask, log_beta - z, 0.0)
        between = lax.cumsum(log_1m, axis=3, reverse=True) - log_1m
        a = jnp.where(mask, jnp.exp(log_beta + between), 0.0)
        return jnp.einsum('bhts,bhsd->bhtd', a, v)

    out = lax.map(block, (qb, jnp.arange(NB, dtype=jnp.int32) * Q_BLOCK))
    return jnp.moveaxis(out, 0, 2).reshape(B, H, S, d)


def shared_kv(x, kv_norm, w_kv, g_k):
    B, S, _ = x.shape
    kv = rmsnorm(x, kv_norm) @ w_kv
    k = kv[..., :SB_WIDTH].reshape(B, S, SB_HEADS, SB_HEAD_DIM)
    v = kv[..., SB_WIDTH:].reshape(B, S, SB_HEADS, SB_HEAD_DIM)
    k = jnp.transpose(head_rms(k, g_k), (0, 2, 1, 3))
    v = jnp.transpose(v.astype(jnp.float32), (0, 2, 1, 3))
    return k, v


def sb_mixer(xn, k, v, w_q, g_q, w_o):
    B, S, _ = xn.shape
    q = (xn @ w_q).reshape(B, S, SB_HEADS, SB_HEAD_DIM)
    q = jnp.transpose(head_rms(q, g_q), (0, 2, 1, 3))
    o = stick_breaking(q, k, v)
    o = jnp.transpose(o, (0, 2, 1, 3)).reshape(B, S, SB_WIDTH).astype(xn.dtype)
    return o @ w_o


def moe_swiglu(xn, w_router, w_gate, w_up, w_down):
    B, S, D = xn.shape
    t = xn.reshape(B * S, D)
    logits = (t @ w_router).astype(jnp.float32)
    top_val, top_idx = lax.top_k(logits, TOP_K)
    top_w = jax.nn.softmax(top_val, axis=-1)
    combine = jnp.sum(jax.nn.one_hot(top_idx, N_EXPERTS, dtype=jnp.float32) * top_w[..., None], axis=1)
    combine = combine.astype(t.dtype)
    y = jnp.zeros_like(t)
    for e in range(N_EXPERTS):
        y = y + combine[:, e:e + 1] * swiglu(t, w_gate[e], w_up[e], w_down[e])
    return y.reshape(B, S, D)


def _normal(k, shape, scale):
    return jax.random.normal(k, shape, jnp.float32) * scale


def setup_inputs(seed: int = 0) -> dict:
    key = jax.random.key(seed)
    ks = jax.random.split(key, 24)
    D = D_MODEL
    return {
        "x": _normal(ks[0], (BATCH, SEQ, D), 1.0),
        "mix_norm": 1.0 + _normal(ks[1], (DEPTH, D), 0.05),
        "ffn_norm": 1.0 + _normal(ks[2], (DEPTH, D), 0.05),
        "mlstm_w_in": _normal(ks[3], (N_A_LAYERS, D, MLSTM_PROJ), D ** -0.5),
        "mlstm_b_igate": _normal(ks[4], (N_A_LAYERS, MLSTM_HEADS), 0.1),
        "mlstm_b_fgate": 3.0 + _normal(ks[5], (N_A_LAYERS, MLSTM_HEADS), 0.5),
        "mlstm_g_h": 1.0 + _normal(ks[6], (N_A_LAYERS, MLSTM_V), 0.05),
        "mlstm_w_out": _normal(ks[7], (N_A_LAYERS, MLSTM_V, D), MLSTM_V ** -0.5),
        "kv_norm": 1.0 + _normal(ks[8], (D,), 0.05),
        "w_kv": _normal(ks[9], (D, 2 * SB_WIDTH), D ** -0.5),
        "g_k": 1.0 + _normal(ks[10], (SB_HEAD_DIM,), 0.05),
        "sb_w_q": _normal(ks[11], (N_B_LAYERS, D, SB_WIDTH), D ** -0.5),
        "sb_g_q": 1.0 + _normal(ks[12], (N_B_LAYERS, SB_HEAD_DIM), 0.05),
        "sb_w_o": _normal(ks[13], (N_B_LAYERS, SB_WIDTH, D), SB_WIDTH ** -0.5),
        "ffn_w_gate": _normal(ks[14], (N_DENSE, D, FFN_DIM), D ** -0.5),
        "ffn_w_up": _normal(ks[15], (N_DENSE, D, FFN_DIM), D ** -0.5),
        "ffn_w_down": _normal(ks[16], (N_DENSE, FFN_DIM, D), FFN_DIM ** -0.5),
        "moe_w_router": _normal(ks[17], (N_MOE, D, N_EXPERTS), D ** -0.5),
        "moe_w_gate": _normal(ks[18], (N_MOE, N_EXPERTS, D, EXPERT_DIM), D ** -0.5),
        "moe_w_up": _normal(ks[19], (N_MOE, N_EXPERTS, D, EXPERT_DIM), D ** -0.5),
        "moe_w_down": _normal(ks[20], (N_MOE, N_EXPERTS, EXPERT_DIM, D), EXPERT_DIM ** -0.5),
    }


def reference(x, mix_norm, ffn_norm, mlstm_w_in, mlstm_b_igate, mlstm_b_fgate, mlstm_g_h,
              mlstm_w_out, kv_norm, w_kv, g_k, sb_w_q, sb_g_q, sb_w_o, ffn_w_gate, ffn_w_up,
              ffn_w_down, moe_w_router, moe_w_gate, moe_w_up, moe_w_down):
    k_sh = None
    v_sh = None
    for layer in range(DEPTH):
        xn = rmsnorm(x, mix_norm[layer])
        if layer < N_A_LAYERS:
            x = x + mlstm_mixer(xn, mlstm_w_in[layer], mlstm_b_igate[layer], mlstm_b_fgate[layer],
                                mlstm_g_h[layer], mlstm_w_out[layer])
        else:
            if layer == N_A_LAYERS:
                k_sh, v_sh = shared_kv(x, kv_norm, w_kv, g_k)
                xn = rmsnorm(x, mix_norm[layer])
            j = layer - N_A_LAYERS
            x = x + sb_mixer(xn, k_sh, v_sh, sb_w_q[j], sb_g_q[j], sb_w_o[j])
        xn = rmsnorm(x, ffn_norm[layer])
        if layer % 2 == 0:
            i = layer // 2
            x = x + swiglu(xn, ffn_w_gate[i], ffn_w_up[i], ffn_w_down[i])
        else:
            i = layer // 2
            x = x + moe_swiglu(xn, moe_w_router[i], moe_w_gate[i], moe_w_up[i], moe_w_down[i])
    return x
```

```python
from contextlib import ExitStack
import concourse.bass as bass
import concourse.mybir as mybir

F32 = mybir.dt.float32
BF16 = mybir.dt.bfloat16
I32 = mybir.dt.int32
AF = mybir.ActivationFunctionType
ALU = mybir.AluOpType
AX = mybir.AxisListType

COMPUTE = ("pe", "act", "dve", "pool")
NDMA_SLOTS = 16


class Res:
    __slots__ = ("name", "w", "r", "excl")

    def __init__(self, name):
        self.name = name
        self.excl = False
        self.w = None
        self.r = []


class Instr:
    __slots__ = ("eng", "fn", "deps", "need_inc", "inc_idx", "is_dma", "slot", "slot_val", "prev_slot_val", "pos")

    def __init__(self, eng, fn):
        self.eng = eng
        self.fn = fn
        self.deps = []
        self.need_inc = False
        self.inc_idx = 0
        self.is_dma = False
        self.slot = None
        self.slot_val = 0
        self.prev_slot_val = 0
        self.pos = 0


_GSEM = {}


class Prog:
    def __init__(self, nc, prefix=""):
        self.nc = nc
        self.prefix = prefix
        self.stack = ExitStack()
        self.streams = {e: [] for e in ("pe", "act", "dve", "pool", "sp")}
        self.dma_count = {e: 0 for e in ("act", "pool", "sp")}
        self.nres = 0
        self.max_ops = None
        self.nops = 0

    def sbuf(self, name, shape, dtype):
        return self.stack.enter_context(self.nc.sbuf_tensor(self.prefix + "sb_" + name, list(shape), dtype))

    def psum(self, name, shape, dtype):
        return self.stack.enter_context(self.nc.psum_tensor(self.prefix + "ps_" + name, list(shape), dtype))

    def res(self, name=None):
        self.nres += 1
        return Res(name or f"r{self.nres}")

    def ress(self, n, name="r"):
        return [self.res(f"{name}{i}") for i in range(n)]

    def _track(self, ins, reads, writes):
        writes = list(writes) + [r for r in reads if r.excl and r not in writes]
        reads = [r for r in reads if not r.excl]
        deps = []
        for r in reads:
            if r.w is not None:
                deps.append(r.w)
        for w in writes:
            if w.w is not None:
                deps.append(w.w)
            deps.extend(w.r)
        for r in reads:
            r.r.append(ins)
        for w in writes:
            w.w = ins
            w.r = []
        seen = set()
        for d in deps:
            if d is ins or id(d) in seen:
                continue
            seen.add(id(d))
            if d.eng == "pe" and ins.eng == "pe" and not d.is_dma and not ins.is_dma:
                continue
            ins.deps.append(d)

    def begin_capture(self):
        self._cap = []

    def end_capture(self):
        c, self._cap = self._cap, None
        return c

    def replay_interleaved(self, a, b, chunk=1):
        ca = [a[i:i + chunk] for i in range(0, len(a), chunk)]
        nb = max(1, (len(b) * chunk + len(a) - 1) // max(1, len(a)))
        cb = [b[i:i + nb] for i in range(0, len(b), nb)]
        seq = []
        for i in range(max(len(ca), len(cb))):
            if i < len(ca):
                seq += ca[i]
            if i < len(cb):
                seq += cb[i]
        for k, args, kw in seq:
            (self.op if k == "op" else self.dma)(*args, **kw)

    def op(self, eng, fn, reads=(), writes=()):
        if getattr(self, "_cap", None) is not None:
            self._cap.append(("op", (eng, fn), dict(reads=list(reads), writes=list(writes))))
            return None
        self.nops += 1
        if self.max_ops is not None and self.nops > self.max_ops:
            return None
        ins = Instr(eng, fn)
        ins.pos = len(self.streams[eng])
        self.streams[eng].append(ins)
        self._track(ins, list(reads), list(writes))
        return ins

    def dma(self, q, out, in_, reads=(), writes=(), fn=None, **kw):
        if getattr(self, "_cap", None) is not None:
            self._cap.append(("dma", (q, out, in_), dict(reads=list(reads), writes=list(writes), fn=fn, **kw)))
            return None
        if fn is None:
            def fn(e):
                return e.dma_start(out=out, in_=in_, **kw)
        self.nops += 1
        if self.max_ops is not None and self.nops > self.max_ops:
            return None
        ins = Instr(q, fn)
        ins.is_dma = True
        n = self.dma_count[q]
        self.dma_count[q] = n + 1
        ins.slot = (q, n % NDMA_SLOTS)
        ins.slot_val = 16 * (n // NDMA_SLOTS + 1)
        ins.prev_slot_val = ins.slot_val - 16
        ins.pos = len(self.streams[q])
        self.streams[q].append(ins)
        self._track(ins, list(reads), list(writes))
        return ins

    def emit(self):
        nc = self.nc
        for e, st in self.streams.items():
            for ins in st:
                for d in ins.deps:
                    if not d.is_dma:
                        d.need_inc = True
        for e, st in self.streams.items():
            c = 0
            for ins in st:
                if not ins.is_dma and ins.need_inc:
                    c += 1
                    ins.inc_idx = c
        G = _GSEM.setdefault(id(nc), None)
        if G is None:
            G = {"sems": {}, "dsem": {}, "base": {}}
            for e in COMPUTE:
                G["sems"][e] = nc.semaphore(f"g_s_{e}").__enter__()
            for q in ("act", "pool", "sp"):
                for s in range(NDMA_SLOTS):
                    G["dsem"][(q, s)] = nc.semaphore(f"g_d_{q}{s}").__enter__()
            _GSEM[id(nc)] = G
        sems, dsem, base = G["sems"], G["dsem"], G["base"]
        for e, st in self.streams.items():
            for ins in st:
                if ins.is_dma:
                    b0 = base.get(ins.slot, 0)
                    ins.slot_val += b0
                    ins.prev_slot_val += b0
                else:
                    ins.inc_idx += base.get(e, 0)
        for e in COMPUTE:
            n_inc = sum(1 for ins in self.streams[e] if (not ins.is_dma) and ins.need_inc)
            base[e] = base.get(e, 0) + n_inc
        for q in ("act", "pool", "sp"):
            for ins in self.streams[q]:
                if ins.is_dma:
                    base[ins.slot] = max(base.get(ins.slot, 0), ins.slot_val)
        print("[fw] phase", self.prefix, "sem values:", {e: base.get(e, 0) for e in COMPUTE}, flush=True)
        block = self.stack.enter_context(nc.Block())
        streams = self.streams
        final_waits = []
        last_slot_val = {}
        for q in ("act", "pool", "sp"):
            for ins in streams[q]:
                if ins.is_dma:
                    last_slot_val[ins.slot] = ins.slot_val

        def run(ename, eng):
            known = {}

            def wait(sem_key, sem, val):
                if known.get(sem_key, 0) >= val:
                    return
                known[sem_key] = val
                eng.wait_ge(sem, val)

            for ins in streams[ename]:
                for d in ins.deps:
                    if d.is_dma:
                        wait(d.slot, dsem[d.slot], d.slot_val)
                    else:
                        wait(d.eng, sems[d.eng], d.inc_idx)
                if ins.is_dma:
                    if ins.prev_slot_val > 0 and not getattr(ins, 'first_use', False):
                        wait(ins.slot, dsem[ins.slot], ins.prev_slot_val)
                    bi = ins.fn(eng)
                    bi.then_inc(dsem[ins.slot], 16)
                else:
                    bi = ins.fn(eng)
                    if ins.need_inc:
                        bi.then_inc(sems[ename], 1)
            if ename == "sp":
                for slot, v in last_slot_val.items():
                    wait(slot, dsem[slot], v)

        @block.sync
        def _(e):
            run("sp", e)

        @block.tensor
        def _(e):
            run("pe", e)

        @block.scalar
        def _(e):
            run("act", e)

        @block.vector
        def _(e):
            run("dve", e)

        @block.gpsimd
        def _(e):
            run("pool", e)

    def close(self):
        self.stack.close()


D = 1024
S = 4096
NTILES = S // 128
NEG = -30000.0


class Buf:
    def __init__(self, P, name, shape, dtype, psum=False):
        self.t = (P.psum if psum else P.sbuf)(name, shape, dtype)
        self.r = P.res(name)
        self.r.excl = psum


def make_consts(P):
    C = {}
    ident = Buf(P, "ident", [128, 128], BF16)
    identf = Buf(P, "identf", [128, 128], F32)
    U = Buf(P, "U", [128, 128], F32)
    onesf = Buf(P, "onesf", [128, 128], F32)
    maskneg = Buf(P, "maskneg", [128, 128], F32)
    epsb = Buf(P, "epsb", [128, 1], F32)
    one1 = Buf(P, "one1", [128, 1], F32)
    P.op("pool", lambda e: e.memset(epsb.t[:], 1e-6), writes=[epsb.r])
    P.op("pool", lambda e: e.memset(one1.t[:], 1.0), writes=[one1.r])
    P.op("pool", lambda e: e.memset(onesf.t[:], 1.0), writes=[onesf.r])
    P.op("pool", lambda e: e.memset(identf.t[:], 1.0), writes=[identf.r])
    P.op("pool", lambda e: e.affine_select(out=identf.t[:], in_=identf.t[:], pattern=[[-1, 128]],
                                           compare_op=ALU.is_equal, fill=0.0, base=0, channel_multiplier=1),
         reads=[identf.r], writes=[identf.r])
    P.op("dve", lambda e: e.tensor_copy(out=ident.t[:], in_=identf.t[:]), reads=[identf.r], writes=[ident.r])
    P.op("pool", lambda e: e.affine_select(out=U.t[:], in_=onesf.t[:], pattern=[[1, 128]],
                                           compare_op=ALU.is_ge, fill=0.0, base=0, channel_multiplier=-1),
         reads=[onesf.r], writes=[U.r])
    P.op("pool", lambda e: e.memset(maskneg.t[:], 0.0), writes=[maskneg.r])
    P.op("pool", lambda e: e.affine_select(out=maskneg.t[:], in_=maskneg.t[:], pattern=[[1, 128]],
                                           compare_op=ALU.is_ge, fill=NEG, base=0, channel_multiplier=-1),
         reads=[maskneg.r], writes=[maskneg.r])
    C.update(ident=ident, identf=identf, U=U, onesf=onesf, maskneg=maskneg, epsb=epsb, one1=one1)
    return C


def rmsnorm_T(P, C, xt, gbc, ss, junk, xn, pT, xnT):
    P.op("act", lambda e: e.activation(out=junk.t[:], in_=xt.t[:], func=AF.Square, accum_out=ss.t[:]),
         reads=[xt.r], writes=[junk.r, ss.r])
    P.op("act", lambda e: e.activation(out=ss.t[:], in_=ss.t[:], func=AF.Sqrt, scale=1.0 / D, bias=C["epsb"].t[:]),
         reads=[ss.r, C["epsb"].r], writes=[ss.r])
    P.op("dve", lambda e: e.reciprocal(out=ss.t[:], in_=ss.t[:]), reads=[ss.r], writes=[ss.r])
    P.op("dve", lambda e: e.scalar_tensor_tensor(out=xn.t[:], in0=xt.t[:], scalar=ss.t[:, 0:1], in1=gbc.t[:],
                                                 op0=ALU.mult, op1=ALU.mult),
         reads=[xt.r, ss.r, gbc.r], writes=[xn.r])

    def tr(e):
        for c in range(8):
            i = e.transpose(out=pT.t[:, c, :], in_=xn.t[:, c * 128:(c + 1) * 128], identity=C["ident"].t[:])
        return i
    P.op("pe", tr, reads=[xn.r, C["ident"].r], writes=[pT.r])
    P.op("act", lambda e: e.copy(out=xnT.t[:], in_=pT.t[:]), reads=[pT.r], writes=[xnT.r])


def phase1(P, C, io, ntiles=NTILES):
    x, x1 = io["x"], io["x1"]
    w_in_d, w_out_d = io["w_in"], io["w_out"]
    w_in = Buf(P, "w_in", [128, 8, 3088], BF16)
    w_out = Buf(P, "w_out", [128, 8, 1024], BF16)
    gbc = Buf(P, "gbc1", [128, D], F32)
    ghbc = Buf(P, "ghbc", [128, D], F32)
    bgb = Buf(P, "bgb", [128, 16], F32)
    w_in_v = w_in_d.rearrange("(c p) n -> p c n", p=128)
    for i, (a, b) in enumerate([(3072, 3088), (0, 1024), (1024, 2048), (2048, 3072)]):
        P.dma("pool", w_in.t[:, :, a:b], w_in_v[:, :, a:b], writes=[w_in.r])
    P.dma("pool", w_out.t[:], w_out_d.rearrange("(c p) n -> p c n", p=128), writes=[w_out.r])
    P.dma("sp", gbc.t[:], io["mix_norm0"].partition_broadcast(128), writes=[gbc.r])
    P.dma("sp", ghbc.t[:], io["g_h"].partition_broadcast(128), writes=[ghbc.r])
    P.dma("sp", bgb.t[:, 0:8], io["b_ig"].partition_broadcast(128), writes=[bgb.r])
    P.dma("sp", bgb.t[:, 8:16], io["b_fg"].partition_broadcast(128), writes=[bgb.r])

    xt = [Buf(P, f"xt{i}", [128, D], F32) for i in range(2)]
    junk = Buf(P, "junk", [128, D], F32)
    ss = Buf(P, "ss", [128, 1], F32)
    xn = Buf(P, "xn", [128, D], BF16)
    xnT = Buf(P, "xnT", [128, 8, 128], BF16)
    gt = Buf(P, "gt", [128, 16], F32)
    th = Buf(P, "th", [128, 16], F32)
    e1 = Buf(P, "e1", [128, 8], F32)
    logf = Buf(P, "logf", [128, 8], F32)
    logf_rep = Buf(P, "logf_rep", [128, 8, 128], F32)
    cc = Buf(P, "cc", [128, 8], F32)
    wgt = Buf(P, "wgt", [128, 8], F32)
    dec = Buf(P, "dec", [128, 4], F32)
    EBp = Buf(P, "EBp", [128, 4, 128], F32)
    z1 = Buf(P, "z1", [128, 8, 128], F32)
    z2 = Buf(P, "z2", [128, 8, 128], F32)
    E = Buf(P, "E", [128, 8, 128], F32)
    qT = Buf(P, "qT", [128, 4, 128], BF16)
    kT = Buf(P, "kT", [128, 8, 128], BF16)
    QsT = Buf(P, "QsT", [128, 8, 128], BF16)
    ktok = Buf(P, "ktok", [128, 512], F32)
    Kw = Buf(P, "Kw", [128, 4, 2, 2, 64], BF16)
    v_sb = Buf(P, "v_sb", [128, 8, 129], BF16)
    sg = Buf(P, "sg", [128, D], F32)
    ghsg = Buf(P, "ghsg", [128, D], F32)
    PT = Buf(P, "PT", [128, 8, 128], BF16)
    Cs = Buf(P, "Cs", [128, 4, 128], F32)
    Ct = Buf(P, "Ct", [128, 4, 128], F32)
    ns = Buf(P, "ns", [128, 4], F32)
    Cb = Buf(P, "Cb", [128, 4, 129], BF16)
    rr = Buf(P, "rr", [128, 8], F32)
    ssq = Buf(P, "ssq", [128, 8], F32)
    sc = Buf(P, "sc", [128, 8], F32)
    hh = Buf(P, "hh", [128, 8, 128], F32)
    hg = Buf(P, "hg", [128, D], BF16)
    hgT = Buf(P, "hgT", [128, 8, 128], BF16)
    xo = [Buf(P, f"xo{i}", [128, D], F32) for i in range(2)]
    pT = Buf(P, "pT", [128, 8, 128], BF16, psum=True)
    pQK = Buf(P, "pQK", [128, 8, 128], F32, psum=True)
    pPJ = [Buf(P, f"pPJ{i}", [128, 512], F32, psum=True) for i in range(2)]
    pG = Buf(P, "pG", [128, 512], F32, psum=True)
    pBB = Buf(P, "pBB", [128, 8, 128], F32, psum=True)
    rG = {k: pG.r for k in ("gates", "b", "den", "dn")}

    P.op("pool", lambda e: e.memset(Kw.t[:], 0.0), writes=[Kw.r])
    P.op("pool", lambda e: e.memset(kT.t[:], 0.0), writes=[kT.r])
    P.op("pool", lambda e: e.memset(QsT.t[:], 0.0), writes=[QsT.r])
    P.op("pool", lambda e: e.memset(v_sb.t[:], 1.0), writes=[v_sb.r])
    P.op("pool", lambda e: e.memset(Cs.t[:], 0.0), writes=[Cs.r])
    P.op("pool", lambda e: e.memset(ns.t[:], 0.0), writes=[ns.r])
    P.op("pool", lambda e: e.memset(Cb.t[:], 0.0), writes=[Cb.r])

    U, onesf, maskneg = C["U"], C["onesf"], C["maskneg"]

    def proj_tok(col0, ncols, pout, outr):
        def f(e):
            for c in range(8):
                i = e.matmul(pout, lhsT=xnT.t[:, c, :], rhs=w_in.t[:, c, col0:col0 + ncols],
                             start=(c == 0), stop=(c == 7))
            return i
        return f

    P.dma("sp", xt[0].t[:], x[0:128, :], writes=[xt[0].r])
    for t in range(ntiles):
        b = t % 2
        X = xt[b]
        if t + 1 < ntiles:
            P.dma("sp", xt[1 - b].t[:], x[(t + 1) * 128:(t + 2) * 128, :], writes=[xt[1 - b].r])
        rmsnorm_T(P, C, X, gbc, ss, junk, xn, pT, xnT)
        P.op("pe", proj_tok(3072, 16, pG.t[:, 0:16], None), reads=[xnT.r, w_in.r], writes=[rG["gates"]])
        P.op("dve", lambda e: e.tensor_tensor(out=gt.t[:], in0=pG.t[:, 0:16], in1=bgb.t[:], op=ALU.add),
             reads=[rG["gates"], bgb.r], writes=[gt.r])
        P.op("act", lambda e: e.activation(out=th.t[:], in_=gt.t[:], func=AF.Tanh, scale=1.0 / 15.0),
             reads=[gt.r], writes=[th.r])
        P.op("act", lambda e: e.activation(out=e1.t[:], in_=th.t[:, 8:16], func=AF.Exp, scale=-15.0),
             reads=[th.r], writes=[e1.r])
        P.op("act", lambda e: e.activation(out=e1.t[:], in_=e1.t[:], func=AF.Ln, bias=C["one1"].t[:]),
             reads=[e1.r, C["one1"].r], writes=[e1.r])
        P.op("dve", lambda e: e.tensor_scalar(out=logf.t[:], in0=e1.t[:], scalar1=-1.0, scalar2=None, op0=ALU.mult),
             reads=[e1.r], writes=[logf.r])
        P.op("dve", lambda e: e.tensor_copy(out=logf_rep.t[:], in_=logf.t[:, :].unsqueeze(2).to_broadcast([128, 8, 128])),
             reads=[logf.r], writes=[logf_rep.r])

        def cums(e):
            e.matmul(pG.t[:, 16:24], lhsT=U.t[:], rhs=logf.t[:], start=True, stop=True)
            i = e.matmul(pG.t[:, 24:32], lhsT=onesf.t[:], rhs=logf.t[:], start=True, stop=True)
            return i
        P.op("pe", cums, reads=[U.r, onesf.r, logf.r], writes=[rG["b"]])

        def bbc(e):
            for h in range(8):
                i = e.matmul(pBB.t[:, h, :], lhsT=logf_rep.t[:, h, :], rhs=U.t[:], start=True, stop=True)
            return i
        P.op("pe", bbc, reads=[logf_rep.r, U.r], writes=[pBB.r])
        P.op("dve", lambda e: e.scalar_tensor_tensor(out=cc.t[:], in0=th.t[:, 0:8], scalar=15.0, in1=pG.t[:, 16:24],
                                                     op0=ALU.mult, op1=ALU.subtract),
             reads=[th.r, rG["b"]], writes=[cc.r])
        P.op("dve", lambda e: e.tensor_tensor(out=wgt.t[:], in0=pG.t[:, 24:32], in1=cc.t[:], op=ALU.add),
             reads=[rG["b"], cc.r], writes=[wgt.r])
        P.op("act", lambda e: e.activation(out=wgt.t[:], in_=wgt.t[:], func=AF.Exp), reads=[wgt.r], writes=[wgt.r])
        for hf in range(2):
            lo, hi = hf * 64, hf * 64 + 64
            P.op("act", lambda e, lo=lo, hi=hi, hf=hf: e.activation(out=dec.t[lo:hi, :], in_=pG.t[lo:hi, 24 + hf:32:2], func=AF.Exp),
                 reads=[rG["b"]], writes=[dec.r])
            P.op("act", lambda e, lo=lo, hi=hi, hf=hf: e.activation(out=EBp.t[lo:hi, :, :], in_=pBB.t[lo:hi, hf:8:2, :], func=AF.Exp),
                 reads=[pBB.r], writes=[EBp.r])
        P.op("dve", lambda e: e.tensor_tensor(out=z1.t[:], in0=pBB.t[:], in1=maskneg.t[:, :].unsqueeze(1).to_broadcast([128, 8, 128]),
                                              op=ALU.add), reads=[pBB.r, maskneg.r], writes=[z1.r])
        P.op("pool", lambda e: e.tensor_tensor(out=z2.t[:], in0=z1.t[:], in1=cc.t[:, :].unsqueeze(2).to_broadcast([128, 8, 128]),
                                               op=ALU.add), reads=[z1.r, cc.r], writes=[z2.r])
        P.op("act", lambda e: e.activation(out=E.t[:], in_=z2.t[:], func=AF.Exp), reads=[z2.r], writes=[E.r])

        def qk(e):
            for m in range(8):
                for c in range(8):
                    i = e.matmul(pQK.t[:, m, :], lhsT=w_in.t[:, c, m * 128:(m + 1) * 128], rhs=xnT.t[:, c, :],
                                 start=(c == 0), stop=(c == 7))
            return i
        P.op("pe", qk, reads=[w_in.r, xnT.r], writes=[pQK.r])
        P.op("act", lambda e: e.copy(out=qT.t[:], in_=pQK.t[:, 0:4, :]), reads=[pQK.r], writes=[qT.r])
        for hf in range(2):
            lo, hi = hf * 64, hf * 64 + 64
            P.op("act", lambda e, lo=lo, hi=hi, hf=hf: e.mul(out=kT.t[lo:hi, hf:8:2, :], in_=pQK.t[lo:hi, 4:8, :], mul=0.125),
                 reads=[pQK.r], writes=[kT.r])
            P.op("dve", lambda e, lo=lo, hi=hi, hf=hf: e.tensor_tensor(out=QsT.t[lo:hi, hf:8:2, :], in0=pQK.t[lo:hi, 0:4, :],
                                                                   in1=EBp.t[lo:hi, :, :], op=ALU.mult),
                 reads=[pQK.r, EBp.r], writes=[QsT.r])
        P.op("pe", proj_tok(512, 512, pPJ[0].t[:], None), reads=[xnT.r, w_in.r], writes=[pPJ[0].r])
        P.op("act", lambda e: e.mul(out=ktok.t[:], in_=pPJ[0].t[:], mul=0.125), reads=[pPJ[0].r], writes=[ktok.r])
        for ee in range(2):
            P.op("dve", lambda e, ee=ee: e.tensor_tensor(
                out=Kw.t[:, :, ee, ee, :],
                in0=ktok.t[:, :].rearrange("p (j e k) -> p j e k", j=4, e=2)[:, :, ee, :],
                in1=wgt.t[:, :].rearrange("p (j e) -> p j e", e=2)[:, :, ee:ee + 1].to_broadcast([128, 4, 64]),
                op=ALU.mult), reads=[ktok.r, wgt.r], writes=[Kw.r])
        P.op("pe", proj_tok(1024, 512, pPJ[1].t[:], None), reads=[xnT.r, w_in.r], writes=[pPJ[1].r])
        P.op("act", lambda e: e.copy(out=v_sb.t[:, 0:4, 0:128], in_=pPJ[1].t[:, :].rearrange("p (h d) -> p h d", h=4)),
             reads=[pPJ[1].r], writes=[v_sb.r])
        P.op("pe", proj_tok(1536, 512, pPJ[0].t[:], None), reads=[xnT.r, w_in.r], writes=[pPJ[0].r])
        P.op("act", lambda e: e.copy(out=v_sb.t[:, 4:8, 0:128], in_=pPJ[0].t[:, :].rearrange("p (h d) -> p h d", h=4)),
             reads=[pPJ[0].r], writes=[v_sb.r])
        P.op("pe", proj_tok(2048, 512, pPJ[1].t[:], None), reads=[xnT.r, w_in.r], writes=[pPJ[1].r])
        P.op("act", lambda e: e.activation(out=sg.t[:, 0:512], in_=pPJ[1].t[:], func=AF.Sigmoid),
             reads=[pPJ[1].r], writes=[sg.r])
        P.op("pe", proj_tok(2560, 512, pPJ[0].t[:], None), reads=[xnT.r, w_in.r], writes=[pPJ[0].r])
        P.op("act", lambda e: e.activation(out=sg.t[:, 512:1024], in_=pPJ[0].t[:], func=AF.Sigmoid),
             reads=[pPJ[0].r], writes=[sg.r])
        P.op("pool", lambda e: e.tensor_tensor(out=ghsg.t[:], in0=sg.t[:], in1=ghbc.t[:], op=ALU.mult),
             reads=[sg.r, ghbc.r], writes=[ghsg.r])

        def st(e):
            for h in range(8):
                j, ee = h // 2, h % 2
                lo, hi = ee * 64, ee * 64 + 64
                i = e.matmul(pQK.t[:, h, :], lhsT=kT.t[:, h, :], rhs=qT.t[:, j, :], start=True, stop=True)
            return i
        P.op("pe", st, reads=[kT.r, qT.r], writes=[pQK.r])
        P.op("dve", lambda e: e.tensor_tensor(out=PT.t[:], in0=pQK.t[:], in1=E.t[:], op=ALU.mult),
             reads=[pQK.r, E.r], writes=[PT.r])

        def numden(e):
            for h in range(8):
                j, ee = h // 2, h % 2
                lo, hi = ee * 64, ee * 64 + 64
                e.matmul(pBB.t[:, h, :], lhsT=PT.t[:, h, :], rhs=v_sb.t[:, h, 0:128], start=True, stop=False)
                e.matmul(pBB.t[:, h, :], lhsT=QsT.t[:, h, :], rhs=Cb.t[:, j, 0:128], start=False, stop=True)
            for h in range(8):
                j, ee = h // 2, h % 2
                lo, hi = ee * 64, ee * 64 + 64
                e.matmul(pG.t[:, 32 + h:33 + h], lhsT=PT.t[:, h, :], rhs=v_sb.t[:, h, 128:129], start=True, stop=False)
                i = e.matmul(pG.t[:, 32 + h:33 + h], lhsT=QsT.t[:, h, :], rhs=Cb.t[:, j, 128:129], start=False, stop=True)
            return i
        P.op("pe", numden, reads=[PT.r, v_sb.r, QsT.r, Cb.r], writes=[pBB.r, rG["den"]])

        def dstate(e):
            for j in range(4):
                e.matmul(pPJ[1].t[:, j * 128:(j + 1) * 128], lhsT=Kw.t[:, j, 0, :, :], rhs=v_sb.t[:, 2 * j, 0:128],
                         start=True, stop=False)
                e.matmul(pPJ[1].t[:, j * 128:(j + 1) * 128], lhsT=Kw.t[:, j, 1, :, :], rhs=v_sb.t[:, 2 * j + 1, 0:128],
                         start=False, stop=True)
            for j in range(4):
                e.matmul(pG.t[:, 40 + j:41 + j], lhsT=Kw.t[:, j, 0, :, :], rhs=v_sb.t[:, 2 * j, 128:129],
                         start=True, stop=False)
                i = e.matmul(pG.t[:, 40 + j:41 + j], lhsT=Kw.t[:, j, 1, :, :], rhs=v_sb.t[:, 2 * j + 1, 128:129],
                             start=False, stop=True)
            return i
        P.op("pe", dstate, reads=[Kw.r, v_sb.r], writes=[pPJ[1].r, rG["dn"]])
        P.op("pool", lambda e: e.tensor_tensor(out=Ct.t[:], in0=Cs.t[:], in1=dec.t[:, :].unsqueeze(2).to_broadcast([128, 4, 128]),
                                               op=ALU.mult), reads=[Cs.r, dec.r], writes=[Ct.r])
        P.op("dve", lambda e: e.tensor_tensor(out=Cs.t[:], in0=Ct.t[:], in1=pPJ[1].t[:, :].rearrange("p (j d) -> p j d", j=4),
                                              op=ALU.add), reads=[Ct.r, pPJ[1].r], writes=[Cs.r])
        P.op("dve", lambda e: e.tensor_tensor(out=ns.t[:], in0=ns.t[:], in1=dec.t[:], op=ALU.mult),
             reads=[ns.r, dec.r], writes=[ns.r])
        P.op("dve", lambda e: e.tensor_tensor(out=ns.t[:], in0=ns.t[:], in1=pG.t[:, 40:44], op=ALU.add),
             reads=[ns.r, rG["dn"]], writes=[ns.r])
        P.op("act", lambda e: e.copy(out=Cb.t[:, :, 0:128], in_=Cs.t[:]), reads=[Cs.r], writes=[Cb.r])
        P.op("act", lambda e: e.copy(out=Cb.t[:, :, 128:129], in_=ns.t[:, :].unsqueeze(2)), reads=[ns.r], writes=[Cb.r])

        P.op("act", lambda e: e.activation(out=rr.t[:], in_=pG.t[:, 32:40], func=AF.Abs), reads=[rG["den"]], writes=[rr.r])
        P.op("dve", lambda e: e.tensor_scalar(out=rr.t[:], in0=rr.t[:], scalar1=1.0, scalar2=None, op0=ALU.max),
             reads=[rr.r], writes=[rr.r])
        P.op("dve", lambda e: e.reciprocal(out=rr.t[:], in_=rr.t[:]), reads=[rr.r], writes=[rr.r])
        P.op("act", lambda e: e.activation(out=hh.t[:], in_=pBB.t[:], func=AF.Square), reads=[pBB.r], writes=[hh.r])
        P.op("dve", lambda e: e.tensor_reduce(out=ssq.t[:], in_=hh.t[:], axis=AX.X, op=ALU.add), reads=[hh.r], writes=[ssq.r])
        P.op("dve", lambda e: e.tensor_tensor(out=ssq.t[:], in0=ssq.t[:], in1=rr.t[:], op=ALU.mult), reads=[ssq.r, rr.r], writes=[ssq.r])
        P.op("dve", lambda e: e.tensor_tensor(out=ssq.t[:], in0=ssq.t[:], in1=rr.t[:], op=ALU.mult), reads=[ssq.r, rr.r], writes=[ssq.r])
        P.op("act", lambda e: e.activation(out=ssq.t[:], in_=ssq.t[:], func=AF.Sqrt, scale=1.0 / 128.0, bias=C["epsb"].t[:]),
             reads=[ssq.r, C["epsb"].r], writes=[ssq.r])
        P.op("dve", lambda e: e.reciprocal(out=ssq.t[:], in_=ssq.t[:]), reads=[ssq.r], writes=[ssq.r])
        P.op("dve", lambda e: e.tensor_tensor(out=sc.t[:], in0=ssq.t[:], in1=rr.t[:], op=ALU.mult), reads=[ssq.r, rr.r], writes=[sc.r])
        P.op("dve", lambda e: e.tensor_tensor(out=hh.t[:], in0=pBB.t[:], in1=sc.t[:, :].unsqueeze(2).to_broadcast([128, 8, 128]),
                                              op=ALU.mult), reads=[pBB.r, sc.r], writes=[hh.r])
        P.op("dve", lambda e: e.tensor_tensor(out=hg.t[:], in0=hh.t[:, :, :].rearrange("p h d -> p (h d)"), in1=ghsg.t[:], op=ALU.mult),
             reads=[hh.r, ghsg.r], writes=[hg.r])

        def tr2(e):
            for c in range(8):
                i = e.transpose(out=pT.t[:, c, :], in_=hg.t[:, c * 128:(c + 1) * 128], identity=C["ident"].t[:])
            return i
        P.op("pe", tr2, reads=[hg.r, C["ident"].r], writes=[pT.r])
        P.op("act", lambda e: e.copy(out=hgT.t[:], in_=pT.t[:]), reads=[pT.r], writes=[hgT.r])
        XO = xo[b]
        for n in range(2):
            def op_(e, n=n):
                for c in range(8):
                    i = e.matmul(pPJ[n].t[:], lhsT=hgT.t[:, c, :], rhs=w_out.t[:, c, n * 512:(n + 1) * 512],
                                 start=(c == 0), stop=(c == 7))
                return i
            P.op("pe", op_, reads=[hgT.r, w_out.r], writes=[pPJ[n].r])
            P.op("dve", lambda e, n=n, XO=XO, X=X: e.tensor_tensor(out=XO.t[:, n * 512:(n + 1) * 512], in0=X.t[:, n * 512:(n + 1) * 512],
                                                                in1=pPJ[n].t[:], op=ALU.add),
                 reads=[X.r, pPJ[n].r], writes=[XO.r])
        P.dma("sp", x1[t * 128:(t + 1) * 128, :], XO.t[:], reads=[XO.r])


D = 1024
S = 4096


def ffn_phase(P, C, io, E, F, NTOK=1024, nsg=None, tag="f"):
    x_in, x_out = io["x_in"], io["x_out"]
    wg_d, wu_d, wd_d = io["wg"], io["wu"], io["wd"]
    moe = E > 1
    NFC = F // 128
    NTT = NTOK // 128
    NTG = NTOK // 512
    NSG = S // NTOK if nsg is None else nsg
    ident, identf = C["ident"], C["identf"]

    gbc = Buf(P, tag + "gbc", [128, D], F32)
    P.dma("sp", gbc.t[:], io["norm_g"].partition_broadcast(128), writes=[gbc.r])
    xt = [Buf(P, f"{tag}xt{i}", [128, D], F32) for i in range(2)]
    junk = Buf(P, tag + "junk", [128, D], BF16)
    ss = Buf(P, tag + "ss", [128, 1], F32)
    xn = Buf(P, tag + "xn", [128, D], BF16)
    xnT = Buf(P, tag + "xnT", [128, 8, NTOK], BF16)
    hT = Buf(P, tag + "hT", [128, NFC, NTOK], BF16)
    wgs = [Buf(P, f"{tag}wg{i}", [128, 8, 128], BF16) for i in range(3)]
    wus = [Buf(P, f"{tag}wu{i}", [128, 8, 128], BF16) for i in range(3)]
    wds = [Buf(P, f"{tag}wd{i}", [128, NFC, 512], BF16) for i in range(2)]
    sgt = [Buf(P, f"{tag}sg{i}", [128, 512], F32) for i in range(2)]
    acc = Buf(P, tag + "acc", [128, NTT, D], F32)
    racc = [P.res(f"acc{i}") for i in range(NTT)]
    pT = Buf(P, tag + "pT", [128, 8, 128], BF16, psum=True)
    pF = Buf(P, tag + "pF", [128, 4, 128], F32, psum=True)
    pA = [Buf(P, f"{tag}pA{i}", [128, 512], F32, psum=True) for i in range(2)]
    pB = [Buf(P, f"{tag}pB{i}", [128, 512], F32, psum=True) for i in range(2)]
    pY = [Buf(P, f"{tag}pY{i}", [128, 512], F32, psum=True) for i in range(2)]
    if moe:
        xnf = Buf(P, tag + "xnf", [128, D], F32)
        xnTf = Buf(P, tag + "xnTf", [128, 8, 128], F32)
        wr = Buf(P, tag + "wr", [128, 8, 8], F32)
        P.dma("sp", wr.t[:], io["router"].rearrange("(c p) e -> p c e", p=128), writes=[wr.r])
        lg = Buf(P, tag + "lg", [128, 8], F32)
        lg2 = Buf(P, tag + "lg2", [128, 8], F32)
        eq1 = Buf(P, tag + "eq1", [128, 8], F32)
        eq2 = Buf(P, tag + "eq2", [128, 8], F32)
        m1 = Buf(P, tag + "m1", [128, 1], F32)
        m2 = Buf(P, tag + "m2", [128, 1], F32)
        w1 = Buf(P, tag + "w1", [128, 1], F32)
        w2 = Buf(P, tag + "w2", [128, 1], F32)
        cw = Buf(P, tag + "cw", [128, NTT, 8], F32)
        rcw = [P.res(f"cw{i}") for i in range(NTT)]

    nwk = 0
    nwd = 0
    nev = 0
    nx = 0
    for sgi in range(NSG):
        t0 = sgi * NTOK
        for tt in range(NTT):
            X = xt[nx % 2]
            nx += 1
            r0 = t0 + tt * 128
            P.dma("sp", X.t[:], x_in[r0:r0 + 128, :], writes=[X.r])
            P.op("act", lambda e, X=X: e.activation(out=junk.t[:], in_=X.t[:], func=AF.Square, accum_out=ss.t[:]),
                 reads=[X.r], writes=[junk.r, ss.r])
            P.op("act", lambda e: e.activation(out=ss.t[:], in_=ss.t[:], func=AF.Sqrt, scale=1.0 / D, bias=C["epsb"].t[:]),
                 reads=[ss.r, C["epsb"].r], writes=[ss.r])
            P.op("dve", lambda e: e.reciprocal(out=ss.t[:], in_=ss.t[:]), reads=[ss.r], writes=[ss.r])
            if moe:
                P.op("dve", lambda e, X=X: e.scalar_tensor_tensor(out=xnf.t[:], in0=X.t[:], scalar=ss.t[:, 0:1], in1=gbc.t[:],
                                                                  op0=ALU.mult, op1=ALU.mult),
                     reads=[X.r, ss.r, gbc.r], writes=[xnf.r])
                P.op("pool", lambda e: e.tensor_copy(out=xn.t[:], in_=xnf.t[:]), reads=[xnf.r], writes=[xn.r])
            else:
                P.op("dve", lambda e, X=X: e.scalar_tensor_tensor(out=xn.t[:], in0=X.t[:], scalar=ss.t[:, 0:1], in1=gbc.t[:],
                                                                  op0=ALU.mult, op1=ALU.mult),
                     reads=[X.r, ss.r, gbc.r], writes=[xn.r])
            P.op("pool", lambda e, X=X, tt=tt: e.tensor_copy(out=acc.t[:, tt, :], in_=X.t[:]), reads=[X.r], writes=[racc[tt]])

            def tr(e):
                for c in range(8):
                    i = e.transpose(out=pT.t[:, c, :], in_=xn.t[:, c * 128:(c + 1) * 128], identity=ident.t[:])
                return i
            P.op("pe", tr, reads=[xn.r, ident.r], writes=[pT.r])
            P.op("act", lambda e, tt=tt: e.copy(out=xnT.t[:, :, tt * 128:(tt + 1) * 128], in_=pT.t[:]),
                 reads=[pT.r], writes=[xnT.r])
            if moe:
                for half in range(2):
                    def trf(e, half=half):
                        for c in range(4):
                            cc_ = half * 4 + c
                            i = e.transpose(out=pF.t[:, c, :], in_=xnf.t[:, cc_ * 128:(cc_ + 1) * 128], identity=identf.t[:])
                        return i
                    P.op("pe", trf, reads=[xnf.r, identf.r], writes=[pF.r])
                    P.op("act", lambda e, half=half: e.copy(out=xnTf.t[:, half * 4:half * 4 + 4, :], in_=pF.t[:]),
                         reads=[pF.r], writes=[xnTf.r])

                def rt(e):
                    for c in range(8):
                        i = e.matmul(pF.t[:, 0, 0:8], lhsT=xnTf.t[:, c, :], rhs=wr.t[:, c, :], start=(c == 0), stop=(c == 7))
                    return i
                P.op("pe", rt, reads=[xnTf.r, wr.r], writes=[pF.r])
                P.op("dve", lambda e: e.tensor_copy(out=lg.t[:], in_=pF.t[:, 0, 0:8]), reads=[pF.r], writes=[lg.r])
                P.op("dve", lambda e: e.tensor_reduce(out=m1.t[:], in_=lg.t[:], axis=AX.X, op=ALU.max), reads=[lg.r], writes=[m1.r])
                P.op("dve", lambda e: e.tensor_scalar(out=eq1.t[:], in0=lg.t[:], scalar1=m1.t[:, 0:1], scalar2=None, op0=ALU.is_equal),
                     reads=[lg.r, m1.r], writes=[eq1.r])
                P.op("dve", lambda e: e.scalar_tensor_tensor(out=lg2.t[:], in0=eq1.t[:], scalar=-1e30, in1=lg.t[:],
                                                             op0=ALU.mult, op1=ALU.add), reads=[eq1.r, lg.r], writes=[lg2.r])
                P.op("dve", lambda e: e.tensor_reduce(out=m2.t[:], in_=lg2.t[:], axis=AX.X, op=ALU.max), reads=[lg2.r], writes=[m2.r])
                P.op("dve", lambda e: e.tensor_scalar(out=eq2.t[:], in0=lg2.t[:], scalar1=m2.t[:, 0:1], scalar2=None, op0=ALU.is_equal),
                     reads=[lg2.r, m2.r], writes=[eq2.r])
                P.op("dve", lambda e: e.tensor_tensor(out=w2.t[:], in0=m2.t[:], in1=m1.t[:], op=ALU.subtract),
                     reads=[m1.r, m2.r], writes=[w2.r])
                P.op("act", lambda e: e.activation(out=w2.t[:], in_=w2.t[:], func=AF.Exp), reads=[w2.r], writes=[w2.r])
                P.op("dve", lambda e: e.tensor_scalar(out=w1.t[:], in0=w2.t[:], scalar1=1.0, scalar2=None, op0=ALU.add),
                     reads=[w2.r], writes=[w1.r])
                P.op("dve", lambda e: e.reciprocal(out=w1.t[:], in_=w1.t[:]), reads=[w1.r], writes=[w1.r])
                P.op("dve", lambda e: e.tensor_tensor(out=w2.t[:], in0=w2.t[:], in1=w1.t[:], op=ALU.mult),
                     reads=[w1.r, w2.r], writes=[w2.r])
                P.op("dve", lambda e: e.tensor_scalar(out=eq1.t[:], in0=eq1.t[:], scalar1=w1.t[:, 0:1], scalar2=None, op0=ALU.mult),
                     reads=[eq1.r, w1.r], writes=[eq1.r])
                P.op("dve", lambda e, tt=tt: e.scalar_tensor_tensor(out=cw.t[:, tt, :], in0=eq2.t[:], scalar=w2.t[:, 0:1], in1=eq1.t[:],
                                                                    op0=ALU.mult, op1=ALU.add),
                     reads=[eq2.r, w2.r, eq1.r], writes=[rcw[tt]])

        for ex in range(E):
            WD = []
            for dh in range(2):
                Wd = wds[nwd % 2]
                nwd += 1
                P.dma("pool", Wd.t[:], wd_d[ex].rearrange("(c p) n -> p c n", p=128)[:, :, dh * 512:(dh + 1) * 512], writes=[Wd.r])
                WD.append(Wd)
            for fc in range(NFC):
                Wg = wgs[nwk % 3]
                Wu = wus[nwk % 3]
                nwk += 1
                P.dma("pool", Wg.t[:], wg_d[ex].rearrange("(c p) n -> p c n", p=128)[:, :, fc * 128:(fc + 1) * 128], writes=[Wg.r])
                P.dma("pool", Wu.t[:], wu_d[ex].rearrange("(c p) n -> p c n", p=128)[:, :, fc * 128:(fc + 1) * 128], writes=[Wu.r])
                for tg in range(NTG):
                    A = pA[nev % 2]
                    B = pB[nev % 2]
                    SG = sgt[nev % 2]
                    nev += 1

                    def mmg(e, W=Wg, O=A, tg=tg):
                        for c in range(8):
                            i = e.matmul(O.t[:], lhsT=W.t[:, c, :], rhs=xnT.t[:, c, tg * 512:(tg + 1) * 512], start=(c == 0), stop=(c == 7))
                        return i
                    P.op("pe", mmg, reads=[Wg.r, xnT.r], writes=[A.r])

                    def mmu(e, W=Wu, O=B, tg=tg):
                        for c in range(8):
                            i = e.matmul(O.t[:], lhsT=W.t[:, c, :], rhs=xnT.t[:, c, tg * 512:(tg + 1) * 512], start=(c == 0), stop=(c == 7))
                        return i
                    P.op("pe", mmu, reads=[Wu.r, xnT.r], writes=[B.r])
                    P.op("act", lambda e, A=A, SG=SG: e.activation(out=SG.t[:], in_=A.t[:], func=AF.Silu), reads=[A.r], writes=[SG.r])
                    P.op("dve", lambda e, B=B, SG=SG, fc=fc, tg=tg: e.tensor_tensor(out=hT.t[:, fc, tg * 512:(tg + 1) * 512], in0=B.t[:], in1=SG.t[:],
                                                                                op=ALU.mult), reads=[B.r, SG.r], writes=[hT.r])
            for dh in range(2):
                Wd = WD[dh]
                for tt in range(NTT):
                    Y = pY[(dh * NTT + tt) % 2]

                    def mmd(e, Y=Y, Wd=Wd, tt=tt):
                        for fc in range(NFC):
                            i = e.matmul(Y.t[:], lhsT=hT.t[:, fc, tt * 128:(tt + 1) * 128], rhs=Wd.t[:, fc, :], start=(fc == 0), stop=(fc == NFC - 1))
                        return i
                    P.op("pe", mmd, reads=[hT.r, Wd.r], writes=[Y.r])
                    if moe:
                        P.op("dve", lambda e, Y=Y, tt=tt, dh=dh, ex=ex: e.scalar_tensor_tensor(
                            out=acc.t[:, tt, dh * 512:(dh + 1) * 512], in0=Y.t[:], scalar=cw.t[:, tt, ex:ex + 1],
                            in1=acc.t[:, tt, dh * 512:(dh + 1) * 512], op0=ALU.mult, op1=ALU.add),
                            reads=[Y.r, rcw[tt], racc[tt]], writes=[racc[tt]])
                    else:
                        P.op("dve", lambda e, Y=Y, tt=tt, dh=dh: e.tensor_tensor(
                            out=acc.t[:, tt, dh * 512:(dh + 1) * 512], in0=Y.t[:], in1=acc.t[:, tt, dh * 512:(dh + 1) * 512], op=ALU.add),
                            reads=[Y.r, racc[tt]], writes=[racc[tt]])
        for tt in range(NTT):
            r0 = t0 + tt * 128
            P.dma("sp", x_out[r0:r0 + 128, :], acc.t[:, tt, :], reads=[racc[tt]])


def ffn_dense(P, C, io, F, NTOK=1024, tag="f"):
    x_in, x_out = io["x_in"], io["x_out"]
    wg_d, wu_d, wd_d = io["wg"], io["wu"], io["wd"]
    NFC = F // 128
    NTT = NTOK // 128
    NTG = NTOK // 512
    NSG = S // NTOK
    ident = C["ident"]
    gbc = Buf(P, tag + "gbc", [128, D], F32)
    P.dma("sp", gbc.t[:], io["norm_g"].partition_broadcast(128), writes=[gbc.r])
    xt = [Buf(P, f"{tag}xt{i}", [128, D], F32) for i in range(2)]
    xr = [Buf(P, f"{tag}xr{i}", [128, D], F32) for i in range(2)]
    xo = [Buf(P, f"{tag}xo{i}", [128, D], F32) for i in range(2)]
    junk = Buf(P, tag + "junk", [128, D], BF16)
    ss = Buf(P, tag + "ss", [128, 1], F32)
    xn = Buf(P, tag + "xn", [128, D], BF16)
    xnTs = [Buf(P, f"{tag}xnT{i}", [128, 8, NTOK], BF16) for i in range(2)]
    hT = Buf(P, tag + "hT", [128, NFC, NTOK], BF16)
    wgs = [Buf(P, f"{tag}wg{i}", [128, 8, 128], BF16) for i in range(3)]
    wus = [Buf(P, f"{tag}wu{i}", [128, 8, 128], BF16) for i in range(3)]
    wds = [Buf(P, f"{tag}wd{i}", [128, NFC, 512], BF16) for i in range(2)]
    sgt = [Buf(P, f"{tag}sg{i}", [128, 512], F32) for i in range(2)]
    pT = Buf(P, tag + "pT", [128, 8, 128], BF16, psum=True)
    pA = [Buf(P, f"{tag}pA{i}", [128, 512], F32, psum=True) for i in range(2)]
    pB = [Buf(P, f"{tag}pB{i}", [128, 512], F32, psum=True) for i in range(2)]
    pY = [Buf(P, f"{tag}pY{i}", [128, 512], F32, psum=True) for i in range(2)]
    st = dict(nx=0, nwk=0, nev=0, ny=0)

    ss2 = [ss, Buf(P, tag + "ss1", [128, 1], F32)]
    xn2 = [xn, Buf(P, tag + "xn1", [128, D], BF16)]

    def s0a(sgi, tt):
        X = xt[(sgi * NTT + tt) % 2]
        SS = ss2[tt % 2]
        XN = xn2[tt % 2]
        r0 = sgi * NTOK + tt * 128
        P.dma("sp", X.t[:], x_in[r0:r0 + 128, :], writes=[X.r])
        P.op("act", lambda e: e.activation(out=junk.t[:], in_=X.t[:], func=AF.Square, accum_out=SS.t[:]),
             reads=[X.r], writes=[junk.r, SS.r])
        P.op("act", lambda e: e.activation(out=SS.t[:], in_=SS.t[:], func=AF.Sqrt, scale=1.0 / D, bias=C["epsb"].t[:]),
             reads=[SS.r, C["epsb"].r], writes=[SS.r])
        P.op("dve", lambda e: e.reciprocal(out=SS.t[:], in_=SS.t[:]), reads=[SS.r], writes=[SS.r])
        P.op("dve", lambda e: e.scalar_tensor_tensor(out=XN.t[:], in0=X.t[:], scalar=SS.t[:, 0:1], in1=gbc.t[:],
                                                     op0=ALU.mult, op1=ALU.mult), reads=[X.r, SS.r, gbc.r], writes=[XN.r])

    def s0b(sgi, tt):
        xnT = xnTs[sgi % 2]
        XN = xn2[tt % 2]

        def tr(e):
            for c in range(8):
                i = e.transpose(out=pT.t[:, c, :], in_=XN.t[:, c * 128:(c + 1) * 128], identity=ident.t[:])
            return i
        P.op("pe", tr, reads=[XN.r, ident.r], writes=[pT.r])
        P.op("act", lambda e: e.copy(out=xnT.t[:, :, tt * 128:(tt + 1) * 128], in_=pT.t[:]), reads=[pT.r], writes=[xnT.r])

    def stage0_steps(sgi):
        steps = []
        for k in range(NTT + 1):
            def step(k=k):
                if k < NTT:
                    s0a(sgi, k)
                if k >= 1:
                    s0b(sgi, k - 1)
            steps.append(step)
        return steps

    def stage1(sgi, side_steps=()):
        xnT = xnTs[sgi % 2]
        side = list(side_steps)
        WD = []
        for dh in range(2):
            Wd = wds[dh]
            P.dma("pool", Wd.t[:], wd_d[0].rearrange("(c p) n -> p c n", p=128)[:, :, dh * 512:(dh + 1) * 512], writes=[Wd.r])
            WD.append(Wd)
        for fc in range(NFC):
            Wg = wgs[st["nwk"] % 3]
            Wu = wus[st["nwk"] % 3]
            st["nwk"] += 1
            P.dma("pool", Wg.t[:], wg_d[0].rearrange("(c p) n -> p c n", p=128)[:, :, fc * 128:(fc + 1) * 128], writes=[Wg.r])
            P.dma("pool", Wu.t[:], wu_d[0].rearrange("(c p) n -> p c n", p=128)[:, :, fc * 128:(fc + 1) * 128], writes=[Wu.r])
            for tg in range(NTG):
                A = pA[st["nev"] % 2]
                B = pB[st["nev"] % 2]
                SG = sgt[st["nev"] % 2]
                st["nev"] += 1

                def mmg(e, W=Wg, O=A, tg=tg):
                    for c in range(8):
                        i = e.matmul(O.t[:], lhsT=W.t[:, c, :], rhs=xnT.t[:, c, tg * 512:(tg + 1) * 512], start=(c == 0), stop=(c == 7))
                    return i
                P.op("pe", mmg, reads=[Wg.r, xnT.r], writes=[A.r])

                def mmu(e, W=Wu, O=B, tg=tg):
                    for c in range(8):
                        i = e.matmul(O.t[:], lhsT=W.t[:, c, :], rhs=xnT.t[:, c, tg * 512:(tg + 1) * 512], start=(c == 0), stop=(c == 7))
                    return i
                P.op("pe", mmu, reads=[Wu.r, xnT.r], writes=[B.r])
                P.op("act", lambda e, A=A, SG=SG: e.activation(out=SG.t[:], in_=A.t[:], func=AF.Silu), reads=[A.r], writes=[SG.r])
                P.op("dve", lambda e, B=B, SG=SG, fc=fc, tg=tg: e.tensor_tensor(out=hT.t[:, fc, tg * 512:(tg + 1) * 512], in0=B.t[:], in1=SG.t[:],
                                                                            op=ALU.mult), reads=[B.r, SG.r], writes=[hT.r])
            if side and fc % 2 == 1:
                side.pop(0)()
        while side:
            side.pop(0)()
        return WD

    def stage2(sgi, WD):
        for tt in range(NTT):
            r0 = sgi * NTOK + tt * 128
            XR = xr[st["ny"] % 2]
            XO = xo[st["ny"] % 2]
            st["ny"] += 1
            P.dma("sp", XR.t[:], x_in[r0:r0 + 128, :], writes=[XR.r])
            for dh in range(2):
                Y = pY[dh]
                Wd = WD[dh]

                def mmd(e, Y=Y, Wd=Wd, tt=tt):
                    for fc in range(NFC):
                        i = e.matmul(Y.t[:], lhsT=hT.t[:, fc, tt * 128:(tt + 1) * 128], rhs=Wd.t[:, fc, :], start=(fc == 0), stop=(fc == NFC - 1))
                    return i
                P.op("pe", mmd, reads=[hT.r, Wd.r], writes=[Y.r])
                P.op("dve", lambda e, Y=Y, dh=dh, XR=XR, XO=XO: e.tensor_tensor(out=XO.t[:, dh * 512:(dh + 1) * 512], in0=Y.t[:],
                                                                            in1=XR.t[:, dh * 512:(dh + 1) * 512], op=ALU.add),
                     reads=[Y.r, XR.r], writes=[XO.r])
            P.dma("act", x_out[r0:r0 + 128, :], XO.t[:], reads=[XO.r])

    for st_ in stage0_steps(0):
        st_()
    for sgi in range(NSG):
        WD = stage1(sgi, stage0_steps(sgi + 1) if sgi + 1 < NSG else ())
        stage2(sgi, WD)


D = 1024
S = 4096
NT = S // 128
NQG = S // 512


def phase3(P, C, io, npairs=8, nqg=NQG):
    x_in, x_out, oT_d = io["x_in"], io["x_out"], io["oT"]
    ident = C["ident"]
    trineg = Buf(P, "trineg", [128, 128], BF16)
    negones = Buf(P, "negones", [128, 128], BF16)
    blk1 = Buf(P, "blk1", [128, 128], BF16)
    tmpf = Buf(P, "tmpf", [128, 128], F32)
    mask01 = Buf(P, "mask01", [128, 4, 512], BF16)
    tmpm = Buf(P, "tmpm", [128, 512], F32)
    P.op("pool", lambda e: e.memset(tmpf.t[:], -1.0), writes=[tmpf.r])
    P.op("dve", lambda e: e.tensor_copy(out=negones.t[:], in_=tmpf.t[:]), reads=[tmpf.r], writes=[negones.r])
    P.op("pool", lambda e: e.affine_select(out=tmpf.t[:], in_=tmpf.t[:], pattern=[[-1, 128]], compare_op=ALU.is_ge, fill=0.0,
                                           base=0, channel_multiplier=1), reads=[tmpf.r, negones.r], writes=[tmpf.r])
    P.op("dve", lambda e: e.tensor_copy(out=trineg.t[:], in_=tmpf.t[:]), reads=[tmpf.r], writes=[trineg.r])
    P.op("pool", lambda e: e.memset(blk1.t[:], 0.0), writes=[blk1.r])
    P.op("pool", lambda e: e.memset(blk1.t[0:64, 0:64], 1.0), writes=[blk1.r])
    P.op("pool", lambda e: e.memset(blk1.t[64:128, 64:128], 1.0), writes=[blk1.r])
    for i in range(4):
        P.op("pool", lambda e: e.memset(tmpm.t[:], 1.0), reads=[mask01.r], writes=[tmpm.r])
        P.op("pool", lambda e, i=i: e.affine_select(out=tmpm.t[:], in_=tmpm.t[:], pattern=[[1, 512]], compare_op=ALU.is_gt, fill=0.0,
                                                    base=-128 * i, channel_multiplier=-1), reads=[tmpm.r], writes=[tmpm.r])
        P.op("dve", lambda e, i=i: e.tensor_copy(out=mask01.t[:, i, :], in_=tmpm.t[:]), reads=[tmpm.r], writes=[mask01.r])
    gq_in = Buf(P, "gq_in", [128, 8], F32)
    gkv_in = Buf(P, "gkv_in", [128, 8], F32)
    gq_col = Buf(P, "gq_col", [128, 1], F32)
    gk_col = Buf(P, "gk_col", [128, 1], F32)
    P.dma("sp", gq_in.t[:], io["mix_norm1"].rearrange("(c p) -> p c", p=128), writes=[gq_in.r], allow_slow_non_contiguous=True)
    P.dma("sp", gkv_in.t[:], io["kv_norm"].rearrange("(c p) -> p c", p=128), writes=[gkv_in.r], allow_slow_non_contiguous=True)
    for hf in range(2):
        P.dma("sp", gq_col.t[hf * 64:hf * 64 + 64, :], io["g_q"].rearrange("(p o) -> p o", o=1), writes=[gq_col.r])
        P.dma("sp", gk_col.t[hf * 64:hf * 64 + 64, :], io["g_k"].rearrange("(p o) -> p o", o=1), writes=[gk_col.r])
    P.op("dve", lambda e: e.tensor_scalar(out=gk_col.t[:], in0=gk_col.t[:], scalar1=0.125, scalar2=None, op0=ALU.mult),
         reads=[gk_col.r], writes=[gk_col.r])

    xhatT = Buf(P, "xhatT", [128, 8, S], BF16)
    xt = [Buf(P, f"axt{i}", [128, D], F32) for i in range(2)]
    junk = Buf(P, "ajunk", [128, D], BF16)
    ss = Buf(P, "ass", [128, 1], F32)
    xh = Buf(P, "axh", [128, D], BF16)
    qTz = Buf(P, "qTz", [128, 2, S], BF16)
    kT = Buf(P, "kTa", [128, S], BF16)
    Vz = Buf(P, "Vz", [128, NT, 2, 128], BF16)
    wraw = [Buf(P, f"wraw{i}", [128, 8, 128], BF16) for i in range(3)]
    wf = [Buf(P, f"wf{i}", [128, 8, 128], BF16) for i in range(3)]
    sq = Buf(P, "asq", [128, 512], BF16)
    rs = Buf(P, "ars", [128, 512], F32)
    ebuf = [Buf(P, f"e{i}", [128, 512], F32) for i in range(2)]
    lpb = [Buf(P, f"lp{i}", [128, 512], BF16) for i in range(2)]
    ttb = [Buf(P, f"tt{i}", [128, 512], F32) for i in range(2)]
    Ab = [Buf(P, f"A{i}", [128, 512], BF16) for i in range(4)]
    Rb = Buf(P, "Rb", [128, 512], F32)
    oTt = [Buf(P, f"oTt{i}", [128, 512], BF16) for i in range(2)]
    pS = [Buf(P, f"pS{i}", [128, 512], F32, psum=True) for i in range(2)]
    pG = [Buf(P, f"pGa{i}", [128, 512], F32, psum=True) for i in range(2)]
    pR = [Buf(P, f"pR{i}", [128, 512], F32, psum=True) for i in range(2)]
    pO = [Buf(P, f"pO{i}", [128, 512], F32, psum=True) for i in range(2)]

    P.op("pool", lambda e: e.memset(qTz.t[:], 0.0), writes=[qTz.r])
    P.op("pool", lambda e: e.memset(Vz.t[:], 0.0), writes=[Vz.r])

    ss2 = [ss, Buf(P, "ass1", [128, 1], F32)]
    xh2 = [xh, Buf(P, "axh1", [128, D], BF16)]

    def s3a_a(t):
        X = xt[t % 2]
        SS = ss2[t % 2]
        XH = xh2[t % 2]
        P.dma("sp", X.t[:], x_in[t * 128:(t + 1) * 128, :], writes=[X.r])
        P.op("act", lambda e: e.activation(out=junk.t[:], in_=X.t[:], func=AF.Square, accum_out=SS.t[:]),
             reads=[X.r], writes=[junk.r, SS.r])
        P.op("act", lambda e: e.activation(out=SS.t[:], in_=SS.t[:], func=AF.Sqrt, scale=1.0 / D, bias=C["epsb"].t[:]),
             reads=[SS.r, C["epsb"].r], writes=[SS.r])
        P.op("dve", lambda e: e.reciprocal(out=SS.t[:], in_=SS.t[:]), reads=[SS.r], writes=[SS.r])
        P.op("dve", lambda e: e.tensor_scalar(out=XH.t[:], in0=X.t[:], scalar1=SS.t[:, 0:1], scalar2=None, op0=ALU.mult),
             reads=[X.r, SS.r], writes=[XH.r])

    def s3a_b(t):
        XH = xh2[t % 2]
        pTv = pS[t % 2]

        def tr(e):
            v = pTv.t[:, :].bitcast(BF16)
            for c in range(8):
                i = e.transpose(out=v[:, c * 128:(c + 1) * 128], in_=XH.t[:, c * 128:(c + 1) * 128], identity=ident.t[:])
            return i
        P.op("pe", tr, reads=[XH.r, ident.r], writes=[pTv.r])
        P.op("act", lambda e: e.copy(out=xhatT.t[:, :, t * 128:(t + 1) * 128],
                                     in_=pTv.t[:, :].bitcast(BF16).rearrange("p (c n) -> p c n", c=8)),
             reads=[pTv.r], writes=[xhatT.r])

    n3a = nqg * 4
    s3a_a(0)
    for t in range(n3a):
        if t + 1 < n3a:
            s3a_a(t + 1)
        s3a_b(t)

    for j in range(npairs):
        srcs = [io["w_q"][:, j * 128:(j + 1) * 128], io["w_kv"][:, j * 128:(j + 1) * 128],
                io["w_kv"][:, 1024 + j * 128:1024 + (j + 1) * 128]]
        gains = [gq_in, gkv_in, gkv_in]
        for i in range(3):
            P.dma("pool", wraw[i].t[:], srcs[i].rearrange("(c p) n -> p c n", p=128), writes=[wraw[i].r])
            P.op("pool", lambda e, i=i: e.tensor_tensor(out=wf[i].t[:], in0=wraw[i].t[:],
                                                       in1=gains[i].t[:, :].unsqueeze(2).to_broadcast([128, 8, 128]), op=ALU.mult),
                 reads=[wraw[i].r, gains[i].r], writes=[wf[i].r])
        for which in range(2):
            W = wf[which]
            for tg in range(nqg):
                PS = pS[tg % 2]
                PG = pG[tg % 2]

                def mm(e, W=W, PS=PS, tg=tg):
                    for c in range(8):
                        i = e.matmul(PS.t[:], lhsT=W.t[:, c, :], rhs=xhatT.t[:, c, tg * 512:(tg + 1) * 512], start=(c == 0), stop=(c == 7))
                    return i
                P.op("pe", mm, reads=[W.r, xhatT.r], writes=[PS.r])
                P.op("act", lambda e, PS=PS: e.activation(out=sq.t[:], in_=PS.t[:], func=AF.Square), reads=[PS.r], writes=[sq.r])
                P.op("pe", lambda e, PG=PG: e.matmul(PG.t[:], lhsT=blk1.t[:], rhs=sq.t[:], start=True, stop=True),
                     reads=[blk1.r, sq.r], writes=[PG.r])
                P.op("act", lambda e, PG=PG: e.activation(out=rs.t[:], in_=PG.t[:], func=AF.Sqrt, scale=1.0 / 64.0, bias=C["epsb"].t[:]),
                     reads=[PG.r, C["epsb"].r], writes=[rs.r])
                P.op("dve", lambda e: e.reciprocal(out=rs.t[:], in_=rs.t[:]), reads=[rs.r], writes=[rs.r])
                if which == 0:
                    for hf in range(2):
                        lo, hi = hf * 64, hf * 64 + 64
                        P.op("dve", lambda e, PS=PS, lo=lo, hi=hi, hf=hf, tg=tg: e.scalar_tensor_tensor(
                            out=qTz.t[lo:hi, hf, tg * 512:(tg + 1) * 512], in0=PS.t[lo:hi, :], scalar=gq_col.t[lo:hi, 0:1],
                            in1=rs.t[lo:hi, :], op0=ALU.mult, op1=ALU.mult), reads=[PS.r, gq_col.r, rs.r], writes=[qTz.r])
                else:
                    P.op("dve", lambda e, PS=PS, tg=tg: e.scalar_tensor_tensor(
                        out=kT.t[:, tg * 512:(tg + 1) * 512], in0=PS.t[:], scalar=gk_col.t[:, 0:1],
                        in1=rs.t[:], op0=ALU.mult, op1=ALU.mult), reads=[PS.r, gk_col.r, rs.r], writes=[kT.r])
        for b4 in range(nqg):
            PS = pS[b4 % 2]

            def mmv(e, PS=PS, b4=b4):
                for i4 in range(4):
                    tb = b4 * 4 + i4
                    for c in range(8):
                        i = e.matmul(PS.t[:, i4 * 128:(i4 + 1) * 128], lhsT=xhatT.t[:, c, tb * 128:(tb + 1) * 128], rhs=wf[2].t[:, c, :],
                                     start=(c == 0), stop=(c == 7))
                return i
            P.op("pe", mmv, reads=[xhatT.r, wf[2].r], writes=[PS.r])
            for hf in range(2):
                P.op("act", lambda e, PS=PS, b4=b4, hf=hf: e.copy(
                    out=Vz.t[:, b4 * 4:(b4 + 1) * 4, hf, hf * 64:hf * 64 + 64],
                    in_=PS.t[:, :].rearrange("p (i n) -> p i n", i=4)[:, :, hf * 64:hf * 64 + 64]), reads=[PS.r], writes=[Vz.r])

        its = []
        for qg in range(nqg):
            first = True
            for ee in range(2):
                kbs = list(range(4 * qg + 3, -1, -1))
                for n_, kb in enumerate(kbs):
                    its.append(dict(qg=qg, ee=ee, kb=kb, firstkb=(n_ == 0), diag=(kb >= 4 * qg), di=kb - 4 * qg,
                                    ostart=first, ostop=(ee == 1 and n_ == len(kbs) - 1)))
                    first = False
        n_it = len(its)

        def stageA(n):
            it = its[n]
            b = n % 2
            c0 = 128 * it["di"] if it["diag"] else 0
            q_ap = qTz.t[:, it["ee"], it["qg"] * 512 + c0:(it["qg"] + 1) * 512]
            k_ap = kT.t[:, it["kb"] * 128:(it["kb"] + 1) * 128]
            P.op("pe", lambda e: e.matmul(pS[b].t[:, c0:], lhsT=k_ap, rhs=q_ap, start=True, stop=True), reads=[kT.r, qTz.r], writes=[pS[b].r])
            P.op("act", lambda e: e.activation(out=ebuf[b].t[:, c0:], in_=pS[b].t[:, c0:], func=AF.Exp), reads=[pS[b].r], writes=[ebuf[b].r])
            P.op("act", lambda e: e.activation(out=lpb[b].t[:, c0:], in_=ebuf[b].t[:, c0:], func=AF.Ln, bias=C["one1"].t[:]),
                 reads=[ebuf[b].r, C["one1"].r], writes=[lpb[b].r])
            if it["diag"]:
                P.op("pool", lambda e: e.tensor_tensor(out=lpb[b].t[:, c0:], in0=lpb[b].t[:, c0:], in1=mask01.t[:, it["di"], c0:], op=ALU.mult),
                     reads=[lpb[b].r, mask01.r], writes=[lpb[b].r])

        def stageB(n):
            it = its[n]
            b = n % 2
            c0 = 128 * it["di"] if it["diag"] else 0
            q_ap = qTz.t[:, it["ee"], it["qg"] * 512 + c0:(it["qg"] + 1) * 512]
            k_ap = kT.t[:, it["kb"] * 128:(it["kb"] + 1) * 128]

            def g(e):
                e.matmul(pG[b].t[:, c0:], lhsT=trineg.t[:], rhs=lpb[b].t[:, c0:], start=True, stop=False)
                return e.matmul(pG[b].t[:, c0:], lhsT=k_ap, rhs=q_ap, start=False, stop=True)
            P.op("pe", g, reads=[trineg.r, lpb[b].r, kT.r, qTz.r], writes=[pG[b].r])
            P.op("pe", lambda e: e.matmul(pR[b].t[:, c0:], lhsT=negones.t[:], rhs=lpb[b].t[:, c0:], start=True, stop=True),
                 reads=[negones.r, lpb[b].r], writes=[pR[b].r])
            if it["firstkb"]:
                P.op("dve", lambda e: e.tensor_copy(out=ttb[b].t[:, c0:], in_=pG[b].t[:, c0:]), reads=[pG[b].r], writes=[ttb[b].r])
                P.op("pool", lambda e: e.memset(Rb.t[:, 0:c0], 0.0), writes=[Rb.r])
                P.op("dve", lambda e: e.tensor_copy(out=Rb.t[:, c0:], in_=pR[b].t[:, c0:]), reads=[pR[b].r], writes=[Rb.r])
            else:
                P.op("dve", lambda e: e.tensor_tensor(out=ttb[b].t[:, c0:], in0=pG[b].t[:, c0:], in1=Rb.t[:, c0:], op=ALU.add),
                     reads=[pG[b].r, Rb.r], writes=[ttb[b].r])
                P.op("dve", lambda e: e.tensor_tensor(out=Rb.t[:, c0:], in0=pR[b].t[:, c0:], in1=Rb.t[:, c0:], op=ALU.add),
                     reads=[pR[b].r, Rb.r], writes=[Rb.r])

        def stageB2(n):
            it = its[n]
            b = n % 2
            A = Ab[n % 4]
            c0 = 128 * it["di"] if it["diag"] else 0
            P.op("act", lambda e: e.activation(out=A.t[:, c0:], in_=ttb[b].t[:, c0:], func=AF.Exp), reads=[ttb[b].r], writes=[A.r])
            if it["diag"]:
                P.op("pool", lambda e: e.tensor_tensor(out=A.t[:, c0:], in0=A.t[:, c0:], in1=mask01.t[:, it["di"], c0:], op=ALU.mult),
                     reads=[A.r, mask01.r], writes=[A.r])
                if c0 > 0:
                    P.op("pool", lambda e: e.memset(A.t[:, 0:c0], 0.0), reads=[A.r], writes=[A.r])

        def stageC(n):
            it = its[n]
            b = n % 2
            ob = it["qg"] % 2
            A = Ab[n % 4]
            P.op("pe", lambda e: e.matmul(pO[ob].t[:], lhsT=Vz.t[:, it["kb"], it["ee"], :], rhs=A.t[:],
                                          start=it["ostart"], stop=it["ostop"]),
                 reads=[Vz.r, A.r], writes=[pO[ob].r])
            if it["ostop"]:
                qg = it["qg"]
                O = oTt[qg % 2]
                P.op("dve", lambda e: e.tensor_copy(out=O.t[:], in_=pO[ob].t[:]), reads=[pO[ob].r], writes=[O.r])
                P.dma("sp", oT_d[j, :, qg * 512:(qg + 1) * 512], O.t[:], reads=[O.r], writes=[io["r_oT"]])

        for n in range(n_it + 4):
            if n < n_it:
                stageA(n)
            if 0 <= n - 1 < n_it:
                stageB(n - 1)
            if 0 <= n - 2 < n_it:
                stageB2(n - 2)
            if 0 <= n - 4 < n_it:
                stageC(n - 4)

    w_o = Buf(P, "w_o", [128, 8, D], BF16)
    P.dma("pool", w_o.t[:], io["w_o"].rearrange("(c p) n -> p c n", p=128), writes=[w_o.r])
    oin = [Buf(P, f"oin{i}", [128, 8, 128], BF16) for i in range(2)]
    xo = [Buf(P, f"axo{i}", [128, D], F32) for i in range(2)]
    for t in range(nqg * 4):
        X = xt[t % 2]
        OI = oin[t % 2]
        XO = xo[t % 2]
        P.dma("sp", X.t[:], x_in[t * 128:(t + 1) * 128, :], writes=[X.r])
        P.dma("sp", OI.t[:, 0:npairs, :], oT_d[0:npairs, :, t * 128:(t + 1) * 128].rearrange("j p n -> p j n"),
              reads=[io["r_oT"]], writes=[OI.r])
        for n in range(2):
            PS = pS[n]

            def op_(e, PS=PS, OI=OI, n=n):
                for c in range(npairs):
                    i = e.matmul(PS.t[:], lhsT=OI.t[:, c, :], rhs=w_o.t[:, c, n * 512:(n + 1) * 512], start=(c == 0), stop=(c == npairs - 1))
                return i
            P.op("pe", op_, reads=[OI.r, w_o.r], writes=[PS.r])
            P.op("dve", lambda e, PS=PS, X=X, XO=XO, n=n: e.tensor_tensor(out=XO.t[:, n * 512:(n + 1) * 512], in0=X.t[:, n * 512:(n + 1) * 512],
                                                                      in1=PS.t[:], op=ALU.add), reads=[X.r, PS.r], writes=[XO.r])
        P.dma("act", x_out[t * 128:(t + 1) * 128, :], XO.t[:], reads=[XO.r])


D = 1024
S = 4096
NJ = 23
NSLOT = NJ * 512
FQ = 896


def moe_route(P, C, io):
    x_in = io["x_in"]
    ident, identf = C["ident"], C["identf"]
    gbc = Buf(P, "rgbc", [128, D], F32)
    P.dma("sp", gbc.t[:], io["norm_g"].partition_broadcast(128), writes=[gbc.r])
    wr = Buf(P, "rwr", [128, 8, 8], F32)
    P.dma("sp", wr.t[:], io["router"].rearrange("(c p) e -> p c e", p=128), writes=[wr.r])
    xt = [Buf(P, f"rxt{i}", [128, D], F32) for i in range(2)]
    junk = Buf(P, "rjunk", [128, D], BF16)
    ss = Buf(P, "rss", [128, 1], F32)
    xnf = Buf(P, "rxnf", [128, D], F32)
    xn = [Buf(P, f"rxn{i}", [128, D], BF16) for i in range(2)]
    xnTf = Buf(P, "rxnTf", [128, 8, 128], F32)
    pF = [Buf(P, f"rpF{i}", [128, 4, 128], F32, psum=True) for i in range(2)]
    pR = Buf(P, "rpR", [128, 512], F32, psum=True)
    lg = Buf(P, "rlg", [128, 8], F32)
    lg2 = Buf(P, "rlg2", [128, 8], F32)
    m1 = Buf(P, "rm1", [128, 1], F32)
    m2 = Buf(P, "rm2", [128, 1], F32)
    EQ1 = Buf(P, "EQ1", [128, 32, 8], F32)
    EQ2 = Buf(P, "EQ2", [128, 32, 8], F32)
    W12 = Buf(P, "W12", [128, 32, 2], F32)
    POS = Buf(P, "POS", [128, 2, 32], F32)
    maskb = Buf(P, "rmaskb", [128, 8], BF16)
    pose = Buf(P, "rpose", [128, 8], F32)
    tmp8 = Buf(P, "rtmp8", [128, 8], F32)
    run = Buf(P, "rrun", [128, 8], F32)
    lts = Buf(P, "rlts", [128, 128], BF16)
    onesb = Buf(P, "ronesb", [128, 128], BF16)
    tf = Buf(P, "rtf", [128, 128], F32)
    P.op("pool", lambda e: e.memset(tf.t[:], 1.0), writes=[tf.r])
    P.op("dve", lambda e: e.tensor_copy(out=onesb.t[:], in_=tf.t[:]), reads=[tf.r], writes=[onesb.r])
    P.op("pool", lambda e: e.affine_select(out=tf.t[:], in_=tf.t[:], pattern=[[1, 128]], compare_op=ALU.is_gt, fill=0.0,
                                           base=0, channel_multiplier=-1), reads=[tf.r, onesb.r], writes=[tf.r])
    P.op("dve", lambda e: e.tensor_copy(out=lts.t[:], in_=tf.t[:]), reads=[tf.r], writes=[lts.r])
    P.op("pool", lambda e: e.memset(run.t[:], 0.0), writes=[run.r])

    xnf2 = [xnf, Buf(P, "rxnf1", [128, D], F32)]
    xnTf2 = [xnTf, Buf(P, "rxnTf1", [128, 8, 128], F32)]
    LG = Buf(P, "rLG", [128, 32, 8], F32)
    LG2 = Buf(P, "rLG2", [128, 32, 8], F32)
    M1 = Buf(P, "rM1", [128, 32], F32)
    M2 = Buf(P, "rM2", [128, 32], F32)
    MASK = Buf(P, "rMASK", [128, 32, 8], BF16)
    POSE = Buf(P, "rPOSE", [128, 32, 8], F32)
    T3 = Buf(P, "rT3", [128, 32, 8], F32)
    ss2 = [ss, Buf(P, "rss1", [128, 1], F32)]

    def r1a(tt):
        X = xt[tt % 2]
        XN = xn[tt % 2]
        XF = xnf2[tt % 2]
        SS = ss2[tt % 2]
        P.dma("sp", X.t[:], x_in[tt * 128:(tt + 1) * 128, :], writes=[X.r])
        P.op("act", lambda e: e.activation(out=junk.t[:], in_=X.t[:], func=AF.Square, accum_out=SS.t[:]),
             reads=[X.r], writes=[junk.r, SS.r])
        P.op("act", lambda e: e.activation(out=SS.t[:], in_=SS.t[:], func=AF.Sqrt, scale=1.0 / D, bias=C["epsb"].t[:]),
             reads=[SS.r, C["epsb"].r], writes=[SS.r])
        P.op("dve", lambda e: e.reciprocal(out=SS.t[:], in_=SS.t[:]), reads=[SS.r], writes=[SS.r])
        P.op("dve", lambda e: e.scalar_tensor_tensor(out=XF.t[:], in0=X.t[:], scalar=SS.t[:, 0:1], in1=gbc.t[:],
                                                     op0=ALU.mult, op1=ALU.mult), reads=[X.r, SS.r, gbc.r], writes=[XF.r])
        P.op("act", lambda e: e.copy(out=XN.t[:], in_=XF.t[:]), reads=[XF.r], writes=[XN.r])
        P.dma("act", io["xn_d"][tt * 128:(tt + 1) * 128, :], XN.t[:], reads=[XN.r])

    def r1b(tt):
        XF = xnf2[tt % 2]
        XTF = xnTf2[tt % 2]
        for half in range(2):
            PF = pF[half]

            def trf(e, half=half, PF=PF):
                for c in range(4):
                    cc_ = half * 4 + c
                    i = e.transpose(out=PF.t[:, c, :], in_=XF.t[:, cc_ * 128:(cc_ + 1) * 128], identity=identf.t[:])
                return i
            P.op("pe", trf, reads=[XF.r, identf.r], writes=[PF.r])
            P.op("act", lambda e, half=half, PF=PF: e.copy(out=XTF.t[:, half * 4:half * 4 + 4, :], in_=PF.t[:]),
                 reads=[PF.r], writes=[XTF.r])

        def rt(e):
            for c in range(8):
                i = e.matmul(pR.t[:, 0:8], lhsT=XTF.t[:, c, :], rhs=wr.t[:, c, :], start=(c == 0), stop=(c == 7))
            return i
        P.op("pe", rt, reads=[XTF.r, wr.r], writes=[pR.r])
        P.op("dve", lambda e: e.tensor_copy(out=LG.t[:, tt, :], in_=pR.t[:, 0:8]), reads=[pR.r], writes=[LG.r])

    r1a(0)
    for tt in range(32):
        if tt + 1 < 32:
            r1a(tt + 1)
        r1b(tt)
    bc = lambda ap: ap.unsqueeze(2).to_broadcast([128, 32, 8])
    P.op("dve", lambda e: e.tensor_reduce(out=M1.t[:], in_=LG.t[:], axis=AX.X, op=ALU.max), reads=[LG.r], writes=[M1.r])
    P.op("dve", lambda e: e.tensor_tensor(out=EQ1.t[:], in0=LG.t[:], in1=bc(M1.t[:, :]), op=ALU.is_equal), reads=[LG.r, M1.r], writes=[EQ1.r])
    P.op("dve", lambda e: e.scalar_tensor_tensor(out=LG2.t[:], in0=EQ1.t[:], scalar=-1e30, in1=LG.t[:], op0=ALU.mult, op1=ALU.add),
         reads=[EQ1.r, LG.r], writes=[LG2.r])
    P.op("dve", lambda e: e.tensor_reduce(out=M2.t[:], in_=LG2.t[:], axis=AX.X, op=ALU.max), reads=[LG2.r], writes=[M2.r])
    P.op("dve", lambda e: e.tensor_tensor(out=EQ2.t[:], in0=LG2.t[:], in1=bc(M2.t[:, :]), op=ALU.is_equal), reads=[LG2.r, M2.r], writes=[EQ2.r])
    W1v = W12.t[:, :, 0]
    W2v = W12.t[:, :, 1]
    P.op("dve", lambda e: e.tensor_tensor(out=M2.t[:], in0=M2.t[:], in1=M1.t[:], op=ALU.subtract), reads=[M1.r, M2.r], writes=[M2.r])
    P.op("act", lambda e: e.activation(out=M2.t[:], in_=M2.t[:], func=AF.Exp), reads=[M2.r], writes=[M2.r])
    P.op("dve", lambda e: e.tensor_scalar(out=M1.t[:], in0=M2.t[:], scalar1=1.0, scalar2=None, op0=ALU.add), reads=[M2.r], writes=[M1.r])
    P.op("dve", lambda e: e.reciprocal(out=M1.t[:], in_=M1.t[:]), reads=[M1.r], writes=[M1.r])
    P.op("dve", lambda e: e.tensor_copy(out=W1v, in_=M1.t[:]), reads=[M1.r], writes=[W12.r])
    P.op("dve", lambda e: e.tensor_tensor(out=W2v, in0=M2.t[:], in1=M1.t[:], op=ALU.mult), reads=[M1.r, M2.r], writes=[W12.r])
    P.op("dve", lambda e: e.tensor_tensor(out=MASK.t[:], in0=EQ1.t[:], in1=EQ2.t[:], op=ALU.add), reads=[EQ1.r, EQ2.r], writes=[MASK.r])
    pP = pF[0]
    pPv = pP.t[:, :, :].rearrange("p a b -> p (a b)")

    def pfx(e):
        for tt in range(32):
            e.matmul(pPv[:, tt * 8:(tt + 1) * 8], lhsT=lts.t[:], rhs=MASK.t[:, tt, :], start=True, stop=(tt == 0))
            for t2 in range(tt):
                r = e.matmul(pPv[:, tt * 8:(tt + 1) * 8], lhsT=onesb.t[:], rhs=MASK.t[:, t2, :], start=False, stop=(t2 == tt - 1))
        for t2 in range(32):
            r = e.matmul(pPv[:, 256:264], lhsT=onesb.t[:], rhs=MASK.t[:, t2, :], start=(t2 == 0), stop=(t2 == 31))
        return r
    P.op("pe", pfx, reads=[lts.r, onesb.r, MASK.r], writes=[pP.r])
    P.op("dve", lambda e: e.tensor_copy(out=POSE.t[:, :, :].rearrange("p a b -> p (a b)"), in_=pPv[:, 0:256]), reads=[pP.r], writes=[POSE.r])
    P.op("dve", lambda e: e.tensor_copy(out=run.t[:], in_=pPv[:, 256:264]), reads=[pP.r], writes=[run.r])
    for k, EQ in enumerate((EQ1, EQ2)):
        P.op("dve", lambda e, EQ=EQ: e.tensor_tensor(out=T3.t[:], in0=EQ.t[:], in1=POSE.t[:], op=ALU.mult), reads=[EQ.r, POSE.r], writes=[T3.r])
        P.op("dve", lambda e, k=k: e.tensor_reduce(out=POS.t[:, k, :], in_=T3.t[:], axis=AX.X, op=ALU.add), reads=[T3.r], writes=[POS.r])

    thr = Buf(P, "rthr", [128, 8, 8], F32)
    cmp = Buf(P, "rcmp", [128, 8, 8], F32)
    tiles = Buf(P, "rtiles", [128, 8], F32)
    ca = [Buf(P, f"rca{i}", [128, 8], F32) for i in range(3)]
    base = Buf(P, "rbase", [128, 8], F32)
    for k in range(8):
        P.op("pool", lambda e, k=k: e.memset(thr.t[:, :, k:k + 1], 512.0 * k), writes=[thr.r])
    P.op("dve", lambda e: e.tensor_tensor(out=cmp.t[:], in0=run.t[:, :].unsqueeze(2).to_broadcast([128, 8, 8]), in1=thr.t[:], op=ALU.is_gt),
         reads=[run.r, thr.r], writes=[cmp.r])
    P.op("dve", lambda e: e.tensor_reduce(out=tiles.t[:], in_=cmp.t[:], axis=AX.X, op=ALU.add), reads=[cmp.r], writes=[tiles.r])
    prev = tiles
    for i, sh in enumerate((1, 2, 4)):
        cur = ca[i]
        P.op("dve", lambda e, cur=cur, prev=prev: e.tensor_copy(out=cur.t[:], in_=prev.t[:]), reads=[prev.r], writes=[cur.r])
        P.op("dve", lambda e, cur=cur, prev=prev, sh=sh: e.tensor_tensor(out=cur.t[:, sh:8], in0=cur.t[:, sh:8], in1=prev.t[:, 0:8 - sh], op=ALU.add),
             reads=[prev.r, cur.r], writes=[cur.r])
        prev = cur
    incl = prev
    P.op("dve", lambda e: e.tensor_tensor(out=base.t[:], in0=incl.t[:], in1=tiles.t[:], op=ALU.subtract), reads=[incl.r, tiles.r], writes=[base.r])
    P.op("dve", lambda e: e.tensor_scalar(out=base.t[:], in0=base.t[:], scalar1=512.0, scalar2=None, op0=ALU.mult), reads=[base.r], writes=[base.r])
    jv = Buf(P, "rjv", [128, NJ, 8], F32)
    cj = Buf(P, "rcj", [128, NJ, 8], F32)
    ej = Buf(P, "rej", [128, NJ], F32)
    for j in range(NJ):
        P.op("pool", lambda e, j=j: e.memset(jv.t[:, j:j + 1, :], float(j)), writes=[jv.r])
    P.op("dve", lambda e: e.tensor_tensor(out=cj.t[:], in0=incl.t[:, :].unsqueeze(1).to_broadcast([128, NJ, 8]), in1=jv.t[:], op=ALU.is_le),
         reads=[incl.r, jv.r], writes=[cj.r])
    P.op("dve", lambda e: e.tensor_reduce(out=ej.t[:], in_=cj.t[:], axis=AX.X, op=ALU.add), reads=[cj.r], writes=[ej.r])
    P.op("dve", lambda e: e.tensor_scalar(out=ej.t[:], in0=ej.t[:], scalar1=7.0, scalar2=None, op0=ALU.min), reads=[ej.r], writes=[ej.r])
    cgi = Buf(P, "rcgi", [128, 32], I32)
    cgf = Buf(P, "rcgf", [128, 32], F32)
    cdi = Buf(P, "rcdi", [128, 4], I32)
    cdf = Buf(P, "rcdf", [128, 4], F32)
    igf = Buf(P, "rigf", [128, NJ, 32], F32)
    idf = Buf(P, "ridf", [128, NJ, 4], F32)
    igi = Buf(P, "rigi", [128, NJ * 32], I32)
    idi = Buf(P, "ridi", [128, NJ * 4], I32)
    P.op("pool", lambda e: e.iota(cgi.t[:], pattern=[[1, 32]], base=0, channel_multiplier=32), writes=[cgi.r])
    P.op("pool", lambda e: e.iota(cdi.t[:], pattern=[[128, 4]], base=0, channel_multiplier=1), writes=[cdi.r])
    P.op("dve", lambda e: e.tensor_copy(out=cgf.t[:], in_=cgi.t[:]), reads=[cgi.r], writes=[cgf.r])
    P.op("dve", lambda e: e.tensor_copy(out=cdf.t[:], in_=cdi.t[:]), reads=[cdi.r], writes=[cdf.r])
    P.op("dve", lambda e: e.scalar_tensor_tensor(out=igf.t[:], in0=ej.t[:, :].unsqueeze(2).to_broadcast([128, NJ, 32]), scalar=4096.0,
                                                 in1=cgf.t[:, :].unsqueeze(1).to_broadcast([128, NJ, 32]), op0=ALU.mult, op1=ALU.add),
         reads=[ej.r, cgf.r], writes=[igf.r])
    P.op("dve", lambda e: e.scalar_tensor_tensor(out=idf.t[:], in0=ej.t[:, :].unsqueeze(2).to_broadcast([128, NJ, 4]), scalar=512.0,
                                                 in1=cdf.t[:, :].unsqueeze(1).to_broadcast([128, NJ, 4]), op0=ALU.mult, op1=ALU.add),
         reads=[ej.r, cdf.r], writes=[idf.r])
    P.op("dve", lambda e: e.tensor_copy(out=igi.t[:], in_=igf.t[:, :, :].rearrange("p j r -> p (j r)")), reads=[igf.r], writes=[igi.r])
    P.op("dve", lambda e: e.tensor_copy(out=idi.t[:], in_=idf.t[:, :, :].rearrange("p j r -> p (j r)")), reads=[idf.r], writes=[idi.r])
    sl3 = Buf(P, "rsl3", [128, 32, 8], F32)
    slf = Buf(P, "rslf", [128, 2, 32], F32)
    sli = Buf(P, "rsli", [128, 2 * 32], I32)
    for k, EQ in enumerate((EQ1, EQ2)):
        P.op("dve", lambda e, EQ=EQ: e.tensor_tensor(out=sl3.t[:], in0=EQ.t[:], in1=base.t[:, :].unsqueeze(1).to_broadcast([128, 32, 8]), op=ALU.mult),
             reads=[EQ.r, base.r], writes=[sl3.r])
        P.op("dve", lambda e, k=k: e.tensor_reduce(out=slf.t[:, k, :], in_=sl3.t[:], axis=AX.X, op=ALU.add), reads=[sl3.r], writes=[slf.r])
    P.op("dve", lambda e: e.tensor_tensor(out=slf.t[:], in0=slf.t[:], in1=POS.t[:], op=ALU.add), reads=[slf.r, POS.r], writes=[slf.r])
    P.op("dve", lambda e: e.tensor_copy(out=sli.t[:], in_=slf.t[:, :, :].rearrange("p k t -> p (k t)")), reads=[slf.r], writes=[sli.r])
    tokid = Buf(P, "rtokid", [128, 32], I32)
    zi = Buf(P, "rzi", [128, NSLOT // 128], I32)
    P.op("pool", lambda e: e.iota(tokid.t[:], pattern=[[128, 32]], base=0, channel_multiplier=1), writes=[tokid.r])
    P.op("pool", lambda e: e.memset(zi.t[:], 0), writes=[zi.r])
    P.dma("sp", io["slot_tok"].rearrange("(p i) o -> p (i o)", p=128), zi.t[:], reads=[zi.r], writes=[io["r_slot"]])
    for tt in range(32):
        for k in range(2):
            P.dma("pool", None, None, fn=lambda e, tt=tt, k=k: e.indirect_dma_start(
                out=io["slot_tok"][:, :], out_offset=bass.IndirectOffsetOnAxis(ap=sli.t[:, k * 32 + tt:k * 32 + tt + 1], axis=0),
                in_=tokid.t[:, tt:tt + 1], in_offset=None, bounds_check=None),
                reads=[sli.r, tokid.r, io["r_slot"]])
    P.dma("sp", io["t_sli"], sli.t[:], reads=[sli.r], writes=[io["r_tab"]])
    P.dma("sp", io["t_w12"], W12.t[:, :, :].rearrange("p t k -> p (t k)"), reads=[W12.r], writes=[io["r_tab"]])
    P.dma("sp", io["t_igi"], igi.t[:], reads=[igi.r], writes=[io["r_tab"]])
    P.dma("sp", io["t_idi"], idi.t[:], reads=[idi.r], writes=[io["r_tab"]])


def moe_experts(P, C, io, nj=NJ):
    ident = C["ident"]
    sli = Buf(P, "esli", [128, 64], I32)
    W12 = Buf(P, "eW12", [128, 64], F32)
    igi = Buf(P, "eigi", [128, NJ * 32], I32)
    idi = Buf(P, "eidi", [128, NJ * 4], I32)
    P.dma("sp", sli.t[:], io["t_sli"], writes=[sli.r])
    P.dma("sp", W12.t[:], io["t_w12"], writes=[W12.r])
    P.dma("sp", igi.t[:], io["t_igi"], writes=[igi.r])
    P.dma("sp", idi.t[:], io["t_idi"], writes=[idi.r])
    wd_v = io["wd"].rearrange("e (r i) n -> (e r) (i n)", i=7)
    TI = [Buf(P, f"eTI{i}", [128, 4], I32) for i in range(2)]
    Xg = Buf(P, "eXg", [128, 4, D], BF16)
    xgT = [Buf(P, f"exgT{i}", [128, 8, 512], BF16) for i in range(2)]
    Wgu = [Buf(P, f"eWgu{i}", [128, 2, 8, FQ], BF16) for i in range(2)]
    Wd = Buf(P, "eWd", [128, 28, D], BF16)
    rWd = [P.res(f"eWdq{q}") for q in range(4)]
    hT = Buf(P, "ehT", [128, 28, 512], BF16)
    sgt = [Buf(P, f"esg{i}", [128, 512], F32) for i in range(2)]
    ysb = [Buf(P, f"eys{i}", [128, D], F32) for i in range(2)]
    xt = [Buf(P, f"ext{i}", [128, D], F32) for i in range(2)]
    pT2 = [Buf(P, f"epT{i}", [128, 8, 128], BF16, psum=True) for i in range(2)]
    pA = [Buf(P, f"epA{i}", [128, 512], F32, psum=True) for i in range(2)]
    pB = [Buf(P, f"epB{i}", [128, 512], F32, psum=True) for i in range(2)]
    pY = [Buf(P, f"epY{i}", [128, 512], F32, psum=True) for i in range(2)]
    P.op("pool", lambda e: e.memset(Xg.t[:], 0.0), writes=[Xg.r])

    nq = 0
    nev = 0
    ny = 0
    def tok_stage(j):
        ti = TI[j % 2]
        XT = xgT[j % 2]
        P.dma("sp", ti.t[:], io["slot_tok"][j * 512:(j + 1) * 512, :].rearrange("(i p) o -> p (i o)", p=128),
              reads=[io["r_slot"]], writes=[ti.r], allow_slow_non_contiguous=True)
        for i in range(4):
            P.dma("pool", None, None, fn=lambda e, i=i, ti=ti: e.indirect_dma_start(
                out=Xg.t[:, i, :], out_offset=None, in_=io["xn_d"][:, :],
                in_offset=bass.IndirectOffsetOnAxis(ap=ti.t[:, i:i + 1], axis=0), bounds_check=None),
                reads=[ti.r, io["r_xn"], Xg.r], writes=[Xg.r])
        for i in range(4):
            pT = pT2[i % 2]

            def tr(e, i=i, pT=pT):
                src = Xg.t[:, i, :].rearrange("p (m c) -> p c m", c=8)
                for dc in range(8):
                    r = e.transpose(out=pT.t[:, dc, :], in_=src[:, dc, :], identity=ident.t[:])
                return r
            P.op("pe", tr, reads=[Xg.r, ident.r], writes=[pT.r])
            if i % 2 == 0:
                P.op("act", lambda e, i=i, XT=XT, pT=pT: e.copy(out=XT.t[:, :, i * 128:(i + 1) * 128], in_=pT.t[:]), reads=[pT.r], writes=[XT.r])
            else:
                P.op("dve", lambda e, i=i, XT=XT, pT=pT: e.tensor_copy(out=XT.t[:, :, i * 128:(i + 1) * 128], in_=pT.t[:]), reads=[pT.r], writes=[XT.r])

    tok_stage(0)
    for j in range(nj):
        XT = xgT[j % 2]
        def wd_dmas(j=j):
            for q in range(4):
                P.dma("pool", None, None, fn=lambda e, q=q, j=j: e.indirect_dma_start(
                    out=Wd.t[:, q * 7:(q + 1) * 7, :].rearrange("p a b -> p (a b)"), out_offset=None, in_=wd_v[:, :],
                    in_offset=bass.IndirectOffsetOnAxis(ap=idi.t[:, j * 4 + q:j * 4 + q + 1], axis=0), bounds_check=None),
                    reads=[idi.r, rWd[q]], writes=[rWd[q]])

        def gu_dmas(q, WGU, j=j):
            P.dma("pool", None, None, fn=lambda e, q=q, j=j, WGU=WGU: e.indirect_dma_start(
                out=WGU.t[:, :, :, :].rearrange("p a b c -> p (a b c)"), out_offset=None, in_=io["wgu"][:, :],
                in_offset=bass.IndirectOffsetOnAxis(ap=idi.t[:, j * 4 + q:j * 4 + q + 1], axis=0), bounds_check=None),
                reads=[idi.r, WGU.r], writes=[WGU.r])

        def gu_compute(q, WGU, XT=XT):
            nonlocal nev
            for i in range(7):
                c = q * 7 + i
                A = pA[nev % 2]
                B = pB[nev % 2]
                SG = sgt[nev % 2]
                nev += 1

                def mm(e, W=WGU, O=A, i=i, XT=XT):
                    for dc in range(8):
                        r = e.matmul(O.t[:], lhsT=W.t[:, 0, dc, i:FQ:7], rhs=XT.t[:, dc, :], start=(dc == 0), stop=(dc == 7))
                    return r
                P.op("pe", mm, reads=[WGU.r, XT.r], writes=[A.r])

                def mm2(e, W=WGU, O=B, i=i, XT=XT):
                    for dc in range(8):
                        r = e.matmul(O.t[:], lhsT=W.t[:, 1, dc, i:FQ:7], rhs=XT.t[:, dc, :], start=(dc == 0), stop=(dc == 7))
                    return r
                P.op("pe", mm2, reads=[WGU.r, XT.r], writes=[B.r])
                P.op("act", lambda e, A=A, SG=SG: e.activation(out=SG.t[:], in_=A.t[:], func=AF.Silu), reads=[A.r], writes=[SG.r])
                P.op("dve", lambda e, B=B, SG=SG, c=c: e.tensor_tensor(out=hT.t[:, c, :], in0=B.t[:], in1=SG.t[:], op=ALU.mult),
                     reads=[B.r, SG.r], writes=[hT.r])

        bufs = [Wgu[(nq + q) % 2] for q in range(4)]
        nq += 4
        gu_dmas(0, bufs[0])
        gu_dmas(1, bufs[1])
        gu_compute(0, bufs[0])
        wd_dmas()
        gu_dmas(2, bufs[2])
        gu_compute(1, bufs[1])
        gu_dmas(3, bufs[3])
        gu_compute(2, bufs[2])
        if j + 1 < nj:
            tok_stage(j + 1)
        gu_compute(3, bufs[3])
        for tt in range(4):
            YS = ysb[ny % 2]
            ny += 1
            for dh in range(2):
                Y = pY[dh]

                def mmd(e, Y=Y, tt=tt, dh=dh):
                    for c in range(28):
                        r = e.matmul(Y.t[:], lhsT=hT.t[:, c, tt * 128:(tt + 1) * 128], rhs=Wd.t[:, c, dh * 512:(dh + 1) * 512],
                                     start=(c == 0), stop=(c == 27))
                    return r
                P.op("pe", mmd, reads=[hT.r] + rWd, writes=[Y.r])
                if dh == 0:
                    P.op("act", lambda e, Y=Y, YS=YS: e.copy(out=YS.t[:, 0:512], in_=Y.t[:]), reads=[Y.r], writes=[YS.r])
                else:
                    P.op("dve", lambda e, Y=Y, YS=YS: e.tensor_copy(out=YS.t[:, 512:1024], in_=Y.t[:]), reads=[Y.r], writes=[YS.r])
            r0 = j * 512 + tt * 128
            P.dma("act", io["ys_d"][r0:r0 + 128, :], YS.t[:], reads=[YS.r], writes=[io["r_ys"]])

    hflat = hT.t[:, :, :].rearrange("p a b -> p (a b)").bitcast(F32)
    ybufs = [(hflat[:, i * D:(i + 1) * D], P.res(f"yb{i}")) for i in range(6)]
    xbuf = [Buf(P, f"ecx{i}", [128, D], F32) for i in range(1)] + xt
    first = True
    for tt in range(32 if nj == NJ else 0):
        X = xbuf[tt % 3]
        P.dma("sp", X.t[:], io["x_in"][tt * 128:(tt + 1) * 128, :], writes=[X.r])
        for k in range(2):
            yap, yr = ybufs[(tt % 3) * 2 + k]
            extra = [hT.r] if tt < 3 else []
            P.dma("pool", None, None, fn=lambda e, k=k, tt=tt, yap=yap: e.indirect_dma_start(
                out=yap, out_offset=None, in_=io["ys_d"][:, :],
                in_offset=bass.IndirectOffsetOnAxis(ap=sli.t[:, k * 32 + tt:k * 32 + tt + 1], axis=0), bounds_check=None),
                reads=[sli.r, io["r_ys"], yr], writes=[yr] + extra)
            P.op("dve", lambda e, k=k, tt=tt, yap=yap, X=X: e.scalar_tensor_tensor(
                out=X.t[:], in0=yap, scalar=W12.t[:, tt * 2 + k:tt * 2 + k + 1], in1=X.t[:], op0=ALU.mult, op1=ALU.add),
                reads=[yr, W12.r, X.r], writes=[X.r])
        P.dma("act", io["x_out"][tt * 128:(tt + 1) * 128, :], X.t[:], reads=[X.r])

import numpy as np
from concourse.bass_utils import run_bass_kernel_spmd

_NC_CACHE = {}


def build_program(dbg=False):
    nc = bass.Bass("TRN2", target_bir_lowering=False)
    def dt(name, shape, kind="ExternalInput", dtype=F32):
        if dbg and kind == "Internal":
            kind = "ExternalOutput"
        return nc.dram_tensor(name, list(shape), dtype, kind=kind).ap()
    x = dt("x", [S, D])
    mix_norm = dt("mix_norm", [2, D])
    ffn_norm = dt("ffn_norm", [2, D])
    w_in = dt("mlstm_w_in", [D, 3088])
    b_ig = dt("mlstm_b_igate", [8])
    b_fg = dt("mlstm_b_fgate", [8])
    g_h = dt("mlstm_g_h", [D])
    w_out = dt("mlstm_w_out", [D, D])
    kv_norm = dt("kv_norm", [D])
    w_kv = dt("w_kv", [D, 2048])
    g_k = dt("g_k", [64])
    w_q = dt("sb_w_q", [D, D])
    g_q = dt("sb_g_q", [64])
    w_o = dt("sb_w_o", [D, D])
    f_wg = dt("ffn_w_gate", [1, D, 2816])
    f_wu = dt("ffn_w_up", [1, D, 2816])
    f_wd = dt("ffn_w_down", [1, 2816, D])
    m_wr = dt("moe_w_router", [D, 8])
    m_wgu = dt("moe_w_gu", [8 * 4 * 128, 2 * 8 * 896])
    m_wd = dt("moe_w_down", [8, 3584, D])
    out = dt("out", [S, D], kind="ExternalOutput")
    x1 = dt("scr_x1", [S, D], kind="Internal")
    x2 = dt("scr_x2", [S, D], kind="Internal")
    x3 = dt("scr_x3", [S, D], kind="Internal")
    oT = dt("scr_oT", [8, 128, S], kind="Internal", dtype=BF16)

    P = Prog(nc, "p1_")
    C = make_consts(P)
    phase1(P, C, dict(x=x, x1=x1, w_in=w_in, w_out=w_out, mix_norm0=mix_norm[0], g_h=g_h, b_ig=b_ig, b_fg=b_fg))
    P.emit()
    P.close()
    nc.all_engine_barrier()
    P = Prog(nc, "p2_")
    C = make_consts(P)
    ffn_dense(P, C, dict(x_in=x1, x_out=x2, norm_g=ffn_norm[0], wg=f_wg, wu=f_wu, wd=f_wd), 2816, NTOK=1024, tag="f")
    P.emit()
    P.close()
    nc.all_engine_barrier()
    P = Prog(nc, "p3_")
    C = make_consts(P)
    phase3(P, C, dict(x_in=x2, x_out=x3, oT=oT, r_oT=P.res("oT"), mix_norm1=mix_norm[1], kv_norm=kv_norm, g_q=g_q, g_k=g_k,
                      w_q=w_q, w_kv=w_kv, w_o=w_o))
    P.emit()
    P.close()
    nc.all_engine_barrier()
    xn_d = dt("scr_xn", [S, D], kind="Internal", dtype=BF16)
    slot_tok = dt("scr_slot_tok", [NSLOT, 1], kind="Internal", dtype=I32)
    ys_d = dt("scr_ys", [NSLOT, D], kind="Internal")
    t_sli = dt("scr_sli", [128, 64], kind="Internal", dtype=I32)
    t_w12 = dt("scr_w12", [128, 64], kind="Internal")
    t_igi = dt("scr_igi", [128, NJ * 32], kind="Internal", dtype=I32)
    t_idi = dt("scr_idi", [128, NJ * 4], kind="Internal", dtype=I32)
    io4 = dict(x_in=x3, x_out=out, norm_g=ffn_norm[1], router=m_wr, wgu=m_wgu, wd=m_wd, xn_d=xn_d, slot_tok=slot_tok,
               ys_d=ys_d, t_sli=t_sli, t_w12=t_w12, t_igi=t_igi, t_idi=t_idi)
    P = Prog(nc, "p4a_")
    for k in ("r_xn", "r_slot", "r_ys", "r_tab"):
        io4[k] = P.res(k)
    C = make_consts(P)
    moe_route(P, C, io4)
    P.emit()
    P.close()
    nc.all_engine_barrier()
    P = Prog(nc, "p4b_")
    for k in ("r_xn", "r_slot", "r_ys", "r_tab"):
        io4[k] = P.res(k)
    C = make_consts(P)
    moe_experts(P, C, io4)
    P.emit()
    P.close()
    return nc


def kernel(**inputs):
    if "nc" not in _NC_CACHE:
        _NC_CACHE["nc"] = build_program()
    nc = _NC_CACHE["nc"]
    f = lambda k: np.ascontiguousarray(np.asarray(inputs[k], dtype=np.float32))
    shared = {
        "mix_norm": f("mix_norm"), "ffn_norm": f("ffn_norm"),
        "mlstm_w_in": f("mlstm_w_in")[0], "mlstm_b_igate": f("mlstm_b_igate")[0], "mlstm_b_fgate": f("mlstm_b_fgate")[0],
        "mlstm_g_h": f("mlstm_g_h")[0], "mlstm_w_out": f("mlstm_w_out")[0],
        "kv_norm": f("kv_norm"), "w_kv": f("w_kv"), "g_k": f("g_k"),
        "sb_w_q": f("sb_w_q")[0], "sb_g_q": f("sb_g_q")[0], "sb_w_o": f("sb_w_o")[0],
        "ffn_w_gate": f("ffn_w_gate"), "ffn_w_up": f("ffn_w_up"), "ffn_w_down": f("ffn_w_down"),
        "moe_w_router": f("moe_w_router")[0], "moe_w_down": f("moe_w_down")[0],
    }
    gu = np.stack([f("moe_w_gate")[0], f("moe_w_up")[0]], axis=1).reshape(8, 2, 128, 8, 4, 896)
    shared["moe_w_gu"] = np.ascontiguousarray(gu.transpose(0, 4, 2, 1, 3, 5)).reshape(8 * 4 * 128, 2 * 8 * 896)
    xs = f("x")
    in_maps = [dict(shared, x=np.ascontiguousarray(xs[b])) for b in range(8)]
    res = run_bass_kernel_spmd(nc, in_maps, core_ids=list(range(8)))
    return np.stack([np.asarray(res.results[b]["out"], dtype=np.float32) for b in range(8)], axis=0)
```

```python
from contextlib import ExitStack
import concourse.bass as bass
import concourse.mybir as mybir

F32 = mybir.dt.float32
BF16 = mybir.dt.bfloat16
I32 = mybir.dt.int32
AF = mybir.ActivationFunctionType
ALU = mybir.AluOpType
AX = mybir.AxisListType

COMPUTE = ("pe", "act", "dve", "pool")
NDMA_SLOTS = 16


class Res:
    __slots__ = ("name", "w", "r", "excl")

    def __init__(self, name):
        self.name = name
        self.excl = False
        self.w = None
        self.r = []


class Instr:
    __slots__ = ("eng", "fn", "deps", "need_inc", "inc_idx", "is_dma", "slot", "slot_val", "prev_slot_val", "pos")

    def __init__(self, eng, fn):
        self.eng = eng
        self.fn = fn
        self.deps = []
        self.need_inc = False
        self.inc_idx = 0
        self.is_dma = False
        self.slot = None
        self.slot_val = 0
        self.prev_slot_val = 0
        self.pos = 0


_GSEM = {}


class Prog:
    def __init__(self, nc, prefix=""):
        self.nc = nc
        self.prefix = prefix
        self.stack = ExitStack()
        self.streams = {e: [] for e in ("pe", "act", "dve", "pool", "sp")}
        self.dma_count = {e: 0 for e in ("act", "pool", "sp")}
        self.nres = 0
        self.max_ops = None
        self.nops = 0

    def sbuf(self, name, shape, dtype):
        return self.stack.enter_context(self.nc.sbuf_tensor(self.prefix + "sb_" + name, list(shape), dtype))

    def psum(self, name, shape, dtype):
        return self.stack.enter_context(self.nc.psum_tensor(self.prefix + "ps_" + name, list(shape), dtype))

    def res(self, name=None):
        self.nres += 1
        return Res(name or f"r{self.nres}")

    def ress(self, n, name="r"):
        return [self.res(f"{name}{i}") for i in range(n)]

    def _track(self, ins, reads, writes):
        writes = list(writes) + [r for r in reads if r.excl and r not in writes]
        reads = [r for r in reads if not r.excl]
        deps = []
        for r in reads:
            if r.w is not None:
                deps.append(r.w)
        for w in writes:
            if w.w is not None:
                deps.append(w.w)
            deps.extend(w.r)
        for r in reads:
            r.r.append(ins)
        for w in writes:
            w.w = ins
            w.r = []
        seen = set()
        for d in deps:
            if d is ins or id(d) in seen:
                continue
            seen.add(id(d))
            if d.eng == "pe" and ins.eng == "pe" and not d.is_dma and not ins.is_dma:
                continue
            ins.deps.append(d)

    def begin_capture(self):
        self._cap = []

    def end_capture(self):
        c, self._cap = self._cap, None
        return c

    def replay_interleaved(self, a, b, chunk=1):
        ca = [a[i:i + chunk] for i in range(0, len(a), chunk)]
        nb = max(1, (len(b) * chunk + len(a) - 1) // max(1, len(a)))
        cb = [b[i:i + nb] for i in range(0, len(b), nb)]
        seq = []
        for i in range(max(len(ca), len(cb))):
            if i < len(ca):
                seq += ca[i]
            if i < len(cb):
                seq += cb[i]
        for k, args, kw in seq:
            (self.op if k == "op" else self.dma)(*args, **kw)

    def op(self, eng, fn, reads=(), writes=()):
        if getattr(self, "_cap", None) is not None:
            self._cap.append(("op", (eng, fn), dict(reads=list(reads), writes=list(writes))))
            return None
        self.nops += 1
        if self.max_ops is not None and self.nops > self.max_ops:
            return None
        ins = Instr(eng, fn)
        ins.pos = len(self.streams[eng])
        self.streams[eng].append(ins)
        self._track(ins, list(reads), list(writes))
        return ins

    def dma(self, q, out, in_, reads=(), writes=(), fn=None, **kw):
        if getattr(self, "_cap", None) is not None:
            self._cap.append(("dma", (q, out, in_), dict(reads=list(reads), writes=list(writes), fn=fn, **kw)))
            return None
        if fn is None:
            def fn(e):
                return e.dma_start(out=out, in_=in_, **kw)
        self.nops += 1
        if self.max_ops is not None and self.nops > self.max_ops:
            return None
        ins = Instr(q, fn)
        ins.is_dma = True
        n = self.dma_count[q]
        self.dma_count[q] = n + 1
        ins.slot = (q, n % NDMA_SLOTS)
        ins.slot_val = 16 * (n // NDMA_SLOTS + 1)
        ins.prev_slot_val = ins.slot_val - 16
        ins.pos = len(self.streams[q])
        self.streams[q].append(ins)
        self._track(ins, list(reads), list(writes))
        return ins

    def emit(self):
        nc = self.nc
        for e, st in self.streams.items():
            for ins in st:
                for d in ins.deps:
                    if not d.is_dma:
                        d.need_inc = True
        for e, st in self.streams.items():
            c = 0
            for ins in st:
                if not ins.is_dma and ins.need_inc:
                    c += 1
                    ins.inc_idx = c
        G = _GSEM.setdefault(id(nc), None)
        if G is None:
            G = {"sems": {}, "dsem": {}, "base": {}}
            for e in COMPUTE:
                G["sems"][e] = nc.semaphore(f"g_s_{e}").__enter__()
            for q in ("act", "pool", "sp"):
                for s in range(NDMA_SLOTS):
                    G["dsem"][(q, s)] = nc.semaphore(f"g_d_{q}{s}").__enter__()
            _GSEM[id(nc)] = G
        sems, dsem, base = G["sems"], G["dsem"], G["base"]
        for e, st in self.streams.items():
            for ins in st:
                if ins.is_dma:
                    b0 = base.get(ins.slot, 0)
                    ins.slot_val += b0
                    ins.prev_slot_val += b0
                else:
                    ins.inc_idx += base.get(e, 0)
        for e in COMPUTE:
            n_inc = sum(1 for ins in self.streams[e] if (not ins.is_dma) and ins.need_inc)
            base[e] = base.get(e, 0) + n_inc
        for q in ("act", "pool", "sp"):
            for ins in self.streams[q]:
                if ins.is_dma:
                    base[ins.slot] = max(base.get(ins.slot, 0), ins.slot_val)
        print("[fw] phase", self.prefix, "sem values:", {e: base.get(e, 0) for e in COMPUTE}, flush=True)
        block = self.stack.enter_context(nc.Block())
        streams = self.streams
        final_waits = []
        last_slot_val = {}
        for q in ("act", "pool", "sp"):
            for ins in streams[q]:
                if ins.is_dma:
                    last_slot_val[ins.slot] = ins.slot_val

        def run(ename, eng):
            known = {}

            def wait(sem_key, sem, val):
                if known.get(sem_key, 0) >= val:
                    return
                known[sem_key] = val
                eng.wait_ge(sem, val)

            for ins in streams[ename]:
                for d in ins.deps:
                    if d.is_dma:
                        wait(d.slot, dsem[d.slot], d.slot_val)
                    else:
                        wait(d.eng, sems[d.eng], d.inc_idx)
                if ins.is_dma:
                    if ins.prev_slot_val > 0 and not getattr(ins, 'first_use', False):
                        wait(ins.slot, dsem[ins.slot], ins.prev_slot_val)
                    bi = ins.fn(eng)
                    bi.then_inc(dsem[ins.slot], 16)
                else:
                    bi = ins.fn(eng)
                    if ins.need_inc:
                        bi.then_inc(sems[ename], 1)
            if ename == "sp":
                for slot, v in last_slot_val.items():
                    wait(slot, dsem[slot], v)

        @block.sync
        def _(e):
            run("sp", e)

        @block.tensor
        def _(e):
            run("pe", e)

        @block.scalar
        def _(e):
            run("act", e)

        @block.vector
        def _(e):
            run("dve", e)

        @block.gpsimd
        def _(e):
            run("pool", e)

    def close(self):
        self.stack.close()


D = 1024
S = 4096
NTILES = S // 128
NEG = -30000.0


class Buf:
    def __init__(self, P, name, shape, dtype, psum=False):
        self.t = (P.psum if psum else P.sbuf)(name, shape, dtype)
        self.r = P.res(name)
        self.r.excl = psum


def make_consts(P):
    C = {}
    ident = Buf(P, "ident", [128, 128], BF16)
    identf = Buf(P, "identf", [128, 128], F32)
    U = Buf(P, "U", [128, 128], F32)
    onesf = Buf(P, "onesf", [128, 128], F32)
    maskneg = Buf(P, "maskneg", [128, 128], F32)
    epsb = Buf(P, "epsb", [128, 1], F32)
    one1 = Buf(P, "one1", [128, 1], F32)
    P.op("pool", lambda e: e.memset(epsb.t[:], 1e-6), writes=[epsb.r])
    P.op("pool", lambda e: e.memset(one1.t[:], 1.0), writes=[one1.r])
    P.op("pool", lambda e: e.memset(onesf.t[:], 1.0), writes=[onesf.r])
    P.op("pool", lambda e: e.memset(identf.t[:], 1.0), writes=[identf.r])
    P.op("pool", lambda e: e.affine_select(out=identf.t[:], in_=identf.t[:], pattern=[[-1, 128]],
                                           compare_op=ALU.is_equal, fill=0.0, base=0, channel_multiplier=1),
         reads=[identf.r], writes=[identf.r])
    P.op("dve", lambda e: e.tensor_copy(out=ident.t[:], in_=identf.t[:]), reads=[identf.r], writes=[ident.r])
    P.op("pool", lambda e: e.affine_select(out=U.t[:], in_=onesf.t[:], pattern=[[1, 128]],
                                           compare_op=ALU.is_ge, fill=0.0, base=0, channel_multiplier=-1),
         reads=[onesf.r], writes=[U.r])
    P.op("pool", lambda e: e.memset(maskneg.t[:], 0.0), writes=[maskneg.r])
    P.op("pool", lambda e: e.affine_select(out=maskneg.t[:], in_=maskneg.t[:], pattern=[[1, 128]],
                                           compare_op=ALU.is_ge, fill=NEG, base=0, channel_multiplier=-1),
         reads=[maskneg.r], writes=[maskneg.r])
    C.update(ident=ident, identf=identf, U=U, onesf=onesf, maskneg=maskneg, epsb=epsb, one1=one1)
    return C


def rmsnorm_T(P, C, xt, gbc, ss, junk, xn, pT, xnT):
    P.op("act", lambda e: e.activation(out=junk.t[:], in_=xt.t[:], func=AF.Square, accum_out=ss.t[:]),
         reads=[xt.r], writes=[junk.r, ss.r])
    P.op("act", lambda e: e.activation(out=ss.t[:], in_=ss.t[:], func=AF.Sqrt, scale=1.0 / D, bias=C["epsb"].t[:]),
         reads=[ss.r, C["epsb"].r], writes=[ss.r])
    P.op("dve", lambda e: e.reciprocal(out=ss.t[:], in_=ss.t[:]), reads=[ss.r], writes=[ss.r])
    P.op("dve", lambda e: e.scalar_tensor_tensor(out=xn.t[:], in0=xt.t[:], scalar=ss.t[:, 0:1], in1=gbc.t[:],
                                                 op0=ALU.mult, op1=ALU.mult),
         reads=[xt.r, ss.r, gbc.r], writes=[xn.r])

    def tr(e):
        for c in range(8):
            i = e.transpose(out=pT.t[:, c, :], in_=xn.t[:, c * 128:(c + 1) * 128], identity=C["ident"].t[:])
        return i
    P.op("pe", tr, reads=[xn.r, C["ident"].r], writes=[pT.r])
    P.op("act", lambda e: e.copy(out=xnT.t[:], in_=pT.t[:]), reads=[pT.r], writes=[xnT.r])


def phase1(P, C, io, ntiles=NTILES):
    x, x1 = io["x"], io["x1"]
    w_in_d, w_out_d = io["w_in"], io["w_out"]
    w_in = Buf(P, "w_in", [128, 8, 3088], BF16)
    w_out = Buf(P, "w_out", [128, 8, 1024], BF16)
    gbc = Buf(P, "gbc1", [128, D], F32)
    ghbc = Buf(P, "ghbc", [128, D], F32)
    bgb = Buf(P, "bgb", [128, 16], F32)
    w_in_v = w_in_d.rearrange("(c p) n -> p c n", p=128)
    for i, (a, b) in enumerate([(3072, 3088), (0, 1024), (1024, 2048), (2048, 3072)]):
        P.dma("pool", w_in.t[:, :, a:b], w_in_v[:, :, a:b], writes=[w_in.r])
    P.dma("pool", w_out.t[:], w_out_d.rearrange("(c p) n -> p c n", p=128), writes=[w_out.r])
    P.dma("sp", gbc.t[:], io["mix_norm0"].partition_broadcast(128), writes=[gbc.r])
    P.dma("sp", ghbc.t[:], io["g_h"].partition_broadcast(128), writes=[ghbc.r])
    P.dma("sp", bgb.t[:, 0:8], io["b_ig"].partition_broadcast(128), writes=[bgb.r])
    P.dma("sp", bgb.t[:, 8:16], io["b_fg"].partition_broadcast(128), writes=[bgb.r])

    xt = [Buf(P, f"xt{i}", [128, D], F32) for i in range(2)]
    junk = Buf(P, "junk", [128, D], F32)
    ss = Buf(P, "ss", [128, 1], F32)
    xn = Buf(P, "xn", [128, D], BF16)
    xnT = Buf(P, "xnT", [128, 8, 128], BF16)
    gt = Buf(P, "gt", [128, 16], F32)
    th = Buf(P, "th", [128, 16], F32)
    e1 = Buf(P, "e1", [128, 8], F32)
    logf = Buf(P, "logf", [128, 8], F32)
    logf_rep = Buf(P, "logf_rep", [128, 8, 128], F32)
    cc = Buf(P, "cc", [128, 8], F32)
    wgt = Buf(P, "wgt", [128, 8], F32)
    dec = Buf(P, "dec", [128, 4], F32)
    EBp = Buf(P, "EBp", [128, 4, 128], F32)
    z1 = Buf(P, "z1", [128, 8, 128], F32)
    z2 = Buf(P, "z2", [128, 8, 128], F32)
    E = Buf(P, "E", [128, 8, 128], F32)
    qT = Buf(P, "qT", [128, 4, 128], BF16)
    kT = Buf(P, "kT", [128, 8, 128], BF16)
    QsT = Buf(P, "QsT", [128, 8, 128], BF16)
    ktok = Buf(P, "ktok", [128, 512], F32)
    Kw = Buf(P, "Kw", [128, 4, 2, 2, 64], BF16)
    v_sb = Buf(P, "v_sb", [128, 8, 129], BF16)
    sg = Buf(P, "sg", [128, D], F32)
    ghsg = Buf(P, "ghsg", [128, D], F32)
    PT = Buf(P, "PT", [128, 8, 128], BF16)
    Cs = Buf(P, "Cs", [128, 4, 128], F32)
    Ct = Buf(P, "Ct", [128, 4, 128], F32)
    ns = Buf(P, "ns", [128, 4], F32)
    Cb = Buf(P, "Cb", [128, 4, 129], BF16)
    rr = Buf(P, "rr", [128, 8], F32)
    ssq = Buf(P, "ssq", [128, 8], F32)
    sc = Buf(P, "sc", [128, 8], F32)
    hh = Buf(P, "hh", [128, 8, 128], F32)
    hg = Buf(P, "hg", [128, D], BF16)
    hgT = Buf(P, "hgT", [128, 8, 128], BF16)
    xo = [Buf(P, f"xo{i}", [128, D], F32) for i in range(2)]
    pT = Buf(P, "pT", [128, 8, 128], BF16, psum=True)
    pQK = Buf(P, "pQK", [128, 8, 128], F32, psum=True)
    pPJ = [Buf(P, f"pPJ{i}", [128, 512], F32, psum=True) for i in range(2)]
    pG = Buf(P, "pG", [128, 512], F32, psum=True)
    pBB = Buf(P, "pBB", [128, 8, 128], F32, psum=True)
    rG = {k: pG.r for k in ("gates", "b", "den", "dn")}

    P.op("pool", lambda e: e.memset(Kw.t[:], 0.0), writes=[Kw.r])
    P.op("pool", lambda e: e.memset(kT.t[:], 0.0), writes=[kT.r])
    P.op("pool", lambda e: e.memset(QsT.t[:], 0.0), writes=[QsT.r])
    P.op("pool", lambda e: e.memset(v_sb.t[:], 1.0), writes=[v_sb.r])
    P.op("pool", lambda e: e.memset(Cs.t[:], 0.0), writes=[Cs.r])
    P.op("pool", lambda e: e.memset(ns.t[:], 0.0), writes=[ns.r])
    P.op("pool", lambda e: e.memset(Cb.t[:], 0.0), writes=[Cb.r])

    U, onesf, maskneg = C["U"], C["onesf"], C["maskneg"]

    def proj_tok(col0, ncols, pout, outr):
        def f(e):
            for c in range(8):
                i = e.matmul(pout, lhsT=xnT.t[:, c, :], rhs=w_in.t[:, c, col0:col0 + ncols],
                             start=(c == 0), stop=(c == 7))
            return i
        return f

    P.dma("sp", xt[0].t[:], x[0:128, :], writes=[xt[0].r])
    for t in range(ntiles):
        b = t % 2
        X = xt[b]
        if t + 1 < ntiles:
            P.dma("sp", xt[1 - b].t[:], x[(t + 1) * 128:(t + 2) * 128, :], writes=[xt[1 - b].r])
        rmsnorm_T(P, C, X, gbc, ss, junk, xn, pT, xnT)
        P.op("pe", proj_tok(3072, 16, pG.t[:, 0:16], None), reads=[xnT.r, w_in.r], writes=[rG["gates"]])
        P.op("dve", lambda e: e.tensor_tensor(out=gt.t[:], in0=pG.t[:, 0:16], in1=bgb.t[:], op=ALU.add),
             reads=[rG["gates"], bgb.r], writes=[gt.r])
        P.op("act", lambda e: e.activation(out=th.t[:], in_=gt.t[:], func=AF.Tanh, scale=1.0 / 15.0),
             reads=[gt.r], writes=[th.r])
        P.op("act", lambda e: e.activation(out=e1.t[:], in_=th.t[:, 8:16], func=AF.Exp, scale=-15.0),
             reads=[th.r], writes=[e1.r])
        P.op("act", lambda e: e.activation(out=e1.t[:], in_=e1.t[:], func=AF.Ln, bias=C["one1"].t[:]),
             reads=[e1.r, C["one1"].r], writes=[e1.r])
        P.op("dve", lambda e: e.tensor_scalar(out=logf.t[:], in0=e1.t[:], scalar1=-1.0, scalar2=None, op0=ALU.mult),
             reads=[e1.r], writes=[logf.r])
        P.op("dve", lambda e: e.tensor_copy(out=logf_rep.t[:], in_=logf.t[:, :].unsqueeze(2).to_broadcast([128, 8, 128])),
             reads=[logf.r], writes=[logf_rep.r])

        def cums(e):
            e.matmul(pG.t[:, 16:24], lhsT=U.t[:], rhs=logf.t[:], start=True, stop=True)
            i = e.matmul(pG.t[:, 24:32], lhsT=onesf.t[:], rhs=logf.t[:], start=True, stop=True)
            return i
        P.op("pe", cums, reads=[U.r, onesf.r, logf.r], writes=[rG["b"]])

        def bbc(e):
            for h in range(8):
                i = e.matmul(pBB.t[:, h, :], lhsT=logf_rep.t[:, h, :], rhs=U.t[:], start=True, stop=True)
            return i
        P.op("pe", bbc, reads=[logf_rep.r, U.r], writes=[pBB.r])
        P.op("dve", lambda e: e.scalar_tensor_tensor(out=cc.t[:], in0=th.t[:, 0:8], scalar=15.0, in1=pG.t[:, 16:24],
                                                     op0=ALU.mult, op1=ALU.subtract),
             reads=[th.r, rG["b"]], writes=[cc.r])
        P.op("dve", lambda e: e.tensor_tensor(out=wgt.t[:], in0=pG.t[:, 24:32], in1=cc.t[:], op=ALU.add),
             reads=[rG["b"], cc.r], writes=[wgt.r])
        P.op("act", lambda e: e.activation(out=wgt.t[:], in_=wgt.t[:], func=AF.Exp), reads=[wgt.r], writes=[wgt.r])
        for hf in range(2):
            lo, hi = hf * 64, hf * 64 + 64
            P.op("act", lambda e, lo=lo, hi=hi, hf=hf: e.activation(out=dec.t[lo:hi, :], in_=pG.t[lo:hi, 24 + hf:32:2], func=AF.Exp),
                 reads=[rG["b"]], writes=[dec.r])
            P.op("act", lambda e, lo=lo, hi=hi, hf=hf: e.activation(out=EBp.t[lo:hi, :, :], in_=pBB.t[lo:hi, hf:8:2, :], func=AF.Exp),
                 reads=[pBB.r], writes=[EBp.r])
        P.op("dve", lambda e: e.tensor_tensor(out=z1.t[:], in0=pBB.t[:], in1=maskneg.t[:, :].unsqueeze(1).to_broadcast([128, 8, 128]),
                                              op=ALU.add), reads=[pBB.r, maskneg.r], writes=[z1.r])
        P.op("pool", lambda e: e.tensor_tensor(out=z2.t[:], in0=z1.t[:], in1=cc.t[:, :].unsqueeze(2).to_broadcast([128, 8, 128]),
                                               op=ALU.add), reads=[z1.r, cc.r], writes=[z2.r])
        P.op("act", lambda e: e.activation(out=E.t[:], in_=z2.t[:], func=AF.Exp), reads=[z2.r], writes=[E.r])

        def qk(e):
            for m in range(8):
                for c in range(8):
                    i = e.matmul(pQK.t[:, m, :], lhsT=w_in.t[:, c, m * 128:(m + 1) * 128], rhs=xnT.t[:, c, :],
                                 start=(c == 0), stop=(c == 7))
            return i
        P.op("pe", qk, reads=[w_in.r, xnT.r], writes=[pQK.r])
        P.op("act", lambda e: e.copy(out=qT.t[:], in_=pQK.t[:, 0:4, :]), reads=[pQK.r], writes=[qT.r])
        for hf in range(2):
            lo, hi = hf * 64, hf * 64 + 64
            P.op("act", lambda e, lo=lo, hi=hi, hf=hf: e.mul(out=kT.t[lo:hi, hf:8:2, :], in_=pQK.t[lo:hi, 4:8, :], mul=0.125),
                 reads=[pQK.r], writes=[kT.r])
            P.op("dve", lambda e, lo=lo, hi=hi, hf=hf: e.tensor_tensor(out=QsT.t[lo:hi, hf:8:2, :], in0=pQK.t[lo:hi, 0:4, :],
                                                                   in1=EBp.t[lo:hi, :, :], op=ALU.mult),
                 reads=[pQK.r, EBp.r], writes=[QsT.r])
        P.op("pe", proj_tok(512, 512, pPJ[0].t[:], None), reads=[xnT.r, w_in.r], writes=[pPJ[0].r])
        P.op("act", lambda e: e.mul(out=ktok.t[:], in_=pPJ[0].t[:], mul=0.125), reads=[pPJ[0].r], writes=[ktok.r])
        for ee in range(2):
            P.op("dve", lambda e, ee=ee: e.tensor_tensor(
                out=Kw.t[:, :, ee, ee, :],
                in0=ktok.t[:, :].rearrange("p (j e k) -> p j e k", j=4, e=2)[:, :, ee, :],
                in1=wgt.t[:, :].rearrange("p (j e) -> p j e", e=2)[:, :, ee:ee + 1].to_broadcast([128, 4, 64]),
                op=ALU.mult), reads=[ktok.r, wgt.r], writes=[Kw.r])
        P.op("pe", proj_tok(1024, 512, pPJ[1].t[:], None), reads=[xnT.r, w_in.r], writes=[pPJ[1].r])
        P.op("act", lambda e: e.copy(out=v_sb.t[:, 0:4, 0:128], in_=pPJ[1].t[:, :].rearrange("p (h d) -> p h d", h=4)),
             reads=[pPJ[1].r], writes=[v_sb.r])
        P.op("pe", proj_tok(1536, 512, pPJ[0].t[:], None), reads=[xnT.r, w_in.r], writes=[pPJ[0].r])
        P.op("act", lambda e: e.copy(out=v_sb.t[:, 4:8, 0:128], in_=pPJ[0].t[:, :].rearrange("p (h d) -> p h d", h=4)),
             reads=[pPJ[0].r], writes=[v_sb.r])
        P.op("pe", proj_tok(2048, 512, pPJ[1].t[:], None), reads=[xnT.r, w_in.r], writes=[pPJ[1].r])
        P.op("act", lambda e: e.activation(out=sg.t[:, 0:512], in_=pPJ[1].t[:], func=AF.Sigmoid),
             reads=[pPJ[1].r], writes=[sg.r])
        P.op("pe", proj_tok(2560, 512, pPJ[0].t[:], None), reads=[xnT.r, w_in.r], writes=[pPJ[0].r])
        P.op("act", lambda e: e.activation(out=sg.t[:, 512:1024], in_=pPJ[0].t[:], func=AF.Sigmoid),
             reads=[pPJ[0].r], writes=[sg.r])
        P.op("pool", lambda e: e.tensor_tensor(out=ghsg.t[:], in0=sg.t[:], in1=ghbc.t[:], op=ALU.mult),
             reads=[sg.r, ghbc.r], writes=[ghsg.r])

        def st(e):
            for h in range(8):
                j, ee = h // 2, h % 2
                lo, hi = ee * 64, ee * 64 + 64
                i = e.matmul(pQK.t[:, h, :], lhsT=kT.t[:, h, :], rhs=qT.t[:, j, :], start=True, stop=True)
            return i
        P.op("pe", st, reads=[kT.r, qT.r], writes=[pQK.r])
        P.op("dve", lambda e: e.tensor_tensor(out=PT.t[:], in0=pQK.t[:], in1=E.t[:], op=ALU.mult),
             reads=[pQK.r, E.r], writes=[PT.r])

        def numden(e):
            for h in range(8):
                j, ee = h // 2, h % 2
                lo, hi = ee * 64, ee * 64 + 64
                e.matmul(pBB.t[:, h, :], lhsT=PT.t[:, h, :], rhs=v_sb.t[:, h, 0:128], start=True, stop=False)
                e.matmul(pBB.t[:, h, :], lhsT=QsT.t[:, h, :], rhs=Cb.t[:, j, 0:128], start=False, stop=True)
            for h in range(8):
                j, ee = h // 2, h % 2
                lo, hi = ee * 64, ee * 64 + 64
                e.matmul(pG.t[:, 32 + h:33 + h], lhsT=PT.t[:, h, :], rhs=v_sb.t[:, h, 128:129], start=True, stop=False)
                i = e.matmul(pG.t[:, 32 + h:33 + h], lhsT=QsT.t[:, h, :], rhs=Cb.t[:, j, 128:129], start=False, stop=True)
            return i
        P.op("pe", numden, reads=[PT.r, v_sb.r, QsT.r, Cb.r], writes=[pBB.r, rG["den"]])

        def dstate(e):
            for j in range(4):
                e.matmul(pPJ[1].t[:, j * 128:(j + 1) * 128], lhsT=Kw.t[:, j, 0, :, :], rhs=v_sb.t[:, 2 * j, 0:128],
                         start=True, stop=False)
                e.matmul(pPJ[1].t[:, j * 128:(j + 1) * 128], lhsT=Kw.t[:, j, 1, :, :], rhs=v_sb.t[:, 2 * j + 1, 0:128],
                         start=False, stop=True)
            for j in range(4):
                e.matmul(pG.t[:, 40 + j:41 + j], lhsT=Kw.t[:, j, 0, :, :], rhs=v_sb.t[:, 2 * j, 128:129],
                         start=True, stop=False)
                i = e.matmul(pG.t[:, 40 + j:41 + j], lhsT=Kw.t[:, j, 1, :, :], rhs=v_sb.t[:, 2 * j + 1, 128:129],
                             start=False, stop=True)
            return i
        P.op("pe", dstate, reads=[Kw.r, v_sb.r], writes=[pPJ[1].r, rG["dn"]])
        P.op("pool", lambda e: e.tensor_tensor(out=Ct.t[:], in0=Cs.t[:], in1=dec.t[:, :].unsqueeze(2).to_broadcast([128, 4, 128]),
                                               op=ALU.mult), reads=[Cs.r, dec.r], writes=[Ct.r])
        P.op("dve", lambda e: e.tensor_tensor(out=Cs.t[:], in0=Ct.t[:], in1=pPJ[1].t[:, :].rearrange("p (j d) -> p j d", j=4),
                                              op=ALU.add), reads=[Ct.r, pPJ[1].r], writes=[Cs.r])
        P.op("dve", lambda e: e.tensor_tensor(out=ns.t[:], in0=ns.t[:], in1=dec.t[:], op=ALU.mult),
             reads=[ns.r, dec.r], writes=[ns.r])
        P.op("dve", lambda e: e.tensor_tensor(out=ns.t[:], in0=ns.t[:], in1=pG.t[:, 40:44], op=ALU.add),
             reads=[ns.r, rG["dn"]], writes=[ns.r])
        P.op("act", lambda e: e.copy(out=Cb.t[:, :, 0:128], in_=Cs.t[:]), reads=[Cs.r], writes=[Cb.r])
        P.op("act", lambda e: e.copy(out=Cb.t[:, :, 128:129], in_=ns.t[:, :].unsqueeze(2)), reads=[ns.r], writes=[Cb.r])

        P.op("act", lambda e: e.activation(out=rr.t[:], in_=pG.t[:, 32:40], func=AF.Abs), reads=[rG["den"]], writes=[rr.r])
        P.op("dve", lambda e: e.tensor_scalar(out=rr.t[:], in0=rr.t[:], scalar1=1.0, scalar2=None, op0=ALU.max),
             reads=[rr.r], writes=[rr.r])
        P.op("dve", lambda e: e.reciprocal(out=rr.t[:], in_=rr.t[:]), reads=[rr.r], writes=[rr.r])
        P.op("act", lambda e: e.activation(out=hh.t[:], in_=pBB.t[:], func=AF.Square), reads=[pBB.r], writes=[hh.r])
        P.op("dve", lambda e: e.tensor_reduce(out=ssq.t[:], in_=hh.t[:], axis=AX.X, op=ALU.add), reads=[hh.r], writes=[ssq.r])
        P.op("dve", lambda e: e.tensor_tensor(out=ssq.t[:], in0=ssq.t[:], in1=rr.t[:], op=ALU.mult), reads=[ssq.r, rr.r], writes=[ssq.r])
        P.op("dve", lambda e: e.tensor_tensor(out=ssq.t[:], in0=ssq.t[:], in1=rr.t[:], op=ALU.mult), reads=[ssq.r, rr.r], writes=[ssq.r])
        P.op("act", lambda e: e.activation(out=ssq.t[:], in_=ssq.t[:], func=AF.Sqrt, scale=1.0 / 128.0, bias=C["epsb"].t[:]),
             reads=[ssq.r, C["epsb"].r], writes=[ssq.r])
        P.op("dve", lambda e: e.reciprocal(out=ssq.t[:], in_=ssq.t[:]), reads=[ssq.r], writes=[ssq.r])
        P.op("dve", lambda e: e.tensor_tensor(out=sc.t[:], in0=ssq.t[:], in1=rr.t[:], op=ALU.mult), reads=[ssq.r, rr.r], writes=[sc.r])
        P.op("dve", lambda e: e.tensor_tensor(out=hh.t[:], in0=pBB.t[:], in1=sc.t[:, :].unsqueeze(2).to_broadcast([128, 8, 128]),
                                              op=ALU.mult), reads=[pBB.r, sc.r], writes=[hh.r])
        P.op("dve", lambda e: e.tensor_tensor(out=hg.t[:], in0=hh.t[:, :, :].rearrange("p h d -> p (h d)"), in1=ghsg.t[:], op=ALU.mult),
             reads=[hh.r, ghsg.r], writes=[hg.r])

        def tr2(e):
            for c in range(8):
                i = e.transpose(out=pT.t[:, c, :], in_=hg.t[:, c * 128:(c + 1) * 128], identity=C["ident"].t[:])
            return i
        P.op("pe", tr2, reads=[hg.r, C["ident"].r], writes=[pT.r])
        P.op("act", lambda e: e.copy(out=hgT.t[:], in_=pT.t[:]), reads=[pT.r], writes=[hgT.r])
        XO = xo[b]
        for n in range(2):
            def op_(e, n=n):
                for c in range(8):
                    i = e.matmul(pPJ[n].t[:], lhsT=hgT.t[:, c, :], rhs=w_out.t[:, c, n * 512:(n + 1) * 512],
                                 start=(c == 0), stop=(c == 7))
                return i
            P.op("pe", op_, reads=[hgT.r, w_out.r], writes=[pPJ[n].r])
            P.op("dve", lambda e, n=n, XO=XO, X=X: e.tensor_tensor(out=XO.t[:, n * 512:(n + 1) * 512], in0=X.t[:, n * 512:(n + 1) * 512],
                                                                in1=pPJ[n].t[:], op=ALU.add),
                 reads=[X.r, pPJ[n].r], writes=[XO.r])
        P.dma("sp", x1[t * 128:(t + 1) * 128, :], XO.t[:], reads=[XO.r])


D = 1024
S = 4096


def ffn_phase(P, C, io, E, F, NTOK=1024, nsg=None, tag="f"):
    x_in, x_out = io["x_in"], io["x_out"]
    wg_d, wu_d, wd_d = io["wg"], io["wu"], io["wd"]
    moe = E > 1
    NFC = F // 128
    NTT = NTOK // 128
    NTG = NTOK // 512
    NSG = S // NTOK if nsg is None else nsg
    ident, identf = C["ident"], C["identf"]

    gbc = Buf(P, tag + "gbc", [128, D], F32)
    P.dma("sp", gbc.t[:], io["norm_g"].partition_broadcast(128), writes=[gbc.r])
    xt = [Buf(P, f"{tag}xt{i}", [128, D], F32) for i in range(2)]
    junk = Buf(P, tag + "junk", [128, D], BF16)
    ss = Buf(P, tag + "ss", [128, 1], F32)
    xn = Buf(P, tag + "xn", [128, D], BF16)
    xnT = Buf(P, tag + "xnT", [128, 8, NTOK], BF16)
    hT = Buf(P, tag + "hT", [128, NFC, NTOK], BF16)
    wgs = [Buf(P, f"{tag}wg{i}", [128, 8, 128], BF16) for i in range(3)]
    wus = [Buf(P, f"{tag}wu{i}", [128, 8, 128], BF16) for i in range(3)]
    wds = [Buf(P, f"{tag}wd{i}", [128, NFC, 512], BF16) for i in range(2)]
    sgt = [Buf(P, f"{tag}sg{i}", [128, 512], F32) for i in range(2)]
    acc = Buf(P, tag + "acc", [128, NTT, D], F32)
    racc = [P.res(f"acc{i}") for i in range(NTT)]
    pT = Buf(P, tag + "pT", [128, 8, 128], BF16, psum=True)
    pF = Buf(P, tag + "pF", [128, 4, 128], F32, psum=True)
    pA = [Buf(P, f"{tag}pA{i}", [128, 512], F32, psum=True) for i in range(2)]
    pB = [Buf(P, f"{tag}pB{i}", [128, 512], F32, psum=True) for i in range(2)]
    pY = [Buf(P, f"{tag}pY{i}", [128, 512], F32, psum=True) for i in range(2)]
    if moe:
        xnf = Buf(P, tag + "xnf", [128, D], F32)
        xnTf = Buf(P, tag + "xnTf", [128, 8, 128], F32)
        wr = Buf(P, tag + "wr", [128, 8, 8], F32)
        P.dma("sp", wr.t[:], io["router"].rearrange("(c p) e -> p c e", p=128), writes=[wr.r])
        lg = Buf(P, tag + "lg", [128, 8], F32)
        lg2 = Buf(P, tag + "lg2", [128, 8], F32)
        eq1 = Buf(P, tag + "eq1", [128, 8], F32)
        eq2 = Buf(P, tag + "eq2", [128, 8], F32)
        m1 = Buf(P, tag + "m1", [128, 1], F32)
        m2 = Buf(P, tag + "m2", [128, 1], F32)
        w1 = Buf(P, tag + "w1", [128, 1], F32)
        w2 = Buf(P, tag + "w2", [128, 1], F32)
        cw = Buf(P, tag + "cw", [128, NTT, 8], F32)
        rcw = [P.res(f"cw{i}") for i in range(NTT)]

    nwk = 0
    nwd = 0
    nev = 0
    nx = 0
    for sgi in range(NSG):
        t0 = sgi * NTOK
        for tt in range(NTT):
            X = xt[nx % 2]
            nx += 1
            r0 = t0 + tt * 128
            P.dma("sp", X.t[:], x_in[r0:r0 + 128, :], writes=[X.r])
            P.op("act", lambda e, X=X: e.activation(out=junk.t[:], in_=X.t[:], func=AF.Square, accum_out=ss.t[:]),
                 reads=[X.r], writes=[junk.r, ss.r])
            P.op("act", lambda e: e.activation(out=ss.t[:], in_=ss.t[:], func=AF.Sqrt, scale=1.0 / D, bias=C["epsb"].t[:]),
                 reads=[ss.r, C["epsb"].r], writes=[ss.r])
            P.op("dve", lambda e: e.reciprocal(out=ss.t[:], in_=ss.t[:]), reads=[ss.r], writes=[ss.r])
            if moe:
                P.op("dve", lambda e, X=X: e.scalar_tensor_tensor(out=xnf.t[:], in0=X.t[:], scalar=ss.t[:, 0:1], in1=gbc.t[:],
                                                                  op0=ALU.mult, op1=ALU.mult),
                     reads=[X.r, ss.r, gbc.r], writes=[xnf.r])
                P.op("pool", lambda e: e.tensor_copy(out=xn.t[:], in_=xnf.t[:]), reads=[xnf.r], writes=[xn.r])
            else:
                P.op("dve", lambda e, X=X: e.scalar_tensor_tensor(out=xn.t[:], in0=X.t[:], scalar=ss.t[:, 0:1], in1=gbc.t[:],
                                                                  op0=ALU.mult, op1=ALU.mult),
                     reads=[X.r, ss.r, gbc.r], writes=[xn.r])
            P.op("pool", lambda e, X=X, tt=tt: e.tensor_copy(out=acc.t[:, tt, :], in_=X.t[:]), reads=[X.r], writes=[racc[tt]])

            def tr(e):
                for c in range(8):
                    i = e.transpose(out=pT.t[:, c, :], in_=xn.t[:, c * 128:(c + 1) * 128], identity=ident.t[:])
                return i
            P.op("pe", tr, reads=[xn.r, ident.r], writes=[pT.r])
            P.op("act", lambda e, tt=tt: e.copy(out=xnT.t[:, :, tt * 128:(tt + 1) * 128], in_=pT.t[:]),
                 reads=[pT.r], writes=[xnT.r])
            if moe:
                for half in range(2):
                    def trf(e, half=half):
                        for c in range(4):
                            cc_ = half * 4 + c
                            i = e.transpose(out=pF.t[:, c, :], in_=xnf.t[:, cc_ * 128:(cc_ + 1) * 128], identity=identf.t[:])
                        return i
                    P.op("pe", trf, reads=[xnf.r, identf.r], writes=[pF.r])
                    P.op("act", lambda e, half=half: e.copy(out=xnTf.t[:, half * 4:half * 4 + 4, :], in_=pF.t[:]),
                         reads=[pF.r], writes=[xnTf.r])

                def rt(e):
                    for c in range(8):
                        i = e.matmul(pF.t[:, 0, 0:8], lhsT=xnTf.t[:, c, :], rhs=wr.t[:, c, :], start=(c == 0), stop=(c == 7))
                    return i
                P.op("pe", rt, reads=[xnTf.r, wr.r], writes=[pF.r])
                P.op("dve", lambda e: e.tensor_copy(out=lg.t[:], in_=pF.t[:, 0, 0:8]), reads=[pF.r], writes=[lg.r])
                P.op("dve", lambda e: e.tensor_reduce(out=m1.t[:], in_=lg.t[:], axis=AX.X, op=ALU.max), reads=[lg.r], writes=[m1.r])
                P.op("dve", lambda e: e.tensor_scalar(out=eq1.t[:], in0=lg.t[:], scalar1=m1.t[:, 0:1], scalar2=None, op0=ALU.is_equal),
                     reads=[lg.r, m1.r], writes=[eq1.r])
                P.op("dve", lambda e: e.scalar_tensor_tensor(out=lg2.t[:], in0=eq1.t[:], scalar=-1e30, in1=lg.t[:],
                                                             op0=ALU.mult, op1=ALU.add), reads=[eq1.r, lg.r], writes=[lg2.r])
                P.op("dve", lambda e: e.tensor_reduce(out=m2.t[:], in_=lg2.t[:], axis=AX.X, op=ALU.max), reads=[lg2.r], writes=[m2.r])
                P.op("dve", lambda e: e.tensor_scalar(out=eq2.t[:], in0=lg2.t[:], scalar1=m2.t[:, 0:1], scalar2=None, op0=ALU.is_equal),
                     reads=[lg2.r, m2.r], writes=[eq2.r])
                P.op("dve", lambda e: e.tensor_tensor(out=w2.t[:], in0=m2.t[:], in1=m1.t[:], op=ALU.subtract),
                     reads=[m1.r, m2.r], writes=[w2.r])
                P.op("act", lambda e: e.activation(out=w2.t[:], in_=w2.t[:], func=AF.Exp), reads=[w2.r], writes=[w2.r])
                P.op("dve", lambda e: e.tensor_scalar(out=w1.t[:], in0=w2.t[:], scalar1=1.0, scalar2=None, op0=ALU.add),
                     reads=[w2.r], writes=[w1.r])
                P.op("dve", lambda e: e.reciprocal(out=w1.t[:], in_=w1.t[:]), reads=[w1.r], writes=[w1.r])
                P.op("dve", lambda e: e.tensor_tensor(out=w2.t[:], in0=w2.t[:], in1=w1.t[:], op=ALU.mult),
                     reads=[w1.r, w2.r], writes=[w2.r])
                P.op("dve", lambda e: e.tensor_scalar(out=eq1.t[:], in0=eq1.t[:], scalar1=w1.t[:, 0:1], scalar2=None, op0=ALU.mult),
                     reads=[eq1.r, w1.r], writes=[eq1.r])
                P.op("dve", lambda e, tt=tt: e.scalar_tensor_tensor(out=cw.t[:, tt, :], in0=eq2.t[:], scalar=w2.t[:, 0:1], in1=eq1.t[:],
                                                                    op0=ALU.mult, op1=ALU.add),
                     reads=[eq2.r, w2.r, eq1.r], writes=[rcw[tt]])

        for ex in range(E):
            WD = []
            for dh in range(2):
                Wd = wds[nwd % 2]
                nwd += 1
                P.dma("pool", Wd.t[:], wd_d[ex].rearrange("(c p) n -> p c n", p=128)[:, :, dh * 512:(dh + 1) * 512], writes=[Wd.r])
                WD.append(Wd)
            for fc in range(NFC):
                Wg = wgs[nwk % 3]
                Wu = wus[nwk % 3]
                nwk += 1
                P.dma("pool", Wg.t[:], wg_d[ex].rearrange("(c p) n -> p c n", p=128)[:, :, fc * 128:(fc + 1) * 128], writes=[Wg.r])
                P.dma("pool", Wu.t[:], wu_d[ex].rearrange("(c p) n -> p c n", p=128)[:, :, fc * 128:(fc + 1) * 128], writes=[Wu.r])
                for tg in range(NTG):
                    A = pA[nev % 2]
                    B = pB[nev % 2]
                    SG = sgt[nev % 2]
                    nev += 1

                    def mmg(e, W=Wg, O=A, tg=tg):
                        for c in range(8):
                            i = e.matmul(O.t[:], lhsT=W.t[:, c, :], rhs=xnT.t[:, c, tg * 512:(tg + 1) * 512], start=(c == 0), stop=(c == 7))
                        return i
                    P.op("pe", mmg, reads=[Wg.r, xnT.r], writes=[A.r])

                    def mmu(e, W=Wu, O=B, tg=tg):
                        for c in range(8):
                            i = e.matmul(O.t[:], lhsT=W.t[:, c, :], rhs=xnT.t[:, c, tg * 512:(tg + 1) * 512], start=(c == 0), stop=(c == 7))
                        return i
                    P.op("pe", mmu, reads=[Wu.r, xnT.r], writes=[B.r])
                    P.op("act", lambda e, A=A, SG=SG: e.activation(out=SG.t[:], in_=A.t[:], func=AF.Silu), reads=[A.r], writes=[SG.r])
                    P.op("dve", lambda e, B=B, SG=SG, fc=fc, tg=tg: e.tensor_tensor(out=hT.t[:, fc, tg * 512:(tg + 1) * 512], in0=B.t[:], in1=SG.t[:],
                                                                                op=ALU.mult), reads=[B.r, SG.r], writes=[hT.r])
            for dh in range(2):
                Wd = WD[dh]
                for tt in range(NTT):
                    Y = pY[(dh * NTT + tt) % 2]

                    def mmd(e, Y=Y, Wd=Wd, tt=tt):
                        for fc in range(NFC):
                            i = e.matmul(Y.t[:], lhsT=hT.t[:, fc, tt * 128:(tt + 1) * 128], rhs=Wd.t[:, fc, :], start=(fc == 0), stop=(fc == NFC - 1))
                        return i
                    P.op("pe", mmd, reads=[hT.r, Wd.r], writes=[Y.r])
                    if moe:
                        P.op("dve", lambda e, Y=Y, tt=tt, dh=dh, ex=ex: e.scalar_tensor_tensor(
                            out=acc.t[:, tt, dh * 512:(dh + 1) * 512], in0=Y.t[:], scalar=cw.t[:, tt, ex:ex + 1],
                            in1=acc.t[:, tt, dh * 512:(dh + 1) * 512], op0=ALU.mult, op1=ALU.add),
                            reads=[Y.r, rcw[tt], racc[tt]], writes=[racc[tt]])
                    else:
                        P.op("dve", lambda e, Y=Y, tt=tt, dh=dh: e.tensor_tensor(
                            out=acc.t[:, tt, dh * 512:(dh + 1) * 512], in0=Y.t[:], in1=acc.t[:, tt, dh * 512:(dh + 1) * 512], op=ALU.add),
                            reads=[Y.r, racc[tt]], writes=[racc[tt]])
        for tt in range(NTT):
            r0 = t0 + tt * 128
            P.dma("sp", x_out[r0:r0 + 128, :], acc.t[:, tt, :], reads=[racc[tt]])


def ffn_dense(P, C, io, F, NTOK=1024, tag="f"):
    x_in, x_out = io["x_in"], io["x_out"]
    wg_d, wu_d, wd_d = io["wg"], io["wu"], io["wd"]
    NFC = F // 128
    NTT = NTOK // 128
    NTG = NTOK // 512
    NSG = S // NTOK
    ident = C["ident"]
    gbc = Buf(P, tag + "gbc", [128, D], F32)
    P.dma("sp", gbc.t[:], io["norm_g"].partition_broadcast(128), writes=[gbc.r])
    xt = [Buf(P, f"{tag}xt{i}", [128, D], F32) for i in range(2)]
    xr = [Buf(P, f"{tag}xr{i}", [128, D], F32) for i in range(2)]
    xo = [Buf(P, f"{tag}xo{i}", [128, D], F32) for i in range(2)]
    junk = Buf(P, tag + "junk", [128, D], BF16)
    ss = Buf(P, tag + "ss", [128, 1], F32)
    xn = Buf(P, tag + "xn", [128, D], BF16)
    xnTs = [Buf(P, f"{tag}xnT{i}", [128, 8, NTOK], BF16) for i in range(2)]
    hT = Buf(P, tag + "hT", [128, NFC, NTOK], BF16)
    wgs = [Buf(P, f"{tag}wg{i}", [128, 8, 128], BF16) for i in range(3)]
    wus = [Buf(P, f"{tag}wu{i}", [128, 8, 128], BF16) for i in range(3)]
    wds = [Buf(P, f"{tag}wd{i}", [128, NFC, 512], BF16) for i in range(2)]
    sgt = [Buf(P, f"{tag}sg{i}", [128, 512], F32) for i in range(2)]
    pT = Buf(P, tag + "pT", [128, 8, 128], BF16, psum=True)
    pA = [Buf(P, f"{tag}pA{i}", [128, 512], F32, psum=True) for i in range(2)]
    pB = [Buf(P, f"{tag}pB{i}", [128, 512], F32, psum=True) for i in range(2)]
    pY = [Buf(P, f"{tag}pY{i}", [128, 512], F32, psum=True) for i in range(2)]
    st = dict(nx=0, nwk=0, nev=0, ny=0)

    ss2 = [ss, Buf(P, tag + "ss1", [128, 1], F32)]
    xn2 = [xn, Buf(P, tag + "xn1", [128, D], BF16)]

    def s0a(sgi, tt):
        X = xt[(sgi * NTT + tt) % 2]
        SS = ss2[tt % 2]
        XN = xn2[tt % 2]
        r0 = sgi * NTOK + tt * 128
        P.dma("sp", X.t[:], x_in[r0:r0 + 128, :], writes=[X.r])
        P.op("act", lambda e: e.activation(out=junk.t[:], in_=X.t[:], func=AF.Square, accum_out=SS.t[:]),
             reads=[X.r], writes=[junk.r, SS.r])
        P.op("act", lambda e: e.activation(out=SS.t[:], in_=SS.t[:], func=AF.Sqrt, scale=1.0 / D, bias=C["epsb"].t[:]),
             reads=[SS.r, C["epsb"].r], writes=[SS.r])
        P.op("dve", lambda e: e.reciprocal(out=SS.t[:], in_=SS.t[:]), reads=[SS.r], writes=[SS.r])
        P.op("dve", lambda e: e.scalar_tensor_tensor(out=XN.t[:], in0=X.t[:], scalar=SS.t[:, 0:1], in1=gbc.t[:],
                                                     op0=ALU.mult, op1=ALU.mult), reads=[X.r, SS.r, gbc.r], writes=[XN.r])

    def s0b(sgi, tt):
        xnT = xnTs[sgi % 2]
        XN = xn2[tt % 2]

        def tr(e):
            for c in range(8):
                i = e.transpose(out=pT.t[:, c, :], in_=XN.t[:, c * 128:(c + 1) * 128], identity=ident.t[:])
            return i
        P.op("pe", tr, reads=[XN.r, ident.r], writes=[pT.r])
        P.op("act", lambda e: e.copy(out=xnT.t[:, :, tt * 128:(tt + 1) * 128], in_=pT.t[:]), reads=[pT.r], writes=[xnT.r])

    def stage0_steps(sgi):
        steps = []
        for k in range(NTT + 1):
            def step(k=k):
                if k < NTT:
                    s0a(sgi, k)
                if k >= 1:
                    s0b(sgi, k - 1)
            steps.append(step)
        return steps

    def stage1(sgi, side_steps=()):
        xnT = xnTs[sgi % 2]
        side = list(side_steps)
        WD = []
        for dh in range(2):
            Wd = wds[dh]
            P.dma("pool", Wd.t[:], wd_d[0].rearrange("(c p) n -> p c n", p=128)[:, :, dh * 512:(dh + 1) * 512], writes=[Wd.r])
            WD.append(Wd)
        for fc in range(NFC):
            Wg = wgs[st["nwk"] % 3]
            Wu = wus[st["nwk"] % 3]
            st["nwk"] += 1
            P.dma("pool", Wg.t[:], wg_d[0].rearrange("(c p) n -> p c n", p=128)[:, :, fc * 128:(fc + 1) * 128], writes=[Wg.r])
            P.dma("pool", Wu.t[:], wu_d[0].rearrange("(c p) n -> p c n", p=128)[:, :, fc * 128:(fc + 1) * 128], writes=[Wu.r])
            for tg in range(NTG):
                A = pA[st["nev"] % 2]
                B = pB[st["nev"] % 2]
                SG = sgt[st["nev"] % 2]
                st["nev"] += 1

                def mmg(e, W=Wg, O=A, tg=tg):
                    for c in range(8):
                        i = e.matmul(O.t[:], lhsT=W.t[:, c, :], rhs=xnT.t[:, c, tg * 512:(tg + 1) * 512], start=(c == 0), stop=(c == 7))
                    return i
                P.op("pe", mmg, reads=[Wg.r, xnT.r], writes=[A.r])

                def mmu(e, W=Wu, O=B, tg=tg):
                    for c in range(8):
                        i = e.matmul(O.t[:], lhsT=W.t[:, c, :], rhs=xnT.t[:, c, tg * 512:(tg + 1) * 512], start=(c == 0), stop=(c == 7))
                    return i
                P.op("pe", mmu, reads=[Wu.r, xnT.r], writes=[B.r])
                P.op("act", lambda e, A=A, SG=SG: e.activation(out=SG.t[:], in_=A.t[:], func=AF.Silu), reads=[A.r], writes=[SG.r])
                P.op("dve", lambda e, B=B, SG=SG, fc=fc, tg=tg: e.tensor_tensor(out=hT.t[:, fc, tg * 512:(tg + 1) * 512], in0=B.t[:], in1=SG.t[:],
                                                                            op=ALU.mult), reads=[B.r, SG.r], writes=[hT.r])
            if side and fc % 2 == 1:
                side.pop(0)()
        while side:
            side.pop(0)()
        return WD

    def stage2(sgi, WD):
        for tt in range(NTT):
            r0 = sgi * NTOK + tt * 128
            XR = xr[st["ny"] % 2]
            XO = xo[st["ny"] % 2]
            st["ny"] += 1
            P.dma("sp", XR.t[:], x_in[r0:r0 + 128, :], writes=[XR.r])
            for dh in range(2):
                Y = pY[dh]
                Wd = WD[dh]

                def mmd(e, Y=Y, Wd=Wd, tt=tt):
                    for fc in range(NFC):
                        i = e.matmul(Y.t[:], lhsT=hT.t[:, fc, tt * 128:(tt + 1) * 128], rhs=Wd.t[:, fc, :], start=(fc == 0), stop=(fc == NFC - 1))
                    return i
                P.op("pe", mmd, reads=[hT.r, Wd.r], writes=[Y.r])
                P.op("dve", lambda e, Y=Y, dh=dh, XR=XR, XO=XO: e.tensor_tensor(out=XO.t[:, dh * 512:(dh + 1) * 512], in0=Y.t[:],
                                                                            in1=XR.t[:, dh * 512:(dh + 1) * 512], op=ALU.add),
                     reads=[Y.r, XR.r], writes=[XO.r])
            P.dma("act", x_out[r0:r0 + 128, :], XO.t[:], reads=[XO.r])

    for st_ in stage0_steps(0):
        st_()
    for sgi in range(NSG):
        WD = stage1(sgi, stage0_steps(sgi + 1) if sgi + 1 < NSG else ())
        stage2(sgi, WD)


D = 1024
S = 4096
NT = S // 128
NQG = S // 512


def phase3(P, C, io, npairs=8, nqg=NQG):
    x_in, x_out, oT_d = io["x_in"], io["x_out"], io["oT"]
    ident = C["ident"]
    trineg = Buf(P, "trineg", [128, 128], BF16)
    negones = Buf(P, "negones", [128, 128], BF16)
    blk1 = Buf(P, "blk1", [128, 128], BF16)
    tmpf = Buf(P, "tmpf", [128, 128], F32)
    mask01 = Buf(P, "mask01", [128, 4, 512], BF16)
    tmpm = Buf(P, "tmpm", [128, 512], F32)
    P.op("pool", lambda e: e.memset(tmpf.t[:], -1.0), writes=[tmpf.r])
    P.op("dve", lambda e: e.tensor_copy(out=negones.t[:], in_=tmpf.t[:]), reads=[tmpf.r], writes=[negones.r])
    P.op("pool", lambda e: e.affine_select(out=tmpf.t[:], in_=tmpf.t[:], pattern=[[-1, 128]], compare_op=ALU.is_ge, fill=0.0,
                                           base=0, channel_multiplier=1), reads=[tmpf.r, negones.r], writes=[tmpf.r])
    P.op("dve", lambda e: e.tensor_copy(out=trineg.t[:], in_=tmpf.t[:]), reads=[tmpf.r], writes=[trineg.r])
    P.op("pool", lambda e: e.memset(blk1.t[:], 0.0), writes=[blk1.r])
    P.op("pool", lambda e: e.memset(blk1.t[0:64, 0:64], 1.0), writes=[blk1.r])
    P.op("pool", lambda e: e.memset(blk1.t[64:128, 64:128], 1.0), writes=[blk1.r])
    for i in range(4):
        P.op("pool", lambda e: e.memset(tmpm.t[:], 1.0), reads=[mask01.r], writes=[tmpm.r])
        P.op("pool", lambda e, i=i: e.affine_select(out=tmpm.t[:], in_=tmpm.t[:], pattern=[[1, 512]], compare_op=ALU.is_gt, fill=0.0,
                                                    base=-128 * i, channel_multiplier=-1), reads=[tmpm.r], writes=[tmpm.r])
        P.op("dve", lambda e, i=i: e.tensor_copy(out=mask01.t[:, i, :], in_=tmpm.t[:]), reads=[tmpm.r], writes=[mask01.r])
    gq_in = Buf(P, "gq_in", [128, 8], F32)
    gkv_in = Buf(P, "gkv_in", [128, 8], F32)
    gq_col = Buf(P, "gq_col", [128, 1], F32)
    gk_col = Buf(P, "gk_col", [128, 1], F32)
    P.dma("sp", gq_in.t[:], io["mix_norm1"].rearrange("(c p) -> p c", p=128), writes=[gq_in.r], allow_slow_non_contiguous=True)
    P.dma("sp", gkv_in.t[:], io["kv_norm"].rearrange("(c p) -> p c", p=128), writes=[gkv_in.r], allow_slow_non_contiguous=True)
    for hf in range(2):
        P.dma("sp", gq_col.t[hf * 64:hf * 64 + 64, :], io["g_q"].rearrange("(p o) -> p o", o=1), writes=[gq_col.r])
        P.dma("sp", gk_col.t[hf * 64:hf * 64 + 64, :], io["g_k"].rearrange("(p o) -> p o", o=1), writes=[gk_col.r])
    P.op("dve", lambda e: e.tensor_scalar(out=gk_col.t[:], in0=gk_col.t[:], scalar1=0.125, scalar2=None, op0=ALU.mult),
         reads=[gk_col.r], writes=[gk_col.r])

    xhatT = Buf(P, "xhatT", [128, 8, S], BF16)
    xt = [Buf(P, f"axt{i}", [128, D], F32) for i in range(2)]
    junk = Buf(P, "ajunk", [128, D], BF16)
    ss = Buf(P, "ass", [128, 1], F32)
    xh = Buf(P, "axh", [128, D], BF16)
    qTz = Buf(P, "qTz", [128, 2, S], BF16)
    kT = Buf(P, "kTa", [128, S], BF16)
    Vz = Buf(P, "Vz", [128, NT, 2, 128], BF16)
    wraw = [Buf(P, f"wraw{i}", [128, 8, 128], BF16) for i in range(3)]
    wf = [Buf(P, f"wf{i}", [128, 8, 128], BF16) for i in range(3)]
    sq = Buf(P, "asq", [128, 512], BF16)
    rs = Buf(P, "ars", [128, 512], F32)
    ebuf = [Buf(P, f"e{i}", [128, 512], F32) for i in range(2)]
    lpb = [Buf(P, f"lp{i}", [128, 512], BF16) for i in range(2)]
    ttb = [Buf(P, f"tt{i}", [128, 512], F32) for i in range(2)]
    Ab = [Buf(P, f"A{i}", [128, 512], BF16) for i in range(4)]
    Rb = Buf(P, "Rb", [128, 512], F32)
    oTt = [Buf(P, f"oTt{i}", [128, 512], BF16) for i in range(2)]
    pS = [Buf(P, f"pS{i}", [128, 512], F32, psum=True) for i in range(2)]
    pG = [Buf(P, f"pGa{i}", [128, 512], F32, psum=True) for i in range(2)]
    pR = [Buf(P, f"pR{i}", [128, 512], F32, psum=True) for i in range(2)]
    pO = [Buf(P, f"pO{i}", [128, 512], F32, psum=True) for i in range(2)]

    P.op("pool", lambda e: e.memset(qTz.t[:], 0.0), writes=[qTz.r])
    P.op("pool", lambda e: e.memset(Vz.t[:], 0.0), writes=[Vz.r])

    ss2 = [ss, Buf(P, "ass1", [128, 1], F32)]
    xh2 = [xh, Buf(P, "axh1", [128, D], BF16)]

    def s3a_a(t):
        X = xt[t % 2]
        SS = ss2[t % 2]
        XH = xh2[t % 2]
        P.dma("sp", X.t[:], x_in[t * 128:(t + 1) * 128, :], writes=[X.r])
        P.op("act", lambda e: e.activation(out=junk.t[:], in_=X.t[:], func=AF.Square, accum_out=SS.t[:]),
             reads=[X.r], writes=[junk.r, SS.r])
        P.op("act", lambda e: e.activation(out=SS.t[:], in_=SS.t[:], func=AF.Sqrt, scale=1.0 / D, bias=C["epsb"].t[:]),
             reads=[SS.r, C["epsb"].r], writes=[SS.r])
        P.op("dve", lambda e: e.reciprocal(out=SS.t[:], in_=SS.t[:]), reads=[SS.r], writes=[SS.r])
        P.op("dve", lambda e: e.tensor_scalar(out=XH.t[:], in0=X.t[:], scalar1=SS.t[:, 0:1], scalar2=None, op0=ALU.mult),
             reads=[X.r, SS.r], writes=[XH.r])

    def s3a_b(t):
        XH = xh2[t % 2]
        pTv = pS[t % 2]

        def tr(e):
            v = pTv.t[:, :].bitcast(BF16)
            for c in range(8):
                i = e.transpose(out=v[:, c * 128:(c + 1) * 128], in_=XH.t[:, c * 128:(c + 1) * 128], identity=ident.t[:])
            return i
        P.op("pe", tr, reads=[XH.r, ident.r], writes=[pTv.r])
        P.op("act", lambda e: e.copy(out=xhatT.t[:, :, t * 128:(t + 1) * 128],
                                     in_=pTv.t[:, :].bitcast(BF16).rearrange("p (c n) -> p c n", c=8)),
             reads=[pTv.r], writes=[xhatT.r])

    n3a = nqg * 4
    s3a_a(0)
    for t in range(n3a):
        if t + 1 < n3a:
            s3a_a(t + 1)
        s3a_b(t)

    for j in range(npairs):
        srcs = [io["w_q"][:, j * 128:(j + 1) * 128], io["w_kv"][:, j * 128:(j + 1) * 128],
                io["w_kv"][:, 1024 + j * 128:1024 + (j + 1) * 128]]
        gains = [gq_in, gkv_in, gkv_in]
        for i in range(3):
            P.dma("pool", wraw[i].t[:], srcs[i].rearrange("(c p) n -> p c n", p=128), writes=[wraw[i].r])
            P.op("pool", lambda e, i=i: e.tensor_tensor(out=wf[i].t[:], in0=wraw[i].t[:],
                                                       in1=gains[i].t[:, :].unsqueeze(2).to_broadcast([128, 8, 128]), op=ALU.mult),
                 reads=[wraw[i].r, gains[i].r], writes=[wf[i].r])
        for which in range(2):
            W = wf[which]
            for tg in range(nqg):
                PS = pS[tg % 2]
                PG = pG[tg % 2]

                def mm(e, W=W, PS=PS, tg=tg):
                    for c in range(8):
                        i = e.matmul(PS.t[:], lhsT=W.t[:, c, :], rhs=xhatT.t[:, c, tg * 512:(tg + 1) * 512], start=(c == 0), stop=(c == 7))
                    return i
                P.op("pe", mm, reads=[W.r, xhatT.r], writes=[PS.r])
                P.op("act", lambda e, PS=PS: e.activation(out=sq.t[:], in_=PS.t[:], func=AF.Square), reads=[PS.r], writes=[sq.r])
                P.op("pe", lambda e, PG=PG: e.matmul(PG.t[:], lhsT=blk1.t[:], rhs=sq.t[:], start=True, stop=True),
                     reads=[blk1.r, sq.r], writes=[PG.r])
                P.op("act", lambda e, PG=PG: e.activation(out=rs.t[:], in_=PG.t[:], func=AF.Sqrt, scale=1.0 / 64.0, bias=C["epsb"].t[:]),
                     reads=[PG.r, C["epsb"].r], writes=[rs.r])
                P.op("dve", lambda e: e.reciprocal(out=rs.t[:], in_=rs.t[:]), reads=[rs.r], writes=[rs.r])
                if which == 0:
                    for hf in range(2):
                        lo, hi = hf * 64, hf * 64 + 64
                        P.op("dve", lambda e, PS=PS, lo=lo, hi=hi, hf=hf, tg=tg: e.scalar_tensor_tensor(
                            out=qTz.t[lo:hi, hf, tg * 512:(tg + 1) * 512], in0=PS.t[lo:hi, :], scalar=gq_col.t[lo:hi, 0:1],
                            in1=rs.t[lo:hi, :], op0=ALU.mult, op1=ALU.mult), reads=[PS.r, gq_col.r, rs.r], writes=[qTz.r])
                else:
                    P.op("dve", lambda e, PS=PS, tg=tg: e.scalar_tensor_tensor(
                        out=kT.t[:, tg * 512:(tg + 1) * 512], in0=PS.t[:], scalar=gk_col.t[:, 0:1],
                        in1=rs.t[:], op0=ALU.mult, op1=ALU.mult), reads=[PS.r, gk_col.r, rs.r], writes=[kT.r])
        for b4 in range(nqg):
            PS = pS[b4 % 2]

            def mmv(e, PS=PS, b4=b4):
                for i4 in range(4):
                    tb = b4 * 4 + i4
                    for c in range(8):
                        i = e.matmul(PS.t[:, i4 * 128:(i4 + 1) * 128], lhsT=xhatT.t[:, c, tb * 128:(tb + 1) * 128], rhs=wf[2].t[:, c, :],
                                     start=(c == 0), stop=(c == 7))
                return i
            P.op("pe", mmv, reads=[xhatT.r, wf[2].r], writes=[PS.r])
            for hf in range(2):
                P.op("act", lambda e, PS=PS, b4=b4, hf=hf: e.copy(
                    out=Vz.t[:, b4 * 4:(b4 + 1) * 4, hf, hf * 64:hf * 64 + 64],
                    in_=PS.t[:, :].rearrange("p (i n) -> p i n", i=4)[:, :, hf * 64:hf * 64 + 64]), reads=[PS.r], writes=[Vz.r])

        its = []
        for qg in range(nqg):
            first = True
            for ee in range(2):
                kbs = list(range(4 * qg + 3, -1, -1))
                for n_, kb in enumerate(kbs):
                    its.append(dict(qg=qg, ee=ee, kb=kb, firstkb=(n_ == 0), diag=(kb >= 4 * qg), di=kb - 4 * qg,
                                    ostart=first, ostop=(ee == 1 and n_ == len(kbs) - 1)))
                    first = False
        n_it = len(its)

        def stageA(n):
            it = its[n]
            b = n % 2
            c0 = 128 * it["di"] if it["diag"] else 0
            q_ap = qTz.t[:, it["ee"], it["qg"] * 512 + c0:(it["qg"] + 1) * 512]
            k_ap = kT.t[:, it["kb"] * 128:(it["kb"] + 1) * 128]
            P.op("pe", lambda e: e.matmul(pS[b].t[:, c0:], lhsT=k_ap, rhs=q_ap, start=True, stop=True), reads=[kT.r, qTz.r], writes=[pS[b].r])
            P.op("act", lambda e: e.activation(out=ebuf[b].t[:, c0:], in_=pS[b].t[:, c0:], func=AF.Exp), reads=[pS[b].r], writes=[ebuf[b].r])
            P.op("act", lambda e: e.activation(out=lpb[b].t[:, c0:], in_=ebuf[b].t[:, c0:], func=AF.Ln, bias=C["one1"].t[:]),
                 reads=[ebuf[b].r, C["one1"].r], writes=[lpb[b].r])
            if it["diag"]:
                P.op("pool", lambda e: e.tensor_tensor(out=lpb[b].t[:, c0:], in0=lpb[b].t[:, c0:], in1=mask01.t[:, it["di"], c0:], op=ALU.mult),
                     reads=[lpb[b].r, mask01.r], writes=[lpb[b].r])

        def stageB(n):
            it = its[n]
            b = n % 2
            c0 = 128 * it["di"] if it["diag"] else 0
            q_ap = qTz.t[:, it["ee"], it["qg"] * 512 + c0:(it["qg"] + 1) * 512]
            k_ap = kT.t[:, it["kb"] * 128:(it["kb"] + 1) * 128]

            def g(e):
                e.matmul(pG[b].t[:, c0:], lhsT=trineg.t[:], rhs=lpb[b].t[:, c0:], start=True, stop=False)
                return e.matmul(pG[b].t[:, c0:], lhsT=k_ap, rhs=q_ap, start=False, stop=True)
            P.op("pe", g, reads=[trineg.r, lpb[b].r, kT.r, qTz.r], writes=[pG[b].r])
            P.op("pe", lambda e: e.matmul(pR[b].t[:, c0:], lhsT=negones.t[:], rhs=lpb[b].t[:, c0:], start=True, stop=True),
                 reads=[negones.r, lpb[b].r], writes=[pR[b].r])
            if it["firstkb"]:
                P.op("dve", lambda e: e.tensor_copy(out=ttb[b].t[:, c0:], in_=pG[b].t[:, c0:]), reads=[pG[b].r], writes=[ttb[b].r])
                P.op("pool", lambda e: e.memset(Rb.t[:, 0:c0], 0.0), writes=[Rb.r])
                P.op("dve", lambda e: e.tensor_copy(out=Rb.t[:, c0:], in_=pR[b].t[:, c0:]), reads=[pR[b].r], writes=[Rb.r])
            else:
                P.op("dve", lambda e: e.tensor_tensor(out=ttb[b].t[:, c0:], in0=pG[b].t[:, c0:], in1=Rb.t[:, c0:], op=ALU.add),
                     reads=[pG[b].r, Rb.r], writes=[ttb[b].r])
                P.op("dve", lambda e: e.tensor_tensor(out=Rb.t[:, c0:], in0=pR[b].t[:, c0:], in1=Rb.t[:, c0:], op=ALU.add),
                     reads=[pR[b].r, Rb.r], writes=[Rb.r])

        def stageB2(n):
            it = its[n]
            b = n % 2
            A = Ab[n % 4]
            c0 = 128 * it["di"] if it["diag"] else 0
            P.op("act", lambda e: e.activation(out=A.t[:, c0:], in_=ttb[b].t[:, c0:], func=AF.Exp), reads=[ttb[b].r], writes=[A.r])
            if it["diag"]:
                P.op("pool", lambda e: e.tensor_tensor(out=A.t[:, c0:], in0=A.t[:, c0:], in1=mask01.t[:, it["di"], c0:], op=ALU.mult),
                     reads=[A.r, mask01.r], writes=[A.r])
                if c0 > 0:
                    P.op("pool", lambda e: e.memset(A.t[:, 0:c0], 0.0), reads=[A.r], writes=[A.r])

        def stageC(n):
            it = its[n]
            b = n % 2
            ob = it["qg"] % 2
            A = Ab[n % 4]
            P.op("pe", lambda e: e.matmul(pO[ob].t[:], lhsT=Vz.t[:, it["kb"], it["ee"], :], rhs=A.t[:],
                                          start=it["ostart"], stop=it["ostop"]),
                 reads=[Vz.r, A.r], writes=[pO[ob].r])
            if it["ostop"]:
                qg = it["qg"]
                O = oTt[qg % 2]
                P.op("dve", lambda e: e.tensor_copy(out=O.t[:], in_=pO[ob].t[:]), reads=[pO[ob].r], writes=[O.r])
                P.dma("sp", oT_d[j, :, qg * 512:(qg + 1) * 512], O.t[:], reads=[O.r], writes=[io["r_oT"]])

        for n in range(n_it + 4):
            if n < n_it:
                stageA(n)
            if 0 <= n - 1 < n_it:
                stageB(n - 1)
            if 0 <= n - 2 < n_it:
                stageB2(n - 2)
            if 0 <= n - 4 < n_it:
                stageC(n - 4)

    w_o = Buf(P, "w_o", [128, 8, D], BF16)
    P.dma("pool", w_o.t[:], io["w_o"].rearrange("(c p) n -> p c n", p=128), writes=[w_o.r])
    oin = [Buf(P, f"oin{i}", [128, 8, 128], BF16) for i in range(2)]
    xo = [Buf(P, f"axo{i}", [128, D], F32) for i in range(2)]
    for t in range(nqg * 4):
        X = xt[t % 2]
        OI = oin[t % 2]
        XO = xo[t % 2]
        P.dma("sp", X.t[:], x_in[t * 128:(t + 1) * 128, :], writes=[X.r])
        P.dma("sp", OI.t[:, 0:npairs, :], oT_d[0:npairs, :, t * 128:(t + 1) * 128].rearrange("j p n -> p j n"),
              reads=[io["r_oT"]], writes=[OI.r])
        for n in range(2):
            PS = pS[n]

            def op_(e, PS=PS, OI=OI, n=n):
                for c in range(npairs):
                    i = e.matmul(PS.t[:], lhsT=OI.t[:, c, :], rhs=w_o.t[:, c, n * 512:(n + 1) * 512], start=(c == 0), stop=(c == npairs - 1))
                return i
            P.op("pe", op_, reads=[OI.r, w_o.r], writes=[PS.r])
            P.op("dve", lambda e, PS=PS, X=X, XO=XO, n=n: e.tensor_tensor(out=XO.t[:, n * 512:(n + 1) * 512], in0=X.t[:, n * 512:(n + 1) * 512],
                                                                      in1=PS.t[:], op=ALU.add), reads=[X.r, PS.r], writes=[XO.r])
        P.dma("act", x_out[t * 128:(t + 1) * 128, :], XO.t[:], reads=[XO.r])


D = 1024
S = 4096
NJ = 23
NSLOT = NJ * 512
FQ = 896


def moe_route(P, C, io):
    x_in = io["x_in"]
    ident, identf = C["ident"], C["identf"]
    gbc = Buf(P, "rgbc", [128, D], F32)
    P.dma("sp", gbc.t[:], io["norm_g"].partition_broadcast(128), writes=[gbc.r])
    wr = Buf(P, "rwr", [128, 8, 8], F32)
    P.dma("sp", wr.t[:], io["router"].rearrange("(c p) e -> p c e", p=128), writes=[wr.r])
    xt = [Buf(P, f"rxt{i}", [128, D], F32) for i in range(2)]
    junk = Buf(P, "rjunk", [128, D], BF16)
    ss = Buf(P, "rss", [128, 1], F32)
    xnf = Buf(P, "rxnf", [128, D], F32)
    xn = [Buf(P, f"rxn{i}", [128, D], BF16) for i in range(2)]
    xnTf = Buf(P, "rxnTf", [128, 8, 128], F32)
    pF = [Buf(P, f"rpF{i}", [128, 4, 128], F32, psum=True) for i in range(2)]
    pR = Buf(P, "rpR", [128, 512], F32, psum=True)
    lg = Buf(P, "rlg", [128, 8], F32)
    lg2 = Buf(P, "rlg2", [128, 8], F32)
    m1 = Buf(P, "rm1", [128, 1], F32)
    m2 = Buf(P, "rm2", [128, 1], F32)
    EQ1 = Buf(P, "EQ1", [128, 32, 8], F32)
    EQ2 = Buf(P, "EQ2", [128, 32, 8], F32)
    W12 = Buf(P, "W12", [128, 32, 2], F32)
    POS = Buf(P, "POS", [128, 2, 32], F32)
    maskb = Buf(P, "rmaskb", [128, 8], BF16)
    pose = Buf(P, "rpose", [128, 8], F32)
    tmp8 = Buf(P, "rtmp8", [128, 8], F32)
    run = Buf(P, "rrun", [128, 8], F32)
    lts = Buf(P, "rlts", [128, 128], BF16)
    onesb = Buf(P, "ronesb", [128, 128], BF16)
    tf = Buf(P, "rtf", [128, 128], F32)
    P.op("pool", lambda e: e.memset(tf.t[:], 1.0), writes=[tf.r])
    P.op("dve", lambda e: e.tensor_copy(out=onesb.t[:], in_=tf.t[:]), reads=[tf.r], writes=[onesb.r])
    P.op("pool", lambda e: e.affine_select(out=tf.t[:], in_=tf.t[:], pattern=[[1, 128]], compare_op=ALU.is_gt, fill=0.0,
                                           base=0, channel_multiplier=-1), reads=[tf.r, onesb.r], writes=[tf.r])
    P.op("dve", lambda e: e.tensor_copy(out=lts.t[:], in_=tf.t[:]), reads=[tf.r], writes=[lts.r])
    P.op("pool", lambda e: e.memset(run.t[:], 0.0), writes=[run.r])

    xnf2 = [xnf, Buf(P, "rxnf1", [128, D], F32)]
    xnTf2 = [xnTf, Buf(P, "rxnTf1", [128, 8, 128], F32)]
    LG = Buf(P, "rLG", [128, 32, 8], F32)
    LG2 = Buf(P, "rLG2", [128, 32, 8], F32)
    M1 = Buf(P, "rM1", [128, 32], F32)
    M2 = Buf(P, "rM2", [128, 32], F32)
    MASK = Buf(P, "rMASK", [128, 32, 8], BF16)
    POSE = Buf(P, "rPOSE", [128, 32, 8], F32)
    T3 = Buf(P, "rT3", [128, 32, 8], F32)
    ss2 = [ss, Buf(P, "rss1", [128, 1], F32)]

    def r1a(tt):
        X = xt[tt % 2]
        XN = xn[tt % 2]
        XF = xnf2[tt % 2]
        SS = ss2[tt % 2]
        P.dma("sp", X.t[:], x_in[tt * 128:(tt + 1) * 128, :], writes=[X.r])
        P.op("act", lambda e: e.activation(out=junk.t[:], in_=X.t[:], func=AF.Square, accum_out=SS.t[:]),
             reads=[X.r], writes=[junk.r, SS.r])
        P.op("act", lambda e: e.activation(out=SS.t[:], in_=SS.t[:], func=AF.Sqrt, scale=1.0 / D, bias=C["epsb"].t[:]),
             reads=[SS.r, C["epsb"].r], writes=[SS.r])
        P.op("dve", lambda e: e.reciprocal(out=SS.t[:], in_=SS.t[:]), reads=[SS.r], writes=[SS.r])
        P.op("dve", lambda e: e.scalar_tensor_tensor(out=XF.t[:], in0=X.t[:], scalar=SS.t[:, 0:1], in1=gbc.t[:],
                                                     op0=ALU.mult, op1=ALU.mult), reads=[X.r, SS.r, gbc.r], writes=[XF.r])
        P.op("act", lambda e: e.copy(out=XN.t[:], in_=XF.t[:]), reads=[XF.r], writes=[XN.r])
        P.dma("act", io["xn_d"][tt * 128:(tt + 1) * 128, :], XN.t[:], reads=[XN.r])

    def r1b(tt):
        XF = xnf2[tt % 2]
        XTF = xnTf2[tt % 2]
        for half in range(2):
            PF = pF[half]

            def trf(e, half=half, PF=PF):
                for c in range(4):
                    cc_ = half * 4 + c
                    i = e.transpose(out=PF.t[:, c, :], in_=XF.t[:, cc_ * 128:(cc_ + 1) * 128], identity=identf.t[:])
                return i
            P.op("pe", trf, reads=[XF.r, identf.r], writes=[PF.r])
            P.op("act", lambda e, half=half, PF=PF: e.copy(out=XTF.t[:, half * 4:half * 4 + 4, :], in_=PF.t[:]),
                 reads=[PF.r], writes=[XTF.r])

        def rt(e):
            for c in range(8):
                i = e.matmul(pR.t[:, 0:8], lhsT=XTF.t[:, c, :], rhs=wr.t[:, c, :], start=(c == 0), stop=(c == 7))
            return i
        P.op("pe", rt, reads=[XTF.r, wr.r], writes=[pR.r])
        P.op("dve", lambda e: e.tensor_copy(out=LG.t[:, tt, :], in_=pR.t[:, 0:8]), reads=[pR.r], writes=[LG.r])

    r1a(0)
    for tt in range(32):
        if tt + 1 < 32:
            r1a(tt + 1)
        r1b(tt)
    bc = lambda ap: ap.unsqueeze(2).to_broadcast([128, 32, 8])
    P.op("dve", lambda e: e.tensor_reduce(out=M1.t[:], in_=LG.t[:], axis=AX.X, op=ALU.max), reads=[LG.r], writes=[M1.r])
    P.op("dve", lambda e: e.tensor_tensor(out=EQ1.t[:], in0=LG.t[:], in1=bc(M1.t[:, :]), op=ALU.is_equal), reads=[LG.r, M1.r], writes=[EQ1.r])
    P.op("dve", lambda e: e.scalar_tensor_tensor(out=LG2.t[:], in0=EQ1.t[:], scalar=-1e30, in1=LG.t[:], op0=ALU.mult, op1=ALU.add),
         reads=[EQ1.r, LG.r], writes=[LG2.r])
    P.op("dve", lambda e: e.tensor_reduce(out=M2.t[:], in_=LG2.t[:], axis=AX.X, op=ALU.max), reads=[LG2.r], writes=[M2.r])
    P.op("dve", lambda e: e.tensor_tensor(out=EQ2.t[:], in0=LG2.t[:], in1=bc(M2.t[:, :]), op=ALU.is_equal), reads=[LG2.r, M2.r], writes=[EQ2.r])
    W1v = W12.t[:, :, 0]
    W2v = W12.t[:, :, 1]
    P.op("dve", lambda e: e.tensor_tensor(out=M2.t[:], in0=M2.t[:], in1=M1.t[:], op=ALU.subtract), reads=[M1.r, M2.r], writes=[M2.r])
    P.op("act", lambda e: e.activation(out=M2.t[:], in_=M2.t[:], func=AF.Exp), reads=[M2.r], writes=[M2.r])
    P.op("dve", lambda e: e.tensor_scalar(out=M1.t[:], in0=M2.t[:], scalar1=1.0, scalar2=None, op0=ALU.add), reads=[M2.r], writes=[M1.r])
    P.op("dve", lambda e: e.reciprocal(out=M1.t[:], in_=M1.t[:]), reads=[M1.r], writes=[M1.r])
    P.op("dve", lambda e: e.tensor_copy(out=W1v, in_=M1.t[:]), reads=[M1.r], writes=[W12.r])
    P.op("dve", lambda e: e.tensor_tensor(out=W2v, in0=M2.t[:], in1=M1.t[:], op=ALU.mult), reads=[M1.r, M2.r], writes=[W12.r])
    P.op("dve", lambda e: e.tensor_tensor(out=MASK.t[:], in0=EQ1.t[:], in1=EQ2.t[:], op=ALU.add), reads=[EQ1.r, EQ2.r], writes=[MASK.r])
    pP = pF[0]
    pPv = pP.t[:, :, :].rearrange("p a b -> p (a b)")

    def pfx(e):
        for tt in range(32):
            e.matmul(pPv[:, tt * 8:(tt + 1) * 8], lhsT=lts.t[:], rhs=MASK.t[:, tt, :], start=True, stop=(tt == 0))
            for t2 in range(tt):
                r = e.matmul(pPv[:, tt * 8:(tt + 1) * 8], lhsT=onesb.t[:], rhs=MASK.t[:, t2, :], start=False, stop=(t2 == tt - 1))
        for t2 in range(32):
            r = e.matmul(pPv[:, 256:264], lhsT=onesb.t[:], rhs=MASK.t[:, t2, :], start=(t2 == 0), stop=(t2 == 31))
        return r
    P.op("pe", pfx, reads=[lts.r, onesb.r, MASK.r], writes=[pP.r])
    P.op("dve", lambda e: e.tensor_copy(out=POSE.t[:, :, :].rearrange("p a b -> p (a b)"), in_=pPv[:, 0:256]), reads=[pP.r], writes=[POSE.r])
    P.op("dve", lambda e: e.tensor_copy(out=run.t[:], in_=pPv[:, 256:264]), reads=[pP.r], writes=[run.r])
    for k, EQ in enumerate((EQ1, EQ2)):
        P.op("dve", lambda e, EQ=EQ: e.tensor_tensor(out=T3.t[:], in0=EQ.t[:], in1=POSE.t[:], op=ALU.mult), reads=[EQ.r, POSE.r], writes=[T3.r])
        P.op("dve", lambda e, k=k: e.tensor_reduce(out=POS.t[:, k, :], in_=T3.t[:], axis=AX.X, op=ALU.add), reads=[T3.r], writes=[POS.r])

    thr = Buf(P, "rthr", [128, 8, 8], F32)
    cmp = Buf(P, "rcmp", [128, 8, 8], F32)
    tiles = Buf(P, "rtiles", [128, 8], F32)
    ca = [Buf(P, f"rca{i}", [128, 8], F32) for i in range(3)]
    base = Buf(P, "rbase", [128, 8], F32)
    for k in range(8):
        P.op("pool", lambda e, k=k: e.memset(thr.t[:, :, k:k + 1], 512.0 * k), writes=[thr.r])
    P.op("dve", lambda e: e.tensor_tensor(out=cmp.t[:], in0=run.t[:, :].unsqueeze(2).to_broadcast([128, 8, 8]), in1=thr.t[:], op=ALU.is_gt),
         reads=[run.r, thr.r], writes=[cmp.r])
    P.op("dve", lambda e: e.tensor_reduce(out=tiles.t[:], in_=cmp.t[:], axis=AX.X, op=ALU.add), reads=[cmp.r], writes=[tiles.r])
    prev = tiles
    for i, sh in enumerate((1, 2, 4)):
        cur = ca[i]
        P.op("dve", lambda e, cur=cur, prev=prev: e.tensor_copy(out=cur.t[:], in_=prev.t[:]), reads=[prev.r], writes=[cur.r])
        P.op("dve", lambda e, cur=cur, prev=prev, sh=sh: e.tensor_tensor(out=cur.t[:, sh:8], in0=cur.t[:, sh:8], in1=prev.t[:, 0:8 - sh], op=ALU.add),
             reads=[prev.r, cur.r], writes=[cur.r])
        prev = cur
    incl = prev
    P.op("dve", lambda e: e.tensor_tensor(out=base.t[:], in0=incl.t[:], in1=tiles.t[:], op=ALU.subtract), reads=[incl.r, tiles.r], writes=[base.r])
    P.op("dve", lambda e: e.tensor_scalar(out=base.t[:], in0=base.t[:], scalar1=512.0, scalar2=None, op0=ALU.mult), reads=[base.r], writes=[base.r])
    jv = Buf(P, "rjv", [128, NJ, 8], F32)
    cj = Buf(P, "rcj", [128, NJ, 8], F32)
    ej = Buf(P, "rej", [128, NJ], F32)
    for j in range(NJ):
        P.op("pool", lambda e, j=j: e.memset(jv.t[:, j:j + 1, :], float(j)), writes=[jv.r])
    P.op("dve", lambda e: e.tensor_tensor(out=cj.t[:], in0=incl.t[:, :].unsqueeze(1).to_broadcast([128, NJ, 8]), in1=jv.t[:], op=ALU.is_le),
         reads=[incl.r, jv.r], writes=[cj.r])
    P.op("dve", lambda e: e.tensor_reduce(out=ej.t[:], in_=cj.t[:], axis=AX.X, op=ALU.add), reads=[cj.r], writes=[ej.r])
    P.op("dve", lambda e: e.tensor_scalar(out=ej.t[:], in0=ej.t[:], scalar1=7.0, scalar2=None, op0=ALU.min), reads=[ej.r], writes=[ej.r])
    cgi = Buf(P, "rcgi", [128, 32], I32)
    cgf = Buf(P, "rcgf", [128, 32], F32)
    cdi = Buf(P, "rcdi", [128, 4], I32)
    cdf = Buf(P, "rcdf", [128, 4], F32)
    igf = Buf(P, "rigf", [128, NJ, 32], F32)
    idf = Buf(P, "ridf", [128, NJ, 4], F32)
    igi = Buf(P, "rigi", [128, NJ * 32], I32)
    idi = Buf(P, "ridi", [128, NJ * 4], I32)
    P.op("pool", lambda e: e.iota(cgi.t[:], pattern=[[1, 32]], base=0, channel_multiplier=32), writes=[cgi.r])
    P.op("pool", lambda e: e.iota(cdi.t[:], pattern=[[128, 4]], base=0, channel_multiplier=1), writes=[cdi.r])
    P.op("dve", lambda e: e.tensor_copy(out=cgf.t[:], in_=cgi.t[:]), reads=[cgi.r], writes=[cgf.r])
    P.op("dve", lambda e: e.tensor_copy(out=cdf.t[:], in_=cdi.t[:]), reads=[cdi.r], writes=[cdf.r])
    P.op("dve", lambda e: e.scalar_tensor_tensor(out=igf.t[:], in0=ej.t[:, :].unsqueeze(2).to_broadcast([128, NJ, 32]), scalar=4096.0,
                                                 in1=cgf.t[:, :].unsqueeze(1).to_broadcast([128, NJ, 32]), op0=ALU.mult, op1=ALU.add),
         reads=[ej.r, cgf.r], writes=[igf.r])
    P.op("dve", lambda e: e.scalar_tensor_tensor(out=idf.t[:], in0=ej.t[:, :].unsqueeze(2).to_broadcast([128, NJ, 4]), scalar=512.0,
                                                 in1=cdf.t[:, :].unsqueeze(1).to_broadcast([128, NJ, 4]), op0=ALU.mult, op1=ALU.add),
         reads=[ej.r, cdf.r], writes=[idf.r])
    P.op("dve", lambda e: e.tensor_copy(out=igi.t[:], in_=igf.t[:, :, :].rearrange("p j r -> p (j r)")), reads=[igf.r], writes=[igi.r])
    P.op("dve", lambda e: e.tensor_copy(out=idi.t[:], in_=idf.t[:, :, :].rearrange("p j r -> p (j r)")), reads=[idf.r], writes=[idi.r])
    sl3 = Buf(P, "rsl3", [128, 32, 8], F32)
    slf = Buf(P, "rslf", [128, 2, 32], F32)
    sli = Buf(P, "rsli", [128, 2 * 32], I32)
    for k, EQ in enumerate((EQ1, EQ2)):
        P.op("dve", lambda e, EQ=EQ: e.tensor_tensor(out=sl3.t[:], in0=EQ.t[:], in1=base.t[:, :].unsqueeze(1).to_broadcast([128, 32, 8]), op=ALU.mult),
             reads=[EQ.r, base.r], writes=[sl3.r])
        P.op("dve", lambda e, k=k: e.tensor_reduce(out=slf.t[:, k, :], in_=sl3.t[:], axis=AX.X, op=ALU.add), reads=[sl3.r], writes=[slf.r])
    P.op("dve", lambda e: e.tensor_tensor(out=slf.t[:], in0=slf.t[:], in1=POS.t[:], op=ALU.add), reads=[slf.r, POS.r], writes=[slf.r])
    P.op("dve", lambda e: e.tensor_copy(out=sli.t[:], in_=slf.t[:, :, :].rearrange("p k t -> p (k t)")), reads=[slf.r], writes=[sli.r])
    tokid = Buf(P, "rtokid", [128, 32], I32)
    zi = Buf(P, "rzi", [128, NSLOT // 128], I32)
    P.op("pool", lambda e: e.iota(tokid.t[:], pattern=[[128, 32]], base=0, channel_multiplier=1), writes=[tokid.r])
    P.op("pool", lambda e: e.memset(zi.t[:], 0), writes=[zi.r])
    P.dma("sp", io["slot_tok"].rearrange("(p i) o -> p (i o)", p=128), zi.t[:], reads=[zi.r], writes=[io["r_slot"]])
    for tt in range(32):
        for k in range(2):
            P.dma("pool", None, None, fn=lambda e, tt=tt, k=k: e.indirect_dma_start(
                out=io["slot_tok"][:, :], out_offset=bass.IndirectOffsetOnAxis(ap=sli.t[:, k * 32 + tt:k * 32 + tt + 1], axis=0),
                in_=tokid.t[:, tt:tt + 1], in_offset=None, bounds_check=None),
                reads=[sli.r, tokid.r, io["r_slot"]])
    P.dma("sp", io["t_sli"], sli.t[:], reads=[sli.r], writes=[io["r_tab"]])
    P.dma("sp", io["t_w12"], W12.t[:, :, :].rearrange("p t k -> p (t k)"), reads=[W12.r], writes=[io["r_tab"]])
    P.dma("sp", io["t_igi"], igi.t[:], reads=[igi.r], writes=[io["r_tab"]])
    P.dma("sp", io["t_idi"], idi.t[:], reads=[idi.r], writes=[io["r_tab"]])


def moe_experts(P, C, io, nj=NJ):
    ident = C["ident"]
    sli = Buf(P, "esli", [128, 64], I32)
    W12 = Buf(P, "eW12", [128, 64], F32)
    igi = Buf(P, "eigi", [128, NJ * 32], I32)
    idi = Buf(P, "eidi", [128, NJ * 4], I32)
    P.dma("sp", sli.t[:], io["t_sli"], writes=[sli.r])
    P.dma("sp", W12.t[:], io["t_w12"], writes=[W12.r])
    P.dma("sp", igi.t[:], io["t_igi"], writes=[igi.r])
    P.dma("sp", idi.t[:], io["t_idi"], writes=[idi.r])
    wd_v = io["wd"].rearrange("e (r i) n -> (e r) (i n)", i=7)
    TI = [Buf(P, f"eTI{i}", [128, 4], I32) for i in range(2)]
    Xg = Buf(P, "eXg", [128, 4, D], BF16)
    xgT = [Buf(P, f"exgT{i}", [128, 8, 512], BF16) for i in range(2)]
    Wgu = [Buf(P, f"eWgu{i}", [128, 2, 8, FQ], BF16) for i in range(2)]
    Wd = Buf(P, "eWd", [128, 28, D], BF16)
    rWd = [P.res(f"eWdq{q}") for q in range(4)]
    hT = Buf(P, "ehT", [128, 28, 512], BF16)
    sgt = [Buf(P, f"esg{i}", [128, 512], F32) for i in range(2)]
    ysb = [Buf(P, f"eys{i}", [128, D], F32) for i in range(2)]
    xt = [Buf(P, f"ext{i}", [128, D], F32) for i in range(2)]
    pT2 = [Buf(P, f"epT{i}", [128, 8, 128], BF16, psum=True) for i in range(2)]
    pA = [Buf(P, f"epA{i}", [128, 512], F32, psum=True) for i in range(2)]
    pB = [Buf(P, f"epB{i}", [128, 512], F32, psum=True) for i in range(2)]
    pY = [Buf(P, f"epY{i}", [128, 512], F32, psum=True) for i in range(2)]
    rXg = [P.res(f"eXg{i}") for i in range(4)]
    P.op("pool", lambda e: e.memset(Xg.t[:], 0.0), writes=[Xg.r] + rXg)

    nq = 0
    nev = 0
    ny = 0
    def tok_stage(j):
        ti = TI[j % 2]
        XT = xgT[j % 2]
        P.dma("sp", ti.t[:], io["slot_tok"][j * 512:(j + 1) * 512, :].rearrange("(i p) o -> p (i o)", p=128),
              reads=[io["r_slot"]], writes=[ti.r], allow_slow_non_contiguous=True)
        for i in range(4):
            P.dma("pool", None, None, fn=lambda e, i=i, ti=ti: e.indirect_dma_start(
                out=Xg.t[:, i, :], out_offset=None, in_=io["xn_d"][:, :],
                in_offset=bass.IndirectOffsetOnAxis(ap=ti.t[:, i:i + 1], axis=0), bounds_check=None),
                reads=[ti.r, rXg[i]], writes=[rXg[i]])
        for i in range(4):
            pT = pT2[i % 2]

            def tr(e, i=i, pT=pT):
                src = Xg.t[:, i, :].rearrange("p (m c) -> p c m", c=8)
                for dc in range(8):
                    r = e.transpose(out=pT.t[:, dc, :], in_=src[:, dc, :], identity=ident.t[:])
                return r
            P.op("pe", tr, reads=[rXg[i], ident.r], writes=[pT.r])
            if i % 2 == 0:
                P.op("act", lambda e, i=i, XT=XT, pT=pT: e.copy(out=XT.t[:, :, i * 128:(i + 1) * 128], in_=pT.t[:]), reads=[pT.r], writes=[XT.r])
            else:
                P.op("dve", lambda e, i=i, XT=XT, pT=pT: e.tensor_copy(out=XT.t[:, :, i * 128:(i + 1) * 128], in_=pT.t[:]), reads=[pT.r], writes=[XT.r])

    ys_res = []
    tok_stage(0)
    for j in range(nj):
        XT = xgT[j % 2]
        def wd_dmas(j=j):
            for q in range(4):
                P.dma("pool", None, None, fn=lambda e, q=q, j=j: e.indirect_dma_start(
                    out=Wd.t[:, q * 7:(q + 1) * 7, :].rearrange("p a b -> p (a b)"), out_offset=None, in_=wd_v[:, :],
                    in_offset=bass.IndirectOffsetOnAxis(ap=idi.t[:, j * 4 + q:j * 4 + q + 1], axis=0), bounds_check=None),
                    reads=[idi.r, rWd[q]], writes=[rWd[q]])

        def gu_dmas(q, WGU, j=j):
            P.dma("pool", None, None, fn=lambda e, q=q, j=j, WGU=WGU: e.indirect_dma_start(
                out=WGU.t[:, :, :, :].rearrange("p a b c -> p (a b c)"), out_offset=None, in_=io["wgu"][:, :],
                in_offset=bass.IndirectOffsetOnAxis(ap=idi.t[:, j * 4 + q:j * 4 + q + 1], axis=0), bounds_check=None),
                reads=[idi.r, WGU.r], writes=[WGU.r])

        def gu_compute(q, WGU, XT=XT):
            nonlocal nev
            for i in range(7):
                c = q * 7 + i
                A = pA[nev % 2]
                B = pB[nev % 2]
                SG = sgt[nev % 2]
                nev += 1

                def mm(e, W=WGU, O=A, i=i, XT=XT):
                    for dc in range(8):
                        r = e.matmul(O.t[:], lhsT=W.t[:, 0, dc, i:FQ:7], rhs=XT.t[:, dc, :], start=(dc == 0), stop=(dc == 7))
                    return r
                P.op("pe", mm, reads=[WGU.r, XT.r], writes=[A.r])

                def mm2(e, W=WGU, O=B, i=i, XT=XT):
                    for dc in range(8):
                        r = e.matmul(O.t[:], lhsT=W.t[:, 1, dc, i:FQ:7], rhs=XT.t[:, dc, :], start=(dc == 0), stop=(dc == 7))
                    return r
                P.op("pe", mm2, reads=[WGU.r, XT.r], writes=[B.r])
                P.op("act", lambda e, A=A, SG=SG: e.activation(out=SG.t[:], in_=A.t[:], func=AF.Silu), reads=[A.r], writes=[SG.r])
                P.op("dve", lambda e, B=B, SG=SG, c=c: e.tensor_tensor(out=hT.t[:, c, :], in0=B.t[:], in1=SG.t[:], op=ALU.mult),
                     reads=[B.r, SG.r], writes=[hT.r])

        bufs = [Wgu[(nq + q) % 2] for q in range(4)]
        nq += 4
        gu_dmas(0, bufs[0])
        gu_dmas(1, bufs[1])
        gu_compute(0, bufs[0])
        wd_dmas()
        gu_dmas(2, bufs[2])
        gu_compute(1, bufs[1])
        gu_dmas(3, bufs[3])
        gu_compute(2, bufs[2])
        if j + 1 < nj:
            tok_stage(j + 1)
        gu_compute(3, bufs[3])
        for tt in range(4):
            YS = ysb[ny % 2]
            ny += 1
            for dh in range(2):
                Y = pY[dh]

                def mmd(e, Y=Y, tt=tt, dh=dh):
                    for c in range(28):
                        r = e.matmul(Y.t[:], lhsT=hT.t[:, c, tt * 128:(tt + 1) * 128], rhs=Wd.t[:, c, dh * 512:(dh + 1) * 512],
                                     start=(c == 0), stop=(c == 27))
                    return r
                P.op("pe", mmd, reads=[hT.r] + rWd, writes=[Y.r])
                if dh == 0:
                    P.op("act", lambda e, Y=Y, YS=YS: e.copy(out=YS.t[:, 0:512], in_=Y.t[:]), reads=[Y.r], writes=[YS.r])
                else:
                    P.op("dve", lambda e, Y=Y, YS=YS: e.tensor_copy(out=YS.t[:, 512:1024], in_=Y.t[:]), reads=[Y.r], writes=[YS.r])
            r0 = j * 512 + tt * 128
            rys = P.res(f"ys{j}_{tt}")
            ys_res.append(rys)
            P.dma("act", io["ys_d"][r0:r0 + 128, :], YS.t[:], reads=[YS.r], writes=[rys])

    hflat = hT.t[:, :, :].rearrange("p a b -> p (a b)").bitcast(F32)
    ybufs = [(hflat[:, i * D:(i + 1) * D], P.res(f"yb{i}")) for i in range(6)]
    xbuf = [Buf(P, f"ecx{i}", [128, D], F32) for i in range(1)] + xt
    first = True
    for tt in range(32 if nj == NJ else 0):
        X = xbuf[tt % 3]
        P.dma("sp", X.t[:], io["x_in"][tt * 128:(tt + 1) * 128, :], writes=[X.r])
        for k in range(2):
            yap, yr = ybufs[(tt % 3) * 2 + k]
            extra = [hT.r] if tt < 3 else []
            P.dma("pool", None, None, fn=lambda e, k=k, tt=tt, yap=yap: e.indirect_dma_start(
                out=yap, out_offset=None, in_=io["ys_d"][:, :],
                in_offset=bass.IndirectOffsetOnAxis(ap=sli.t[:, k * 32 + tt:k * 32 + tt + 1], axis=0), bounds_check=None),
                reads=[sli.r, yr] + (ys_res if (tt == 0 and k == 0) else []), writes=[yr] + extra)
            P.op("dve", lambda e, k=k, tt=tt, yap=yap, X=X: e.scalar_tensor_tensor(
                out=X.t[:], in0=yap, scalar=W12.t[:, tt * 2 + k:tt * 2 + k + 1], in1=X.t[:], op0=ALU.mult, op1=ALU.add),
                reads=[yr, W12.r, X.r], writes=[X.r])
        P.dma("act", io["x_out"][tt * 128:(tt + 1) * 128, :], X.t[:], reads=[X.r])

import numpy as np
from concourse.bass_utils import run_bass_kernel_spmd

_NC_CACHE = {}


def build_program(dbg=False):
    nc = bass.Bass("TRN2", target_bir_lowering=False)
    def dt(name, shape, kind="ExternalInput", dtype=F32):
        if dbg and kind == "Internal":
            kind = "ExternalOutput"
        return nc.dram_tensor(name, list(shape), dtype, kind=kind).ap()
    x = dt("x", [S, D])
    mix_norm = dt("mix_norm", [2, D])
    ffn_norm = dt("ffn_norm", [2, D])
    w_in = dt("mlstm_w_in", [D, 3088])
    b_ig = dt("mlstm_b_igate", [8])
    b_fg = dt("mlstm_b_fgate", [8])
    g_h = dt("mlstm_g_h", [D])
    w_out = dt("mlstm_w_out", [D, D])
    kv_norm = dt("kv_norm", [D])
    w_kv = dt("w_kv", [D, 2048])
    g_k = dt("g_k", [64])
    w_q = dt("sb_w_q", [D, D])
    g_q = dt("sb_g_q", [64])
    w_o = dt("sb_w_o", [D, D])
    f_wg = dt("ffn_w_gate", [1, D, 2816])
    f_wu = dt("ffn_w_up", [1, D, 2816])
    f_wd = dt("ffn_w_down", [1, 2816, D])
    m_wr = dt("moe_w_router", [D, 8])
    m_wgu = dt("moe_w_gu", [8 * 4 * 128, 2 * 8 * 896])
    m_wd = dt("moe_w_down", [8, 3584, D])
    out = dt("out", [S, D], kind="ExternalOutput")
    x1 = dt("scr_x1", [S, D], kind="Internal")
    x2 = dt("scr_x2", [S, D], kind="Internal")
    x3 = dt("scr_x3", [S, D], kind="Internal")
    oT = dt("scr_oT", [8, 128, S], kind="Internal", dtype=BF16)

    P = Prog(nc, "p1_")
    C = make_consts(P)
    phase1(P, C, dict(x=x, x1=x1, w_in=w_in, w_out=w_out, mix_norm0=mix_norm[0], g_h=g_h, b_ig=b_ig, b_fg=b_fg))
    P.emit()
    P.close()
    nc.all_engine_barrier()
    P = Prog(nc, "p2_")
    C = make_consts(P)
    ffn_dense(P, C, dict(x_in=x1, x_out=x2, norm_g=ffn_norm[0], wg=f_wg, wu=f_wu, wd=f_wd), 2816, NTOK=1024, tag="f")
    P.emit()
    P.close()
    nc.all_engine_barrier()
    P = Prog(nc, "p3_")
    C = make_consts(P)
    phase3(P, C, dict(x_in=x2, x_out=x3, oT=oT, r_oT=P.res("oT"), mix_norm1=mix_norm[1], kv_norm=kv_norm, g_q=g_q, g_k=g_k,
                      w_q=w_q, w_kv=w_kv, w_o=w_o))
    P.emit()
    P.close()
    nc.all_engine_barrier()
    xn_d = dt("scr_xn", [S, D], kind="Internal", dtype=BF16)
    slot_tok = dt("scr_slot_tok", [NSLOT, 1], kind="Internal", dtype=I32)
    ys_d = dt("scr_ys", [NSLOT, D], kind="Internal")
    t_sli = dt("scr_sli", [128, 64], kind="Internal", dtype=I32)
    t_w12 = dt("scr_w12", [128, 64], kind="Internal")
    t_igi = dt("scr_igi", [128, NJ * 32], kind="Internal", dtype=I32)
    t_idi = dt("scr_idi", [128, NJ * 4], kind="Internal", dtype=I32)
    io4 = dict(x_in=x3, x_out=out, norm_g=ffn_norm[1], router=m_wr, wgu=m_wgu, wd=m_wd, xn_d=xn_d, slot_tok=slot_tok,
               ys_d=ys_d, t_sli=t_sli, t_w12=t_w12, t_igi=t_igi, t_idi=t_idi)
    P = Prog(nc, "p4a_")
    for k in ("r_xn", "r_slot", "r_ys", "r_tab"):
        io4[k] = P.res(k)
    C = make_consts(P)
    moe_route(P, C, io4)
    P.emit()
    P.close()
    nc.all_engine_barrier()
    P = Prog(nc, "p4b_")
    for k in ("r_xn", "r_slot", "r_ys", "r_tab"):
        io4[k] = P.res(k)
    C = make_consts(P)
    moe_experts(P, C, io4)
    P.emit()
    P.close()
    return nc


def kernel(**inputs):
    if "nc" not in _NC_CACHE:
        _NC_CACHE["nc"] = build_program()
    nc = _NC_CACHE["nc"]
    f = lambda k: np.ascontiguousarray(np.asarray(inputs[k], dtype=np.float32))
    shared = {
        "mix_norm": f("mix_norm"), "ffn_norm": f("ffn_norm"),
        "mlstm_w_in": f("mlstm_w_in")[0], "mlstm_b_igate": f("mlstm_b_igate")[0], "mlstm_b_fgate": f("mlstm_b_fgate")[0],
        "mlstm_g_h": f("mlstm_g_h")[0], "mlstm_w_out": f("mlstm_w_out")[0],
        "kv_norm": f("kv_norm"), "w_kv": f("w_kv"), "g_k": f("g_k"),
        "sb_w_q": f("sb_w_q")[0], "sb_g_q": f("sb_g_q")[0], "sb_w_o": f("sb_w_o")[0],
        "ffn_w_gate": f("ffn_w_gate"), "ffn_w_up": f("ffn_w_up"), "ffn_w_down": f("ffn_w_down"),
        "moe_w_router": f("moe_w_router")[0], "moe_w_down": f("moe_w_down")[0],
    }
    gu = np.stack([f("moe_w_gate")[0], f("moe_w_up")[0]], axis=1).reshape(8, 2, 128, 8, 4, 896)
    shared["moe_w_gu"] = np.ascontiguousarray(gu.transpose(0, 4, 2, 1, 3, 5)).reshape(8 * 4 * 128, 2 * 8 * 896)
    xs = f("x")
    in_maps = [dict(shared, x=np.ascontiguousarray(xs[b])) for b in range(8)]
    res = run_bass_kernel_spmd(nc, in_maps, core_ids=list(range(8)))
    return np.stack([np.asarray(res.results[b]["out"], dtype=np.float32) for b in range(8)], axis=0)
```
